# Optimizing a Trainium2 kernel written in Bass

```python
import math
import jax
import jax.numpy as jnp
from jax import lax
import numpy as np

D_MODEL = 1024
BATCH = 4
SEQ = 4096
DEPTH = 2

N_MEM = 256
EPS = 1e-6
HEAD_DIM = 128
RET_HEADS = D_MODEL // (2 * HEAD_DIM)
RET_DK = HEAD_DIM
RET_DV = HEAD_DIM
RET_CHUNK = 128
ROPE_BASE = 10000.0
HG_HEADS = D_MODEL // (2 * HEAD_DIM)
HG_DK = HEAD_DIM
HG_DV = HEAD_DIM
HG_CHUNK = 64
F_FLOOR = 1e-6
DSA_HEADS = D_MODEL // HEAD_DIM
DSA_HD = HEAD_DIM
DSA_BRANCHES = ((128, 1), (512, 4), (2048, 16))
DSA_BLOCK = 128
REL_BUCKETS = 32
REL_MAX_DIST = 2048
XA_HEADS = 4
XA_HD = D_MODEL // XA_HEADS
D_FF = ((8 * D_MODEL // 3 + 127) // 128) * 128
CONV_W = 3

N_EVEN = (DEPTH + 1) // 2
N_ODD = DEPTH // 2
EV_SIZES = (RET_HEADS * RET_DK, RET_HEADS * RET_DK, RET_HEADS * RET_DV, RET_HEADS * RET_DV,
            HG_HEADS * HG_DK, HG_HEADS * HG_DK, HG_HEADS * HG_DV, HG_HEADS * HG_DV)
EV_IN = sum(EV_SIZES)
EV_OUT = RET_HEADS * RET_DV + HG_HEADS * HG_DV
OD_IN = 3 * DSA_HEADS * DSA_HD
OD_OUT = DSA_HEADS * DSA_HD

kernel_name = 'hybrid_retention_hgrn2_dilated_attention_block'


def _rms(x, g):
    xf = x.astype(jnp.float32)
    y = xf * lax.rsqrt(jnp.mean(xf * xf, axis=-1, keepdims=True) + EPS)
    return (y * g.astype(jnp.float32)).astype(x.dtype)


def _head_layer_norm(x, g):
    mu = jnp.mean(x, axis=-1, keepdims=True)
    xc = x - mu
    var = jnp.mean(xc * xc, axis=-1, keepdims=True)
    return xc * lax.rsqrt(var + EPS) * g.astype(jnp.float32)[None, :, None, :]


def _head_rms_norm(x, g):
    y = x * lax.rsqrt(jnp.mean(x * x, axis=-1, keepdims=True) + EPS)
    return y * g.astype(jnp.float32)[None, :, None, :]


def _split_heads(t, n_heads):
    b, s, _ = t.shape
    return t.reshape(b, s, n_heads, -1).transpose(0, 2, 1, 3)


def _merge_heads(t):
    b, h, s, d = t.shape
    return t.transpose(0, 2, 1, 3).reshape(b, s, h * d)


def _split_cols(t, sizes):
    parts, off = [], 0
    for n in sizes:
        parts.append(t[..., off:off + n])
        off += n
    return parts


def _rotary(x, positions):
    half = x.shape[-1] // 2
    inv = 1.0 / (ROPE_BASE ** (jnp.arange(half, dtype=jnp.float32) / half))
    ang = positions.astype(jnp.float32)[:, None] * inv[None, :]
    cos, sin = jnp.cos(ang), jnp.sin(ang)
    xf = x.astype(jnp.float32)
    x1, x2 = xf[..., :half], xf[..., half:]
    return jnp.concatenate([x1 * cos - x2 * sin, x1 * sin + x2 * cos], axis=-1)


def _retention_chunkwise(q, k, v):
    b, h, s, dk = q.shape
    dv = v.shape[-1]
    c = RET_CHUNK
    nc = s // c
    log_gamma = jnp.log(1.0 - jnp.exp2(-5.0 - jnp.arange(h, dtype=jnp.float32)))
    idx = jnp.arange(c, dtype=jnp.float32)
    diff = idx[:, None] - idx[None, :]
    decay = jnp.where(diff >= 0, jnp.exp(log_gamma[:, None, None] * jnp.maximum(diff, 0.0)), 0.0)
    xi = jnp.exp(log_gamma[:, None] * (idx + 1.0))
    zeta = jnp.exp(log_gamma[:, None] * (c - 1.0 - idx))
    chunk_decay = jnp.exp(log_gamma * c)
    qc = q.astype(jnp.float32).reshape(b, h, nc, c, dk)
    kc = k.astype(jnp.float32).reshape(b, h, nc, c, dk)
    vc = v.astype(jnp.float32).reshape(b, h, nc, c, dv)
    scores = jnp.einsum('bhnid,bhnjd->bhnij', qc, kc) * decay[None, :, None]
    inner = jnp.einsum('bhnij,bhnjv->bhniv', scores, vc)
    chunk_kv = jnp.einsum('bhnjd,bhnjv->bhndv', kc * zeta[None, :, None, :, None], vc)

    def step(state, kv):
        return kv + chunk_decay[None, :, None, None] * state, state

    _, prev = lax.scan(step, jnp.zeros((b, h, dk, dv), jnp.float32), jnp.moveaxis(chunk_kv, 2, 0))
    prev = jnp.moveaxis(prev, 0, 2)
    cross = jnp.einsum('bhnid,bhndv->bhniv', qc, prev) * xi[None, :, None, :, None]
    return (inner + cross).reshape(b, h, s, dv)


def _hgrn2_chunkwise(q, f_logit, i, lb):
    b, h, s, dk = q.shape
    dv = i.shape[-1]
    c = HG_CHUNK
    nc = s // c
    lbb = lb.astype(jnp.float32)[None, :, None, :]
    f = lbb + (1.0 - lbb) * jax.nn.sigmoid(f_logit.astype(jnp.float32))
    log_f = jnp.log(jnp.maximum(f, F_FLOOR))
    key = 1.0 - f

    def chunks(t):
        return jnp.moveaxis(t.reshape(b, h, nc, c, t.shape[-1]), 2, 0)

    causal = jnp.tril(jnp.ones((c, c), dtype=bool))[None, None, :, :, None]

    def step(state, xs):
        qn, kn, vn, lfn = xs
        cum = jnp.cumsum(lfn, axis=2)
        rel = cum[:, :, :, None, :] - cum[:, :, None, :, :]
        w = jnp.exp(jnp.where(causal, rel, -jnp.inf))
        scores = jnp.sum(qn[:, :, :, None, :] * kn[:, :, None, :, :] * w, axis=-1)
        out = (jnp.einsum('bhts,bhsv->bhtv', scores, vn)
               + jnp.einsum('bhtc,bhcv->bhtv', qn * jnp.exp(cum), state))
        last = cum[:, :, -1:, :]
        new_state = (jnp.exp(last[:, :, 0, :])[..., None] * state
                     + jnp.einsum('bhsc,bhsv->bhcv', kn * jnp.exp(last - cum), vn))
        return new_state, out

    _, outs = lax.scan(step, jnp.zeros((b, h, dk, dv), jnp.float32),
                       (chunks(q.astype(jnp.float32)), chunks(key), chunks(i.astype(jnp.float32)), chunks(log_f)))
    return jnp.moveaxis(outs, 0, 2).reshape(b, h, s, dv)


def _t5_bucket(dist):
    exact = REL_BUCKETS // 2
    d = jnp.maximum(dist, 0)
    log_ratio = jnp.log(jnp.maximum(d, 1).astype(jnp.float32) / exact) / math.log(REL_MAX_DIST / exact)
    large = jnp.minimum(exact + (log_ratio * (REL_BUCKETS - exact)).astype(jnp.int32), REL_BUCKETS - 1)
    return jnp.where(d < exact, d, large)


def _dilated_branch(q, k, v, rel_bias, window, dil):
    b, h, s, hd = q.shape
    blk = DSA_BLOCK
    n_back = window // dil
    length = s // dil
    nb = -(-length // blk)
    padded = nb * blk

    def to_blocks(t):
        t = t.reshape(b, h, length, dil, hd).transpose(0, 1, 3, 2, 4)
        t = jnp.pad(t, ((0, 0), (0, 0), (0, 0), (0, padded - length), (0, 0)))
        return t.reshape(b, h, dil, nb, blk, hd)

    def band(t):
        tp = jnp.pad(t, ((0, 0), (0, 0), (0, 0), (1, 0), (0, 0), (0, 0)))
        return jnp.concatenate([tp[:, :, :, :-1], tp[:, :, :, 1:]], axis=4)

    qb = to_blocks(q)
    kb = band(to_blocks(k))
    vb = band(to_blocks(v))
    logits = jnp.einsum('bhrnqd,bhrnkd->bhrnqk', qb, kb).astype(jnp.float32) * (hd ** -0.5)
    qi = jnp.arange(blk)
    ki = jnp.arange(2 * blk)
    delta = qi[:, None] + blk - ki[None, :]
    key_idx = jnp.arange(nb)[:, None] * blk - blk + ki[None, :]
    valid = ((delta >= 0) & (delta <= n_back))[None] & (key_idx >= 0)[:, None, :]
    bias = rel_bias[_t5_bucket(delta * dil)].transpose(2, 0, 1).astype(jnp.float32)
    logits = jnp.where(valid[None, None, None], logits + bias[None, :, None, None], -jnp.inf)
    m = jnp.max(logits, axis=-1)
    p = jnp.exp(logits - m[..., None])
    den = jnp.sum(p, axis=-1)
    num = jnp.einsum('bhrnqk,bhrnkd->bhrnqd', p, vb.astype(jnp.float32))

    def to_seq(t):
        t = t.reshape((b, h, dil, padded) + t.shape[5:])[:, :, :, :length]
        t = jnp.swapaxes(t, 2, 3)
        return t.reshape((b, h, s) + t.shape[4:])

    return to_seq(m), to_seq(den), to_seq(num)


def _dilated_attention(q, k, v, rel_bias):
    ms, dens, nums = [], [], []
    for window, dil in DSA_BRANCHES:
        m, den, num = _dilated_branch(q, k, v, rel_bias, window, dil)
        ms.append(m)
        dens.append(den)
        nums.append(num)
    m_all = jnp.max(jnp.stack(ms, axis=0), axis=0)
    w = [jnp.exp(mi - m_all) for mi in ms]
    den_all = w[0] * dens[0] + w[1] * dens[1] + w[2] * dens[2]
    num_all = w[0][..., None] * nums[0] + w[1][..., None] * nums[1] + w[2][..., None] * nums[2]
    return num_all / den_all[..., None]


def _even_mixer(h, w_in, ret_norm_g, hg_norm_g, lb, w_out):
    b, s, _ = h.shape
    rq, rk, rv, rg, gq, gf, gi, gg = _split_cols(h @ w_in, EV_SIZES)
    pos = jnp.arange(s)
    rq = _rotary(_split_heads(rq, RET_HEADS), pos)
    rk = _rotary(_split_heads(rk, RET_HEADS), pos) * (RET_DK ** -0.5)
    ret = _retention_chunkwise(rq, rk, _split_heads(rv, RET_HEADS))
    ret = _merge_heads(_head_layer_norm(ret, ret_norm_g)) * jax.nn.silu(rg.astype(jnp.float32))
    hg = _hgrn2_chunkwise(_split_heads(gq, HG_HEADS), _split_heads(gf, HG_HEADS), _split_heads(gi, HG_HEADS), lb)
    hg = _merge_heads(_head_rms_norm(hg, hg_norm_g)) * jax.nn.silu(gg.astype(jnp.float32))
    y = jnp.concatenate([ret, hg], axis=-1).astype(h.dtype)
    return y @ w_out


def _odd_mixer(h, w_in, q_norm_g, k_norm_g, rel_bias, w_out):
    q, k, v = _split_cols(h @ w_in, (OD_OUT, OD_OUT, OD_OUT))
    q = _rms(_split_heads(q, DSA_HEADS), q_norm_g)
    k = _rms(_split_heads(k, DSA_HEADS), k_norm_g)
    o = _dilated_attention(q, k, _split_heads(v, DSA_HEADS), rel_bias)
    return _merge_heads(o).astype(h.dtype) @ w_out


def _memory_cross_attention(h, mem_h, w_q, w_kv, q_norm_g, k_norm_g, w_o):
    q = _rms(_split_heads(h @ w_q, XA_HEADS), q_norm_g)
    mk, mv = _split_cols(mem_h @ w_kv, (XA_HEADS * XA_HD, XA_HEADS * XA_HD))
    mk = _rms(_split_heads(mk, XA_HEADS), k_norm_g)
    mv = _split_heads(mv, XA_HEADS)
    logits = jnp.einsum('bhqd,bhkd->bhqk', q, mk).astype(jnp.float32) * (XA_HD ** -0.5)
    p = jax.nn.softmax(logits, axis=-1)
    o = jnp.einsum('bhqk,bhkd->bhqd', p, mv.astype(jnp.float32))
    return _merge_heads(o).astype(h.dtype) @ w_o


def _causal_dwconv(u, w, bias):
    kw = w.shape[0]
    s = u.shape[1]
    up = jnp.pad(u, ((0, 0), (kw - 1, 0), (0, 0)))
    y = up[:, 0:s] * w[0]
    for j in range(1, kw):
        y = y + up[:, j:j + s] * w[j]
    return y + bias


def _conv_ffn(h, w_in, conv_w, conv_b, w_out):
    gate, up = _split_cols(h @ w_in, (D_FF, D_FF))
    gate = _causal_dwconv(gate, conv_w, conv_b)
    return (jax.nn.gelu(gate) * up) @ w_out


def setup_inputs(seed: int = 0) -> dict:
    key = jax.random.key(seed)
    ks = iter(jax.random.split(key, 32))

    def nrm(shape, scale):
        return jax.random.normal(next(ks), shape, jnp.float32) * scale

    def gain(shape):
        return 1.0 + nrm(shape, 0.05)

    out_scale = 0.5
    return {
        'x': nrm((BATCH, SEQ, D_MODEL), 1.0),
        'mem': nrm((BATCH, N_MEM, D_MODEL), 1.0),
        'mix_norm_g': gain((DEPTH, D_MODEL)),
        'ev_w_in': nrm((N_EVEN, D_MODEL, EV_IN), D_MODEL ** -0.5),
        'ev_ret_norm_g': gain((N_EVEN, RET_HEADS, RET_DV)),
        'ev_hg_norm_g': gain((N_EVEN, HG_HEADS, HG_DV)),
        'hg_lb_logits': nrm((DEPTH + 1, HG_HEADS * HG_DK), 0.5),
        'ev_w_out': nrm((N_EVEN, EV_OUT, D_MODEL), out_scale * EV_OUT ** -0.5),
        'od_w_in': nrm((N_ODD, D_MODEL, OD_IN), D_MODEL ** -0.5),
        'od_q_norm_g': gain((N_ODD, DSA_HD)),
        'od_k_norm_g': gain((N_ODD, DSA_HD)),
        'rel_bias': nrm((REL_BUCKETS, DSA_HEADS), 0.5),
        'od_w_out': nrm((N_ODD, OD_OUT, D_MODEL), out_scale * OD_OUT ** -0.5),
        'xa_norm_g': gain((DEPTH, D_MODEL)),
        'xa_mem_norm_g': gain((DEPTH, D_MODEL)),
        'xa_w_q': nrm((DEPTH, D_MODEL, XA_HEADS * XA_HD), D_MODEL ** -0.5),
        'xa_w_kv': nrm((DEPTH, D_MODEL, 2 * XA_HEADS * XA_HD), D_MODEL ** -0.5),
        'xa_q_norm_g': gain((DEPTH, XA_HD)),
        'xa_k_norm_g': gain((DEPTH, XA_HD)),
        'xa_w_o': nrm((DEPTH, XA_HEADS * XA_HD, D_MODEL), out_scale * (XA_HEADS * XA_HD) ** -0.5),
        'ffn_norm_g': gain((DEPTH, D_MODEL)),
        'ffn_w_in': nrm((DEPTH, D_MODEL, 2 * D_FF), D_MODEL ** -0.5),
        'ffn_conv_w': nrm((DEPTH, CONV_W, D_FF), CONV_W ** -0.5),
        'ffn_conv_b': nrm((DEPTH, D_FF), 0.02),
        'ffn_w_out': nrm((DEPTH, D_FF, D_MODEL), out_scale * D_FF ** -0.5),
    }


def reference(x, mem, mix_norm_g, ev_w_in, ev_ret_norm_g, ev_hg_norm_g, hg_lb_logits, ev_w_out,
              od_w_in, od_q_norm_g, od_k_norm_g, rel_bias, od_w_out,
              xa_norm_g, xa_mem_norm_g, xa_w_q, xa_w_kv, xa_q_norm_g, xa_k_norm_g, xa_w_o,
              ffn_norm_g, ffn_w_in, ffn_conv_w, ffn_conv_b, ffn_w_out):
    lb_all = jnp.cumsum(jax.nn.softmax(hg_lb_logits.astype(jnp.float32), axis=0), axis=0)
    for l in range(DEPTH):
        h = _rms(x, mix_norm_g[l])
        if l % 2 == 0:
            e = l // 2
            lb = lb_all[l].reshape(HG_HEADS, HG_DK)
            x = x + _even_mixer(h, ev_w_in[e], ev_ret_norm_g[e], ev_hg_norm_g[e], lb, ev_w_out[e])
        else:
            o = l // 2
            x = x + _odd_mixer(h, od_w_in[o], od_q_norm_g[o], od_k_norm_g[o], rel_bias, od_w_out[o])
        x = x + _memory_cross_attention(_rms(x, xa_norm_g[l]), _rms(mem, xa_mem_norm_g[l]),
                                        xa_w_q[l], xa_w_kv[l], xa_q_norm_g[l], xa_k_norm_g[l], xa_w_o[l])
        x = x + _conv_ffn(_rms(x, ffn_norm_g[l]), ffn_w_in[l], ffn_conv_w[l], ffn_conv_b[l], ffn_w_out[l])
    return x
```

```python
import numpy as np
from contextlib import ExitStack
import concourse.bass as bass
import concourse.mybir as mybir
from concourse.bass_utils import run_bass_kernel_spmd

EPOCH = 4000
DMA_SLOTS = 16


class Sched:
    ENGS = ("pe", "act", "dve", "pool", "sp")

    def __init__(self, nc, stack: ExitStack):
        self.nc = nc
        self.stack = stack
        self.ops = []
        self.last_w = {}
        self.readers = {}
        self.dma_count = {}
        self.dma_last_slot = {}
        self.serial = False

    def _deps(self, reads, writes):
        deps = set()
        for k in reads:
            w = self.last_w.get(k)
            if w is not None:
                deps.add(w)
        for k in writes:
            w = self.last_w.get(k)
            if w is not None:
                deps.add(w)
            deps.update(self.readers.get(k, ()))
        return deps

    def _commit(self, oid, reads, writes):
        for k in writes:
            self.last_w[k] = oid
            self.readers[k] = []
        for k in reads:
            if k in writes:
                continue
            self.readers.setdefault(k, []).append(oid)

    def op(self, eng, fn, reads=(), writes=()):
        oid = len(self.ops)
        deps = self._deps(reads, writes)
        if self.serial and oid > 0 and self.ops[oid - 1]["kind"] in ("op", "dma", "cc"):
            deps.add(oid - 1)
        self.ops.append(dict(id=oid, eng=eng, fn=fn, deps=deps, kind="op"))
        self._commit(oid, reads, writes)
        return oid

    def dma(self, queue, out, in_, reads=(), writes=(), **kw):
        oid = len(self.ops)
        deps = self._deps(reads, writes)
        if self.serial and oid > 0 and self.ops[oid - 1]["kind"] in ("op", "dma", "cc"):
            deps.add(oid - 1)
        n = self.dma_count.get(queue, 0)
        self.dma_count[queue] = n + 1
        slot = n % DMA_SLOTS
        prev = self.dma_last_slot.get((queue, slot))
        if prev is not None:
            deps.add(prev)
        self.dma_last_slot[(queue, slot)] = oid
        fn = lambda e: e.dma_start(out=out, in_=in_, **kw)
        self.ops.append(dict(id=oid, eng=queue, fn=fn, deps=deps, kind="dma",
                             slot=slot, val=16 * (n // DMA_SLOTS + 1)))
        self._commit(oid, reads, writes)
        return oid

    def cc(self, kind, alu, groups, in_ap, out_ap, reads=(), writes=()):
        oid = len(self.ops)
        deps = self._deps(reads, writes)
        fn = lambda e: e.collective_compute(kind, alu, replica_groups=groups, ins=[in_ap], outs=[out_ap])
        self.ops.append(dict(id=oid, eng="pool", fn=fn, deps=deps, kind="cc"))
        self._commit(oid, reads, writes)
        return oid

    def barrier(self):
        last = {}
        for o in self.ops:
            if o["kind"] == "dma":
                last[("d", o["eng"], o["slot"])] = o["id"]
            elif o["kind"] == "cc":
                last[("c", o["id"])] = o["id"]
            elif o["kind"] == "op":
                last[("e", o["eng"])] = o["id"]
        deps = set(last.values())
        for e in self.ENGS:
            self.ops.append(dict(id=len(self.ops), eng=e, fn=None, deps=set(deps), kind="bar"))
        self.last_w = {}
        self.readers = {}

    def finalize(self, final_wait_ops=()):
        nc = self.nc
        ops = self.ops
        fin = dict(id=len(ops), eng="sp", fn=None, deps=set(final_wait_ops), kind="fin")
        ops.append(fin)
        needed = set()
        for o in ops:
            for d in list(o["deps"]):
                dop = ops[d]
                if dop["kind"] == "op" and dop["eng"] == "pe" and o["eng"] == "pe" and o["kind"] == "op":
                    o["deps"].discard(d)
                    continue
                needed.add(d)
        cnt = {e: 0 for e in self.ENGS}
        for o in ops:
            if o["kind"] == "op" and o["id"] in needed:
                cnt[o["eng"]] += 1
                o["cval"] = cnt[o["eng"]]
        sems = {}
        for e in self.ENGS:
            n = (cnt[e] + EPOCH - 1) // EPOCH
            sems[e] = [self.stack.enter_context(nc.semaphore(f"s_{e}_{i}")) for i in range(max(n, 1))]
        dsems = {}
        for q in self.dma_count:
            dsems[q] = [self.stack.enter_context(nc.semaphore(f"d_{q}_{i}")) for i in range(DMA_SLOTS)]
        streams = {e: [] for e in self.ENGS}
        for o in ops:
            streams[o["eng"]].append(o)
            if o["kind"] == "cc":
                o["sem"] = self.stack.enter_context(nc.semaphore(f"cc_{o['id']}"))
        self.stats = {e: len(streams[e]) for e in self.ENGS}
        self.stats["needed"] = dict(cnt)

        def emit(eng_name, e):
            clock = {}
            for o in streams[eng_name]:
                waits = {}
                for d in o["deps"]:
                    dop = ops[d]
                    if dop["kind"] == "dma":
                        key = ("d", dop["eng"], dop["slot"])
                        val = dop["val"]
                    elif dop["kind"] == "cc":
                        key = ("c", dop["id"])
                        val = 1
                    else:
                        key = ("e", dop["eng"])
                        val = dop["cval"]
                    if clock.get(key, 0) >= val:
                        continue
                    if waits.get(key, 0) < val:
                        waits[key] = val
                for key, val in waits.items():
                    clock[key] = val
                    if key[0] == "d":
                        e.wait_ge(dsems[key[1]][key[2]], val)
                    elif key[0] == "c":
                        e.wait_ge(ops[key[1]]["sem"], 1)
                    else:
                        idx, loc = (val - 1) // EPOCH, (val - 1) % EPOCH + 1
                        e.wait_ge(sems[key[1]][idx], loc)
                if o["kind"] in ("fin", "bar"):
                    continue
                ins = o["fn"](e)
                if o["kind"] == "dma":
                    ins.then_inc(dsems[o["eng"]][o["slot"]], 16)
                elif o["kind"] == "cc":
                    ins.then_inc(o["sem"])
                elif "cval" in o:
                    v = o["cval"]
                    ins.then_inc(sems[eng_name][(v - 1) // EPOCH], 1)

        with nc.Block() as block:
            @block.tensor
            def _(e):
                emit("pe", e)

            @block.scalar
            def _(e):
                emit("act", e)

            @block.vector
            def _(e):
                emit("dve", e)

            @block.gpsimd
            def _(e):
                emit("pool", e)

            @block.sync
            def _(e):
                emit("sp", e)

F32 = mybir.dt.float32
BF16 = mybir.dt.bfloat16
AF = mybir.ActivationFunctionType
ALU = mybir.AluOpType
AX = mybir.AxisListType

D = 1024
DFF = 2816
NFF = DFF // 128
EPS = 1e-6


class Ctx:
    def __init__(self, nc, S):
        self.nc = nc
        self.S = S
        self.uid = 0

    def sb(self, st, shape, dt, name=None):
        self.uid += 1
        return st.enter_context(self.nc.sbuf_tensor(f"{name or 't'}_{self.uid}", list(shape), dt))

    def ps(self, st, shape, dt, name=None):
        self.uid += 1
        return st.enter_context(self.nc.psum_tensor(f"{name or 'p'}_{self.uid}", list(shape), dt))


def load_weight_bf16(C, W_dram, Wsb, stages, nk, ncols, gk=None, colblk=2048, tag="w", dmaq="sp"):
    S = C.S
    for k in range(nk):
        for c0 in range(0, ncols, colblk):
            cw = min(colblk, ncols - c0)
            i = C.stg_i = getattr(C, "stg_i", -1) + 1
            stg = stages[i % len(stages)]
            skey = ("stg", i % len(stages))
            S.dma(dmaq, stg[:, 0:cw], W_dram[k * 128:(k + 1) * 128, c0:c0 + cw], writes=[skey])
            eng = ("act", "dve")[i % 2]
            dst = Wsb[:, k, c0:c0 + cw]
            if gk is None:
                if eng == "act":
                    S.op("act", lambda e, d=dst, s=stg[:, 0:cw]: e.copy(out=d, in_=s), reads=[skey], writes=[(tag, k, c0)])
                else:
                    S.op(eng, lambda e, d=dst, s=stg[:, 0:cw]: e.tensor_copy(out=d, in_=s), reads=[skey], writes=[(tag, k, c0)])
            else:
                g = gk[:, k:k + 1]
                if eng == "act":
                    S.op("act", lambda e, d=dst, s=stg[:, 0:cw], g=g: e.activation(out=d, in_=s, func=AF.Copy, scale=g),
                         reads=[skey, "gk"], writes=[(tag, k, c0)])
                else:
                    S.op(eng, lambda e, d=dst, s=stg[:, 0:cw], g=g: e.tensor_scalar(out=d, in0=s, scalar1=g, scalar2=None, op0=ALU.mult),
                         reads=[skey, "gk"], writes=[(tag, k, c0)])


def wkeys(tag, nk, ncols, colblk=2048):
    return [(tag, k, c0) for k in range(nk) for c0 in range(0, ncols, colblk)]


def front_vec(C, bufs, x_dram, tok0, sub, par):
    S = C.S
    xin = bufs["xin"][sub % 2]
    nh = len(bufs["h"])
    hb = bufs["h"][sub % nh]
    ss = bufs["ss"][sub % 2]
    junk = bufs["junk"]
    kx = ("xin", sub % 2)
    kh = ("h", sub % nh)
    ks = ("ss", sub % 2)
    S.dma("sp", xin[:], x_dram[tok0:tok0 + 128, :], writes=[kx])
    S.op("act", lambda e: e.activation(out=junk[:], in_=xin[:], func=AF.Square),
         reads=[kx], writes=["junk"])
    S.op("dve", lambda e: e.tensor_reduce(out=ss[:, 0:1], in_=junk[:], axis=AX.X, op=ALU.add),
         reads=["junk"], writes=[ks])
    S.op("act", lambda e: e.activation(out=ss[:, 1:2], in_=ss[:, 0:1], func=AF.Ln, scale=1.0 / D, bias=EPS),
         reads=[ks], writes=[ks])
    S.op("act", lambda e: e.activation(out=ss[:, 2:3], in_=ss[:, 1:2], func=AF.Exp, scale=-0.5),
         reads=[ks], writes=[ks])
    S.op("act", lambda e: e.activation(out=hb[:], in_=xin[:], func=AF.Copy, scale=ss[:, 2:3]),
         reads=[kx, ks], writes=[kh])


def front_pe(C, bufs, sub, par, TT):
    S = C.S
    nh = len(bufs["h"])
    hb = bufs["h"][sub % nh]
    kh = ("h", sub % nh)
    pT = bufs["psT"][sub % 2]
    kp = ("psT", sub % 2)
    ident = bufs["ident"]
    hT = bufs["hT"][par]
    for k in range(8):
        S.op("pe", lambda e, k=k: e.transpose(out=pT[:, k * 128:(k + 1) * 128], in_=hb[:, k * 128:(k + 1) * 128], identity=ident[:]),
             reads=[kh, "ident"], writes=[kp])
    eng = "dve" if sub % 2 == 0 else "act"
    src = pT[:].rearrange("p (k t) -> p k t", k=8)
    dst = hT[:, :, sub * 128:(sub + 1) * 128]
    if eng == "act":
        S.op("act", lambda e: e.copy(out=dst, in_=src), reads=[kp], writes=[("hT", par, sub)])
    else:
        S.op("dve", lambda e: e.tensor_copy(out=dst, in_=src), reads=[kp], writes=[("hT", par, sub)])


def ffn_phase(C, x_in, x_out, w_in_d, w_out_d, pv, ident_d, T, TT=256, halo_x=None):
    S = C.S
    nc = C.nc
    NS = TT // 128
    NT = T // TT
    NB = 5
    with ExitStack() as st:
        W1 = C.sb(st, [128, 8, 2 * DFF], BF16, "W1")
        W2 = C.sb(st, [128, NFF, D], BF16, "W2")
        stages = [C.sb(st, [128, 2048], F32, "stg") for _ in range(2)]
        pvs = C.sb(st, [128, 8 + NFF * 4], F32, "pv")
        ident = C.sb(st, [128, 128], BF16, "ident")
        bufs = dict(
            xin=[C.sb(st, [128, D], F32, "xin") for _ in range(2)],
            h=[C.sb(st, [128, D], BF16, "h") for _ in range(2)],
            ss=[C.sb(st, [128, 4], F32, "ss") for _ in range(2)],
            junk=C.sb(st, [128, D], F32, "junk"),
            hT=[C.sb(st, [128, 8, TT], BF16, "hT") for _ in range(2)],
            psT=[C.ps(st, [128, D], BF16, "psT") for _ in range(2)],
            ident=ident,
        )
        actT = C.sb(st, [128, NFF, TT], BF16, "actT")
        G = [C.sb(st, [128, TT + 2], F32, "G") for _ in range(NB)]
        acc = [C.sb(st, [128, TT], F32, "acc") for _ in range(NB)]
        ge = [C.sb(st, [128, TT], F32, "ge") for _ in range(NB)]
        halo = C.sb(st, [128, NFF, 2], F32, "halo")
        xres = [C.sb(st, [128, D], F32, "xres") for _ in range(2)]
        psG = [C.ps(st, [128, 2, TT], F32, "psG") for _ in range(5)]
        psO = [C.ps(st, [128, 512], F32, "psO") for _ in range(1)]

        S.dma("sp", pvs[:], pv[:, :], writes=["gk"])
        S.dma("sp", ident[:], ident_d[:, :], writes=["ident"])
        S.op("pool", lambda e: e.memset(halo[:], 0.0), writes=[("halo", c) for c in range(NFF)])
        gk = pvs[:, 0:8]
        if halo_x is not None:
            front_vec(C, bufs, x_in, T - 128, 0, 1)
            front_pe(C, bufs, 0, 1, TT)
        w1v = w_in_d.rearrange("(k p) c -> p k c", p=128)
        ci = 0
        for half in range(2):
            for j in range(NFF // 2):
                c0 = half * DFF + j * 256
                i = C.stg_i = getattr(C, "stg_i", -1) + 1
                stg = stages[i % 2]
                skey = ("stg", i % 2)
                sv = stg[:].rearrange("p (k c) -> p k c", k=8)
                S.dma("sp", sv, w1v[:, :, c0:c0 + 256], writes=[skey])
                for k in range(8):
                    eng = ("act", "dve")[ci % 2]
                    ci += 1
                    dst = W1[:, k, c0:c0 + 256]
                    src = sv[:, k, :]
                    g = gk[:, k:k + 1]
                    if eng == "act":
                        S.op("act", lambda e, d=dst, s=src, g=g: e.activation(out=d, in_=s, func=AF.Copy, scale=g), reads=[skey, "gk"], writes=[("W1", half, j)])
                    else:
                        S.op(eng, lambda e, d=dst, s=src, g=g: e.tensor_scalar(out=d, in0=s, scalar1=g, scalar2=None, op0=ALU.mult), reads=[skey, "gk"], writes=[("W1", half, j)])
        load_weight_bf16(C, w_out_d, W2, stages, NFF, D, gk=None, tag="W2")
        W1k = [("W1", half, j) for half in range(2) for j in range(NFF // 2)]
        W2k = wkeys("W2", NFF, D)
        cwv = lambda c, j: pvs[:, 8 + c * 3 + j: 8 + c * 3 + j + 1]
        cbv = lambda c: pvs[:, 8 + NFF * 3 + c: 8 + NFF * 3 + c + 1]

        def A_vec(t):
            for s in range(NS):
                front_vec(C, bufs, x_in, t * TT + s * 128, s, t % 2)

        def A_pe(t):
            for s in range(NS):
                front_pe(C, bufs, s, t % 2, TT)

        cnt = [0]
        NB = 5

        def B(t, hook=None):
            par = t % 2
            hT = bufs["hT"][par]
            hk = [("hT", par, s) for s in range(NS)]
            slot = {}

            def st0(c):
                bi = cnt[0] % NB
                cnt[0] += 1
                slot[c] = bi
                pg = psG[bi]
                kg = ("psG", bi)
                for half, col0 in ((0, c * 128), (1, DFF + c * 128)):
                    for k in range(8):
                        S.op("pe", lambda e, k=k, half=half, col0=col0, pg=pg: e.matmul(
                            pg[:, half, :], lhsT=W1[:, k, col0:col0 + 128], rhs=hT[:, k, :], start=(k == 0), stop=(k == 7)),
                            reads=hk + [("W1", half, c // 2)], writes=[kg])
                g = G[bi]
                kG = ("G", bi)
                S.op("act", lambda e, g=g, pg=pg: e.copy(out=g[:, 2:TT + 2], in_=pg[:, 0, :]), reads=[kg], writes=[kG])
                S.op("dve", lambda e, g=g, c=c: e.tensor_copy(out=g[:, 0:2], in_=halo[:, c, :]), reads=[("halo", c)], writes=[kG])
                S.op("dve", lambda e, g=g, c=c: e.tensor_copy(out=halo[:, c, :], in_=g[:, TT:TT + 2]), reads=[kG], writes=[("halo", c)])

            def st1(c):
                bi = slot[c]
                g, a = G[bi], acc[bi]
                kG, ka = ("G", bi), ("acc", bi)
                S.op("act", lambda e, g=g, a=a, c=c: e.activation(out=a[:], in_=g[:, 2:TT + 2], func=AF.Identity, scale=cwv(c, 2), bias=cbv(c)),
                     reads=[kG, "gk"], writes=[ka])
                S.op("dve", lambda e, g=g, a=a, c=c: e.scalar_tensor_tensor(out=a[:], in0=g[:, 1:TT + 1], scalar=cwv(c, 1), in1=a[:], op0=ALU.mult, op1=ALU.add),
                     reads=[kG, "gk", ka], writes=[ka])
                S.op("dve", lambda e, g=g, a=a, c=c: e.scalar_tensor_tensor(out=a[:], in0=g[:, 0:TT], scalar=cwv(c, 0), in1=a[:], op0=ALU.mult, op1=ALU.add),
                     reads=[kG, "gk", ka], writes=[ka])

            def st2(c):
                bi = slot[c]
                a, gg = acc[bi], ge[bi]
                ka, kge = ("acc", bi), ("ge", bi)
                S.op("act", lambda e, gg=gg, a=a: e.activation(out=gg[:], in_=a[:], func=AF.Gelu_apprx_tanh), reads=[ka], writes=[kge])

            def st3(c):
                bi = slot[c]
                gg, pg = ge[bi], psG[bi]
                kge, kg = ("ge", bi), ("psG", bi)
                S.op("dve", lambda e, gg=gg, pg=pg, c=c: e.tensor_tensor(out=actT[:, c, :], in0=gg[:], in1=pg[:, 1, :], op=ALU.mult),
                     reads=[kge, kg], writes=[("actT", c)])

            stages_ = (st0, st1, st2, st3)
            for i in range(NFF + 3):
                for s_, fn in enumerate(stages_):
                    c = i - s_
                    if 0 <= c < NFF:
                        fn(c)
                if hook is not None and i == 8:
                    hook()

        def Cc(t):
            ak = [("actT", c) for c in range(NFF)]
            for s in range(NS):
                tok0 = t * TT + s * 128
                xr = xres[s % 2]
                kx = ("xres", s % 2)
                S.dma("sp", xr[:], x_in[tok0:tok0 + 128, :], writes=[kx])
                for n in range(2):
                    po = psO[0]
                    kp = ("psO", 0)
                    for c in range(NFF):
                        S.op("pe", lambda e, c=c, n=n, s=s, po=po: e.matmul(
                            po[:], lhsT=actT[:, c, s * 128:(s + 1) * 128], rhs=W2[:, c, n * 512:(n + 1) * 512], start=(c == 0), stop=(c == NFF - 1)),
                            reads=ak + W2k, writes=[kp])
                    S.op("dve", lambda e, n=n, xr=xr, po=po: e.tensor_tensor(out=xr[:, n * 512:(n + 1) * 512], in0=xr[:, n * 512:(n + 1) * 512], in1=po[:], op=ALU.add),
                         reads=[kp, kx], writes=[kx])
                S.dma("pool", x_out[tok0:tok0 + 128, :], xr[:], reads=[kx], writes=[("xout", tok0)])

        if halo_x is not None:
            flag_d, hsrc, hdst, groups = halo_x
            flg = C.sb(st, [128, 2], F32, "flg")
            hs = C.sb(st, [128, NFF, 2], F32, "hs")
            S.dma("pool", flg[:], flag_d[:, :], writes=["flg"])
            hT1 = bufs["hT"][1]
            for c in range(NFF):
                pg = psG[c % 2]
                for k in range(8):
                    S.op("pe", lambda e, k=k, c=c, pg=pg: e.matmul(pg[:, 0, 0:128], lhsT=W1[:, k, c * 128:(c + 1) * 128], rhs=hT1[:, k, 0:128], start=(k == 0), stop=(k == 7)),
                         reads=[("hT", 1, 0), ("W1", 0, c // 2)], writes=[("psG", c % 2)])
                S.op("act", lambda e, c=c, pg=pg: e.copy(out=hs[:, c, :], in_=pg[:, 0, 126:128]), reads=[("psG", c % 2)], writes=["hs"])
            S.dma("pool", hsrc[:, :], hs[:].rearrange("p c j -> p (c j)"), reads=["hs"], writes=["hsrc"])
            S.cc("AllGather", ALU.bypass, groups, hsrc, hdst, reads=["hsrc"], writes=["hdst"])
            S.dma("pool", halo[:].rearrange("p c j -> p (c j)"), hdst[0:128, :], reads=["hdst"], writes=[("halo", c) for c in range(NFF)])
            S.op("dve", lambda e: e.tensor_scalar(out=halo[:], in0=halo[:], scalar1=flg[:, 0:1], scalar2=None, op0=ALU.mult),
                 reads=["flg"] + [("halo", c) for c in range(NFF)], writes=[("halo", c) for c in range(NFF)])
        A_vec(0)
        A_pe(0)
        for t in range(NT):
            nxt = (lambda t=t: A_vec(t + 1)) if t + 1 < NT else None
            B(t, hook=nxt)
            if t + 1 < NT:
                A_pe(t + 1)
            Cc(t)
        S.barrier()


def xattn_phase(C, x_in, x_out, mem_d, wq_d, wkv_d, wo_d, pv, ident_d, T, TT=512):
    S = C.S
    NS = TT // 128
    NT = T // TT
    with ExitStack() as st:
        Wq = C.sb(st, [128, 8, D], BF16, "Wq")
        Wo = C.sb(st, [128, 8, D], BF16, "Wo")
        Wkv = C.sb(st, [128, 8, 2 * D], BF16, "Wkv")
        stages = [C.sb(st, [128, 2048], F32, "stg") for _ in range(2)]
        pvs = C.sb(st, [128, 16 + 512], F32, "pv")
        ident = C.sb(st, [128, 128], BF16, "ident")
        ones = C.sb(st, [128, 128], BF16, "ones")
        gqk = C.sb(st, [128, 256], F32, "gqk")
        bufs = dict(
            xin=[C.sb(st, [128, D], F32, "xin") for _ in range(2)],
            h=[C.sb(st, [128, D], BF16, "h") for _ in range(NS)],
            ss=[C.sb(st, [128, 4], F32, "ss") for _ in range(2)],
            junk=C.sb(st, [128, D], F32, "junk"),
            hT=[C.sb(st, [128, 8, TT], BF16, "hT") for _ in range(2)],
            psT=[C.ps(st, [128, D], BF16, "psT") for _ in range(2)],
            ident=ident,
        )
        mkT = C.sb(st, [128, 8, 256], BF16, "mkT")
        mv = C.sb(st, [128, 2, D], BF16, "mv")
        qn = [C.sb(st, [128, D], BF16, "qn") for _ in range(2)]
        qss = [C.sb(st, [128, 12], F32, "qss") for _ in range(2)]
        qT = C.sb(st, [128, 8, TT], BF16, "qT")
        pT = [C.sb(st, [128, 2, TT], BF16, "pT") for _ in range(2)]
        oT = C.sb(st, [128, 8, TT], BF16, "oT")
        rec = [C.sb(st, [128, TT], F32, "rec") for _ in range(2)]
        xres = [C.sb(st, [128, D], F32, "xres") for _ in range(2)]
        psA = [C.ps(st, [128, 512], F32, "psA") for _ in range(2)]
        psL = [C.ps(st, [128, 512], F32, "psL") for _ in range(2)]
        psO = [C.ps(st, [128, 512], F32, "psO") for _ in range(2)]
        psT = bufs["psT"]
        junk = bufs["junk"]

        S.dma("sp", pvs[:], pv[:, :], writes=["gk"])
        S.dma("sp", ident[:], ident_d[:, :], writes=["ident"])
        S.op("pool", lambda e: e.memset(ones[:], 1.0), writes=["ones"])
        S.op("dve", lambda e: e.scalar_tensor_tensor(out=gqk[:], in0=pvs[:, 16:272], scalar=1.0 / 16.0, in1=pvs[:, 272:528], op0=ALU.mult, op1=ALU.mult),
             reads=["gk"], writes=["gqk"])
        for mt in range(2):
            front_vec(C, bufs, mem_d, mt * 128, mt, 0)
            front_pe(C, bufs, mt, 0, TT)
        load_weight_bf16(C, wkv_d, Wkv, stages, 8, 2 * D, gk=pvs[:, 8:16], tag="Wkv")
        load_weight_bf16(C, wq_d, Wq, stages, 8, D, gk=pvs[:, 0:8], tag="Wq")
        load_weight_bf16(C, wo_d, Wo, stages, 8, D, gk=None, tag="Wo")
        Wkvk, Wqk, Wok = wkeys("Wkv", 8, 2 * D), wkeys("Wq", 8, D), wkeys("Wo", 8, D)

        def headnorm_stats(ps_banks, ss, kss, bank_keys):
            for n in range(2):
                S.op("act", lambda e, n=n: e.activation(out=junk[:, 0:512], in_=ps_banks[n][:], func=AF.Square), reads=[bank_keys[n]], writes=["junk"])
                S.op("dve", lambda e, n=n: e.tensor_reduce(out=ss[:, 2 * n:2 * n + 2], in_=junk[:, 0:512].rearrange("p (a b) -> p a b", a=2), axis=AX.X, op=ALU.add),
                     reads=["junk"], writes=[kss])
            S.op("act", lambda e: e.activation(out=ss[:, 4:8], in_=ss[:, 0:4], func=AF.Ln, scale=1.0 / 256.0, bias=EPS), reads=[kss], writes=[kss])
            S.op("act", lambda e: e.activation(out=ss[:, 8:12], in_=ss[:, 4:8], func=AF.Exp, scale=-0.5), reads=[kss], writes=[kss])

        memT = bufs["hT"][0]
        banks = [psA[0], psA[1], psL[0], psL[1]]
        bkeys = [("psA", 0), ("psA", 1), ("psL", 0), ("psL", 1)]
        for mt in range(2):
            for n in range(4):
                for k in range(8):
                    S.op("pe", lambda e, k=k, n=n, mt=mt: e.matmul(banks[n][:], lhsT=memT[:, k, mt * 128:(mt + 1) * 128], rhs=Wkv[:, k, n * 512:(n + 1) * 512], start=(k == 0), stop=(k == 7)),
                         reads=[("hT", 0, mt)] + Wkvk, writes=[bkeys[n]])
            ss = qss[mt]
            kss = ("qss", mt)
            headnorm_stats(banks[0:2], ss, kss, bkeys[0:2])
            mkn = qn[mt]
            for hd in range(4):
                S.op("dve", lambda e, hd=hd, ss=ss, mkn=mkn: e.scalar_tensor_tensor(
                    out=mkn[:, hd * 256:(hd + 1) * 256], in0=banks[hd // 2][:, (hd % 2) * 256:(hd % 2 + 1) * 256],
                    scalar=ss[:, 8 + hd:9 + hd], in1=gqk[:], op0=ALU.mult, op1=ALU.mult),
                    reads=[bkeys[hd // 2], kss, "gqk"], writes=[("qn", mt)])
            for n in (2, 3):
                S.op("act", lambda e, n=n, mt=mt: e.copy(out=mv[:, mt, (n - 2) * 512:(n - 1) * 512], in_=banks[n][:]), reads=[bkeys[n]], writes=[("mv", mt)])
            pt = psT[mt]
            for c in range(8):
                S.op("pe", lambda e, c=c, pt=pt, mkn=mkn: e.transpose(out=pt[:, c * 128:(c + 1) * 128], in_=mkn[:, c * 128:(c + 1) * 128], identity=ident[:]),
                     reads=[("qn", mt), "ident"], writes=[("psT", mt)])
            S.op("dve", lambda e, pt=pt, mt=mt: e.tensor_copy(out=mkT[:, :, mt * 128:(mt + 1) * 128], in_=pt[:].rearrange("p (k t) -> p k t", k=8)),
                 reads=[("psT", mt)], writes=[("mkT", mt)])
        mkTk = [("mkT", 0), ("mkT", 1)]
        mvk = [("mv", 0), ("mv", 1)]

        def A_vec(t):
            for s in range(NS):
                front_vec(C, bufs, x_in, t * TT + s * 128, s, t % 2)

        def A_pe(t):
            for s in range(NS):
                front_pe(C, bufs, s, t % 2, TT)

        def Q(t):
            par = t % 2
            hT = bufs["hT"][par]
            pairs = ((psA, [("psA", 0), ("psA", 1)]), (psL, [("psL", 0), ("psL", 1)]))

            def proj(s):
                bk, bkk = pairs[s % 2]
                for n in range(2):
                    for k in range(8):
                        S.op("pe", lambda e, k=k, n=n, s=s, bk=bk: e.matmul(bk[n][:], lhsT=hT[:, k, s * 128:(s + 1) * 128], rhs=Wq[:, k, n * 512:(n + 1) * 512], start=(k == 0), stop=(k == 7)),
                             reads=[("hT", par, s)] + Wqk, writes=[bkk[n]])

            def norm(s):
                bk, bkk = pairs[s % 2]
                ss = qss[s % 2]
                kss = ("qss", s % 2)
                headnorm_stats(bk, ss, kss, bkk)
                q = qn[s % 2]
                for hd in range(4):
                    src = bk[hd // 2][:, (hd % 2) * 256:(hd % 2 + 1) * 256]
                    dst = q[:, hd * 256:(hd + 1) * 256]
                    if hd % 2 == 0:
                        S.op("act", lambda e, src=src, dst=dst, ss=ss, hd=hd: e.activation(out=dst, in_=src, func=AF.Copy, scale=ss[:, 8 + hd:9 + hd]),
                             reads=[bkk[hd // 2], kss], writes=[("qn", s % 2)])
                    else:
                        S.op("dve", lambda e, src=src, dst=dst, ss=ss, hd=hd: e.tensor_scalar(out=dst, in0=src, scalar1=ss[:, 8 + hd:9 + hd], scalar2=None, op0=ALU.mult),
                             reads=[bkk[hd // 2], kss], writes=[("qn", s % 2)])

            def trans(s):
                q = qn[s % 2]
                pt = psT[s % 2]
                for c in range(8):
                    S.op("pe", lambda e, c=c, pt=pt, q=q: e.transpose(out=pt[:, c * 128:(c + 1) * 128], in_=q[:, c * 128:(c + 1) * 128], identity=ident[:]),
                         reads=[("qn", s % 2), "ident"], writes=[("psT", s % 2)])
                S.op("act" if s % 2 else "dve",
                     (lambda e, pt=pt, s=s: e.copy(out=qT[:, :, s * 128:(s + 1) * 128], in_=pt[:].rearrange("p (k t) -> p k t", k=8))) if s % 2 else
                     (lambda e, pt=pt, s=s: e.tensor_copy(out=qT[:, :, s * 128:(s + 1) * 128], in_=pt[:].rearrange("p (k t) -> p k t", k=8))),
                     reads=[("psT", s % 2)], writes=[("qT", s)])

            proj(0)
            for s in range(NS):
                norm(s)
                if s + 1 < NS:
                    proj(s + 1)
                trans(s)

        def ATT(t):
            qTk = [("qT", s) for s in range(NS)]

            def logits(hd):
                p = pT[hd % 2]
                kp = ("pT", hd % 2)
                for mt in range(2):
                    for dc in range(2):
                        S.op("pe", lambda e, mt=mt, dc=dc, hd=hd: e.matmul(psL[mt][:, 0:TT], lhsT=mkT[:, hd * 2 + dc, mt * 128:(mt + 1) * 128], rhs=qT[:, hd * 2 + dc, :], start=(dc == 0), stop=(dc == 1)),
                             reads=qTk + mkTk, writes=[("psL", mt)])
                    S.op("act", lambda e, mt=mt, p=p: e.activation(out=p[:, mt, :], in_=psL[mt][:, 0:TT], func=AF.Exp), reads=[("psL", mt)], writes=[kp])

            def pv(hd):
                p = pT[hd % 2]
                kp = ("pT", hd % 2)
                pa = psA[hd % 2]
                for mt in range(2):
                    S.op("pe", lambda e, mt=mt, p=p, pa=pa: e.matmul(pa[:, 0:TT], lhsT=ones[:], rhs=p[:, mt, :], start=(mt == 0), stop=(mt == 1)),
                         reads=[kp, "ones"], writes=[("psA", hd % 2)])
                r = rec[hd % 2]
                S.op("act", lambda e, r=r, pa=pa: e.activation(out=r[:], in_=pa[:, 0:TT], func=AF.Ln), reads=[("psA", hd % 2)], writes=[("rec", hd % 2)])
                S.op("act", lambda e, r=r: e.activation(out=r[:], in_=r[:], func=AF.Exp, scale=-1.0), reads=[("rec", hd % 2)], writes=[("rec", hd % 2)])
                for dc in range(2):
                    for mt in range(2):
                        S.op("pe", lambda e, mt=mt, dc=dc, hd=hd, p=p: e.matmul(psO[dc][:, 0:TT], lhsT=mv[:, mt, hd * 256 + dc * 128: hd * 256 + (dc + 1) * 128], rhs=p[:, mt, :], start=(mt == 0), stop=(mt == 1)),
                             reads=[kp] + mvk, writes=[("psO", dc)])
                    S.op("dve", lambda e, dc=dc, hd=hd, r=r: e.tensor_tensor(out=oT[:, hd * 2 + dc, :], in0=psO[dc][:, 0:TT], in1=r[:], op=ALU.mult),
                         reads=[("psO", dc), ("rec", hd % 2)], writes=[("oT", hd * 2 + dc)])

            logits(0)
            for hd in range(4):
                if hd + 1 < 4:
                    logits(hd + 1)
                pv(hd)

        def OUT(t):
            ok = [("oT", c) for c in range(8)]
            pairs = ((psA, [("psA", 0), ("psA", 1)]), (psL, [("psL", 0), ("psL", 1)]))
            for s in range(NS):
                tok0 = t * TT + s * 128
                bk, bkk = pairs[s % 2]
                xr = xres[s % 2]
                kx = ("xres", s % 2)
                S.dma("sp", xr[:], x_in[tok0:tok0 + 128, :], writes=[kx])
                for n in range(2):
                    for c in range(8):
                        S.op("pe", lambda e, c=c, n=n, s=s, bk=bk: e.matmul(bk[n][:], lhsT=oT[:, c, s * 128:(s + 1) * 128], rhs=Wo[:, c, n * 512:(n + 1) * 512], start=(c == 0), stop=(c == 7)),
                             reads=ok + Wok, writes=[bkk[n]])
                    S.op("dve", lambda e, n=n, xr=xr, bk=bk: e.tensor_tensor(out=xr[:, n * 512:(n + 1) * 512], in0=xr[:, n * 512:(n + 1) * 512], in1=bk[n][:], op=ALU.add),
                         reads=[bkk[n], kx], writes=[kx])
                S.dma("pool", x_out[tok0:tok0 + 128, :], xr[:], reads=[kx], writes=[("xout", tok0)])

        C.dbg_src = dict(qT=qT, mkT=mkT, oT=oT, mv=mv, pT0=pT[0], pT1=pT[1], rec0=rec[0], rec1=rec[1])
        A_vec(0)
        A_pe(0)
        for t in range(NT):
            Q(t)
            if t + 1 < NT:
                A_vec(t + 1)
            ATT(t)
            if t + 1 < NT:
                A_pe(t + 1)
            OUT(t)
        S.barrier()
        for name, ap in getattr(C, "dbg", {}).items():
            S.dma("sp", ap, C.dbg_src[name][:], writes=[("dbg", name)])
        S.barrier()


def even_consts(T):
    half = 64
    inv = (1.0 / (10000.0 ** (np.arange(half, dtype=np.float32) / half))).astype(np.float32)
    pos = np.arange(T, dtype=np.float32)
    ang = (pos[:, None] * inv[None, :]).astype(np.float32)
    cos, sin = np.cos(ang.astype(np.float64)), np.sin(ang.astype(np.float64))
    sc = 128.0 ** -0.5
    rot = np.stack([np.tile(cos, (1, 4)), np.tile(sin, (1, 4)), np.tile(cos * sc, (1, 4)), np.tile(sin * sc, (1, 4))], axis=1)
    rot = rot.astype(np.float32)
    lg = np.log(1.0 - np.exp2(-5.0 - np.arange(4, dtype=np.float64)))
    idx = np.arange(128, dtype=np.float64)
    diff = idx[None, :] - idx[:, None]
    dmask = np.where(diff[None] >= 0, np.exp(lg[:, None, None] * np.maximum(diff[None], 0)), 0.0)
    dmask = dmask.transpose(1, 0, 2).reshape(128, 512)
    xi = np.exp(lg[:, None] * (idx + 1.0))
    zeta = np.exp(lg[:, None] * (127.0 - idx))
    XI = np.repeat(xi.T[:, :, None], 128, axis=2).reshape(128, 512)
    ZE = np.repeat(zeta.T[:, :, None], 128, axis=2).reshape(128, 512)
    ch0 = (idx >= 64)
    cm = ((idx[:, None] <= idx[None, :]) & (ch0[:, None] == ch0[None, :])).astype(np.float64)
    cmask = np.tile(cm[:, None, :], (1, 4, 1)).reshape(128, 512)
    c32 = np.concatenate([dmask, XI, ZE, cmask], axis=1).astype(np.float32)
    MT = (idx[:, None] <= idx[None, :]).astype(np.float64)
    ch = (idx >= 64).astype(np.int64)
    same = (ch[:, None] == ch[None, :]).astype(np.float64)
    midt = np.where(ch == 0, 31, 95)
    MA = same * MT - same * (idx[:, None] <= midt[None, :]).astype(np.float64)
    MD = (idx[:, None] > idx[None, :]).astype(np.float64)
    ones = np.ones((128, 128))
    MB = MT - (idx[:, None] <= 63).astype(np.float64)
    bq = np.where(idx < 64, -300.0, 0.0)[:, None]
    bk = np.where(idx < 64, 0.0, -300.0)[:, None]
    mats = np.concatenate([MA, MT, MD, ones, MB, bq, bk], axis=1).astype(np.float32)
    g128 = [float(np.exp(l * 128.0)) for l in lg]
    return rot, c32, mats, g128


def even_phase(C, x_in, x_out, w_in_d, w_out_d, pv, rot_d, c32_d, mats_d, g128, ident_d, T, TT=256, prev=None):
    S = C.S
    NS = TT // 128
    NT = T // TT
    with ExitStack() as st:
        Win = C.sb(st, [128, 8, 4096], BF16, "Win")
        Wout = C.sb(st, [128, 8, D], BF16, "Wout")
        pvs = C.sb(st, [128, 16 + 1536], F32, "pv")
        ident = C.sb(st, [128, 128], BF16, "ident")
        c32 = C.sb(st, [128, 2048], F32, "c32")
        mats = C.sb(st, [128, 642], F32, "mats")
        lbt = C.sb(st, [128, 1024], F32, "lbt")
        bufs = dict(
            xin=[C.sb(st, [128, D], F32, "xin") for _ in range(2)],
            h=[C.sb(st, [128, D], BF16, "h") for _ in range(NS)],
            ss=[C.sb(st, [128, 4], F32, "ss") for _ in range(2)],
            junk=C.sb(st, [128, D], F32, "junk"),
            hT=[C.sb(st, [128, 8, TT], BF16, "hT") for _ in range(2)],
            psT=[C.ps(st, [128, D], BF16, "psT") for _ in range(2)],
            ident=ident,
        )
        psT = bufs["psT"]
        junk = bufs["junk"]
        P = [C.ps(st, [128, 512], F32, "P") for _ in range(4)]
        X = [C.ps(st, [128, 512], F32, "X") for _ in range(2)]
        Pk = [("P", i) for i in range(4)]
        Xk = [("X", i) for i in range(2)]
        rot = [C.sb(st, [128, 4, 256], F32, "rot") for _ in range(2)]
        tq = [C.sb(st, [128, 256], F32, "tq") for _ in range(4)]
        WF = {"r": [C.sb(st, [128, 512], F32, "wr") for _ in range(4)], "g": [C.sb(st, [128, 512], F32, "wg") for _ in range(8)]}
        WFK = {"r": [("wr", i) for i in range(4)], "g": [("wg", i) for i in range(8)]}
        BB = {"r": {n: C.sb(st, [128, 512], BF16, "r" + n) for n in ("qa", "kb", "qc", "kd", "v", "PT", "qcT")},
              "g": {n: C.sb(st, [128, 512], BF16, "g" + n) for n in ("qa", "kb", "qc", "kd", "v", "PT", "qcT", "qa2", "kb2")}}
        QKT = {m: C.sb(st, [128, 1024], BF16, "qkT" + m) for m in ("r", "g")}
        qkT2 = C.sb(st, [128, 1024], BF16, "qkT2")
        GATE = {m: C.sb(st, [128, 512], F32, "gate" + m) for m in ("r", "g")}
        YT = {m: C.sb(st, [128, 512], F32, "ytmp" + m) for m in ("r", "g")}
        y = C.sb(st, [128, D], BF16, "y")
        yT = C.sb(st, [128, 8, 128], BF16, "yT")
        Sst = {m: C.sb(st, [128, 512], F32, "S" + m) for m in ("r", "g")}
        Sbf = {m: C.sb(st, [128, 512], BF16, "Sb" + m) for m in ("r", "g")}
        stat = C.sb(st, [128, 64], F32, "stat")
        Ecol = C.sb(st, [128, 4], F32, "Ecol")
        xres = [C.sb(st, [128, D], F32, "xres") for _ in range(1)]
        Wg = WF["g"]
        Wgk = WFK["g"]

        def bk(m, n):
            return (m, n)

        S.dma("sp", pvs[:], pv[:, :], writes=["gk"])
        S.dma("sp", ident[:], ident_d[:, :], writes=["ident"])
        S.dma("sp", c32[:], c32_d[:, :], writes=["c32"])
        S.dma("sp", mats[:], mats_d[:, :], writes=["mats"])
        for m in ("r", "g"):
            S.op("pool", lambda e, m=m: e.memset(Sst[m][:], 0.0), writes=[("S", m)])
            S.op("pool", lambda e, m=m: e.memset(Sbf[m][:], 0.0), writes=[("Sb", m)])
        lg3 = pvs[:, 16:16 + 1536]
        S.op("act", lambda e: e.activation(out=junk[:, :], in_=lg3[:, 0:1024], func=AF.Exp), reads=["gk"], writes=["junk"])
        S.op("act", lambda e: e.activation(out=Wg[0][:], in_=lg3[:, 1024:1536], func=AF.Exp), reads=["gk"], writes=[Wgk[0]])
        S.op("dve", lambda e: e.tensor_tensor(out=Wg[0][:], in0=Wg[0][:], in1=junk[:, 512:1024], op=ALU.add), reads=["junk", Wgk[0]], writes=[Wgk[0]])
        S.op("dve", lambda e: e.tensor_tensor(out=Wg[0][:], in0=Wg[0][:], in1=junk[:, 0:512], op=ALU.add), reads=["junk", Wgk[0]], writes=[Wgk[0]])
        S.op("dve", lambda e: e.reciprocal(out=Wg[0][:], in_=Wg[0][:]), reads=[Wgk[0]], writes=[Wgk[0]])
        S.op("dve", lambda e: e.tensor_tensor(out=Wg[1][:], in0=Wg[0][:], in1=junk[:, 0:512], op=ALU.mult), reads=["junk", Wgk[0]], writes=[Wgk[1]])
        S.op("dve", lambda e: e.tensor_scalar(out=lbt[:, 512:1024], in0=Wg[1][:], scalar1=-0.5, scalar2=0.5, op0=ALU.mult, op1=ALU.add), reads=[Wgk[1]], writes=["lbt"])
        S.op("dve", lambda e: e.tensor_tensor(out=lbt[:, 0:512], in0=Wg[1][:], in1=lbt[:, 512:1024], op=ALU.add), reads=[Wgk[1], "lbt"], writes=["lbt"])
        S.op("dve", lambda e: e.tensor_scalar(out=pvs[:, 8:16], in0=pvs[:, 8:16], scalar1=0.5, scalar2=None, op0=ALU.mult), reads=["gk"], writes=["gk"])
        stages = [Wg[4], Wg[5], Wg[6], Wg[7]]
        C.stg_i = -1
        load_weight_bf16(C, w_in_d, Win, stages, 8, 4096, gk=pvs[:, 0:8], colblk=512, tag="Win")
        load_weight_bf16(C, w_out_d, Wout, stages, 8, D, gk=pvs[:, 8:16], colblk=512, tag="Wout")
        S.barrier()
        Wink, Woutk = [], []
        dmask, XI, ZE, cmask = c32[:, 0:512], c32[:, 512:1024], c32[:, 1024:1536], c32[:, 1536:2048]
        MA, MT, MD, ones32, MB = mats[:, 0:128], mats[:, 128:256], mats[:, 256:384], mats[:, 384:512], mats[:, 512:640]
        bq, bkk_ = mats[:, 640:641], mats[:, 641:642]
        lbh, omlh = lbt[:, 0:512], lbt[:, 512:1024]

        def A_vec(t):
            for s in range(NS):
                front_vec(C, bufs, x_in, t * TT + s * 128, s, t % 2)

        def A_pe(t):
            for s in range(NS):
                front_pe(C, bufs, s, t % 2, TT)

        def proj(par, s, groups):
            hT = bufs["hT"][par]
            for gi_, g in enumerate(groups):
                for k in range(8):
                    S.op("pe", lambda e, gi_=gi_, g=g, k=k: e.matmul(P[gi_][:], lhsT=hT[:, k, s * 128:(s + 1) * 128], rhs=Win[:, k, g * 512:(g + 1) * 512], start=(k == 0), stop=(k == 7)),
                         reads=[("hT", par, s)], writes=[Pk[gi_]])

        def rotary_g(rt, kr, eng, src, dst, ci, si, kin, kout, tmps, tk):
            x1 = src[:].rearrange("p (h two d) -> p h two d", h=4, two=2)[:, :, 0, :]
            x2 = src[:].rearrange("p (h two d) -> p h two d", h=4, two=2)[:, :, 1, :]
            o1 = dst[:].rearrange("p (h two d) -> p h two d", h=4, two=2)[:, :, 0, :]
            o2 = dst[:].rearrange("p (h two d) -> p h two d", h=4, two=2)[:, :, 1, :]
            cs = rt[:, ci, :].rearrange("p (h d) -> p h d", h=4)
            sn = rt[:, si, :].rearrange("p (h d) -> p h d", h=4)
            t1 = tmps[0][:].rearrange("p (h d) -> p h d", h=4)
            t2 = tmps[1][:].rearrange("p (h d) -> p h d", h=4)
            S.op(eng, lambda e: e.tensor_tensor(out=t1, in0=x1, in1=cs, op=ALU.mult), reads=[kin, kr], writes=[tk[0]])
            S.op(eng, lambda e: e.tensor_tensor(out=t2, in0=x2, in1=sn, op=ALU.mult), reads=[kin, kr], writes=[tk[1]])
            S.op(eng, lambda e: e.tensor_tensor(out=o1, in0=t1, in1=t2, op=ALU.subtract), reads=[tk[0], tk[1]], writes=[kout])
            S.op(eng, lambda e: e.tensor_tensor(out=t1, in0=x1, in1=sn, op=ALU.mult), reads=[kin, kr, kout], writes=[tk[0]])
            S.op(eng, lambda e: e.tensor_tensor(out=t2, in0=x2, in1=cs, op=ALU.mult), reads=[kin, kr, kout], writes=[tk[1]])
            S.op(eng, lambda e: e.tensor_tensor(out=o2, in0=t1, in1=t2, op=ALU.add), reads=[tk[0], tk[1]], writes=[kout])

        def linattn(mask, Eh, m, second=False):
            Bb = BB[m]
            qa, kb, qc, kd, v, PT, qcT = (Bb[n] for n in ("qa", "kb", "qc", "kd", "v", "PT", "qcT"))
            qkT = QKT[m]
            kqkT = ("qkT", m)
            ytmp = YT[m]
            for hh in range(4):
                blk = slice(hh * 128, (hh + 1) * 128)
                S.op("pe", lambda e, blk=blk: e.transpose(out=psT[0][:, blk], in_=qa[:, blk], identity=ident[:]), reads=[bk(m, "qa"), "ident"], writes=[("psT", 0)])
            for hh in range(4):
                blk = slice(hh * 128, (hh + 1) * 128)
                blk2 = slice(512 + hh * 128, 512 + (hh + 1) * 128)
                S.op("pe", lambda e, blk=blk, blk2=blk2: e.transpose(out=psT[0][:, blk2], in_=kb[:, blk], identity=ident[:]), reads=[bk(m, "kb"), "ident"], writes=[("psT", 0)])
            for hh in range(4):
                blk = slice(hh * 128, (hh + 1) * 128)
                S.op("pe", lambda e, blk=blk: e.transpose(out=psT[1][:, blk], in_=qc[:, blk], identity=ident[:]), reads=[bk(m, "qc"), "ident"], writes=[("psT", 1)])
            S.op("act", lambda e: e.copy(out=qkT[:], in_=psT[0][:]), reads=[("psT", 0)], writes=[kqkT])
            S.op("dve", lambda e: e.tensor_copy(out=qcT[:], in_=psT[1][:, 0:512]), reads=[("psT", 1)], writes=[bk(m, "qcT")])
            for hh in range(4):
                blk = slice(hh * 128, (hh + 1) * 128)
                blk2 = slice(512 + hh * 128, 512 + (hh + 1) * 128)
                S.op("pe", lambda e, blk=blk, blk2=blk2: e.matmul(X[0][:, blk], lhsT=qkT[:, blk2], rhs=qkT[:, blk], start=True, stop=True), reads=[kqkT], writes=[Xk[0]])
            if second:
                qa2, kb2 = Bb["qa2"], Bb["kb2"]
                for hh in range(4):
                    blk = slice(hh * 128, (hh + 1) * 128)
                    S.op("pe", lambda e, blk=blk: e.transpose(out=psT[0][:, blk], in_=qa2[:, blk], identity=ident[:]), reads=[bk(m, "qa2"), "ident"], writes=[("psT", 0)])
                for hh in range(4):
                    blk = slice(hh * 128, (hh + 1) * 128)
                    blk2 = slice(512 + hh * 128, 512 + (hh + 1) * 128)
                    S.op("pe", lambda e, blk=blk, blk2=blk2: e.transpose(out=psT[0][:, blk2], in_=kb2[:, blk], identity=ident[:]), reads=[bk(m, "kb2"), "ident"], writes=[("psT", 0)])
                S.op("act", lambda e: e.copy(out=qkT2[:], in_=psT[0][:]), reads=[("psT", 0)], writes=["qkT2"])
                for hh in range(4):
                    blk = slice(hh * 128, (hh + 1) * 128)
                    blk2 = slice(512 + hh * 128, 512 + (hh + 1) * 128)
                    S.op("pe", lambda e, blk=blk, blk2=blk2: e.matmul(P[0][:, blk], lhsT=qkT2[:, blk2], rhs=qkT2[:, blk], start=True, stop=True), reads=["qkT2"], writes=[Pk[0]])
                S.op("dve", lambda e: e.tensor_tensor(out=ytmp[:], in0=X[0][:], in1=mask, op=ALU.mult), reads=[Xk[0], "c32"], writes=[("ytmp", m)])
                S.op("dve", lambda e: e.tensor_tensor(out=PT[:], in0=ytmp[:], in1=P[0][:], op=ALU.add), reads=[("ytmp", m), Pk[0]], writes=[bk(m, "PT")])
            else:
                S.op("dve", lambda e: e.tensor_tensor(out=PT[:], in0=X[0][:], in1=mask, op=ALU.mult), reads=[Xk[0], "c32"], writes=[bk(m, "PT")])
            for hh in range(4):
                blk = slice(hh * 128, (hh + 1) * 128)
                S.op("pe", lambda e, blk=blk: e.matmul(X[1][:, blk], lhsT=PT[:, blk], rhs=v[:, blk], start=True, stop=False), reads=[bk(m, "PT"), bk(m, "v")], writes=[Xk[1]])
                S.op("pe", lambda e, blk=blk: e.matmul(X[1][:, blk], lhsT=qcT[:, blk], rhs=Sbf[m][:, blk], start=False, stop=True), reads=[bk(m, "qcT"), ("Sb", m)], writes=[Xk[1]])
            for hh in range(4):
                blk = slice(hh * 128, (hh + 1) * 128)
                S.op("pe", lambda e, blk=blk: e.matmul(X[0][:, blk], lhsT=kd[:, blk], rhs=v[:, blk], start=True, stop=True), reads=[bk(m, "kd"), bk(m, "v")], writes=[Xk[0]])
            for hh in range(4):
                blk = slice(hh * 128, (hh + 1) * 128)
                S.op("dve", lambda e, blk=blk, hh=hh: e.scalar_tensor_tensor(out=Sst[m][:, blk], in0=Sst[m][:, blk], scalar=Eh(hh), in1=X[0][:, blk], op0=ALU.mult, op1=ALU.add),
                     reads=[Xk[0], ("S", m), "Ecol"], writes=[("S", m)])
            S.op("act", lambda e: e.copy(out=Sbf[m][:], in_=Sst[m][:]), reads=[("S", m)], writes=[("Sb", m)])

        def gate_evac(m, bank, kbank):
            g = GATE[m]
            S.op("act", lambda e: e.activation(out=g[:], in_=bank[:], func=AF.Tanh, scale=0.5), reads=[kbank], writes=[("gate", m)])
            S.op("dve", lambda e: e.scalar_tensor_tensor(out=g[:], in0=g[:], scalar=1.0, in1=bank[:], op0=ALU.add, op1=ALU.mult), reads=[kbank, ("gate", m)], writes=[("gate", m)])

        def hg_gates(f, kf, key, kkey, logf, klogf):
            S.op("dve", lambda e: e.tensor_tensor(out=f[:], in0=f[:], in1=omlh, op=ALU.mult), reads=[kf, "lbt"], writes=[kf])
            S.op("dve", lambda e: e.tensor_tensor(out=f[:], in0=f[:], in1=lbh, op=ALU.add), reads=[kf, "lbt"], writes=[kf])
            S.op("pool", lambda e: e.tensor_scalar(out=key[:], in0=f[:], scalar1=-1.0, scalar2=1.0, op0=ALU.mult, op1=ALU.add), reads=[kf], writes=[kkey])
            S.op("dve", lambda e: e.tensor_scalar_max(out=logf[:], in0=f[:], scalar1=1e-6), reads=[kf], writes=[klogf])
            S.op("act", lambda e: e.activation(out=logf[:], in_=logf[:], func=AF.Ln), reads=[klogf], writes=[klogf])

        def state_only(par, s, tok0, rotp_d):
            proj(par, s, (1, 2, 5, 6))
            rt = rot[(tok0 // 128) % 2]
            kr = ("rot", (tok0 // 128) % 2)
            S.dma("sp", rt[:], rotp_d[tok0:tok0 + 128, :, :], writes=[kr])
            Wr, Wrk = WF["r"], WFK["r"]
            kf, kr_ = Wr[1], Wr[3]
            f, key, logf, Dk = Wg[1], Wg[2], Wg[3], Wg[7]
            v1, v2, kd1, kd2 = BB["r"]["v"], BB["g"]["v"], BB["r"]["kd"], BB["g"]["kd"]
            S.op("act", lambda e: e.copy(out=kf[:], in_=P[0][:]), reads=[Pk[0]], writes=[Wrk[1]])
            S.op("act", lambda e: e.copy(out=v1[:], in_=P[1][:]), reads=[Pk[1]], writes=[bk("r", "v")])
            S.op("act", lambda e: e.activation(out=f[:], in_=P[2][:], func=AF.Tanh, scale=0.5), reads=[Pk[2]], writes=[Wgk[1]])
            S.op("act", lambda e: e.copy(out=v2[:], in_=P[3][:]), reads=[Pk[3]], writes=[bk("g", "v")])
            rotary_g(rt, kr, "dve", kf, kr_, 2, 3, Wrk[1], Wrk[3], tq[2:4], [("tq", 2), ("tq", 3)])
            S.op("pool", lambda e: e.tensor_tensor(out=kd1[:], in0=kr_[:], in1=ZE, op=ALU.mult), reads=[Wrk[3], "c32"], writes=[bk("r", "kd")])
            hg_gates(f, Wgk[1], key, Wgk[2], logf, Wgk[3])
            S.op("pe", lambda e: e.matmul(P[2][:], lhsT=MD, rhs=logf[:], start=True, stop=True), reads=[Wgk[3], "mats"], writes=[Pk[2]])
            for hh in range(4):
                S.op("pe", lambda e, hh=hh: e.matmul(P[3][:, hh:hh + 1], lhsT=logf[:, hh * 128:(hh + 1) * 128], rhs=ones32[:, 0:1], start=True, stop=True),
                     reads=[Wgk[3], "mats"], writes=[Pk[3]])
            S.op("act", lambda e: e.activation(out=Dk[:], in_=P[2][:], func=AF.Exp), reads=[Pk[2]], writes=[Wgk[7]])
            S.op("act", lambda e: e.activation(out=Ecol[:], in_=P[3][:, 0:4], func=AF.Exp), reads=[Pk[3]], writes=["Ecol"])
            S.op("dve", lambda e: e.tensor_tensor(out=kd2[:], in0=key[:], in1=Dk[:], op=ALU.mult), reads=[Wgk[2], Wgk[7]], writes=[bk("g", "kd")])
            for m, kd_, v_, xi in (("r", kd1, v1, 0), ("g", kd2, v2, 1)):
                for hh in range(4):
                    blk = slice(hh * 128, (hh + 1) * 128)
                    S.op("pe", lambda e, blk=blk, kd_=kd_, v_=v_, xi=xi: e.matmul(X[xi][:, blk], lhsT=kd_[:, blk], rhs=v_[:, blk], start=True, stop=True), reads=[bk(m, "kd"), bk(m, "v")], writes=[Xk[xi]])
                for hh in range(4):
                    blk = slice(hh * 128, (hh + 1) * 128)
                    sc = g128[hh] if m == "r" else Ecol[:, hh:hh + 1]
                    S.op("dve", lambda e, blk=blk, sc=sc, m=m, xi=xi: e.scalar_tensor_tensor(out=Sst[m][:, blk], in0=Sst[m][:, blk], scalar=sc, in1=X[xi][:, blk], op0=ALU.mult, op1=ALU.add),
                         reads=[Xk[xi], ("S", m), "Ecol"], writes=[("S", m)])

        def ret_a(par, s, tok0):
            proj(par, s, (0, 1, 2, 3))
            Wr, Wrk = WF["r"], WFK["r"]
            S.op("act", lambda e: e.copy(out=Wr[0][:], in_=P[0][:]), reads=[Pk[0]], writes=[Wrk[0]])
            S.op("act", lambda e: e.copy(out=Wr[1][:], in_=P[1][:]), reads=[Pk[1]], writes=[Wrk[1]])
            S.op("act", lambda e: e.copy(out=BB["r"]["v"][:], in_=P[2][:]), reads=[Pk[2]], writes=[bk("r", "v")])
            gate_evac("r", P[3], Pk[3])

        def hg_a(par, s, tok0):
            proj(par, s, (4, 5, 6, 7))
            S.op("act", lambda e: e.copy(out=Wg[0][:], in_=P[0][:]), reads=[Pk[0]], writes=[Wgk[0]])
            S.op("act", lambda e: e.activation(out=Wg[1][:], in_=P[1][:], func=AF.Tanh, scale=0.5), reads=[Pk[1]], writes=[Wgk[1]])
            S.op("act", lambda e: e.copy(out=BB["g"]["v"][:], in_=P[2][:]), reads=[Pk[2]], writes=[bk("g", "v")])
            gate_evac("g", P[3], Pk[3])

        def ret_b1(par, s, tok0):
            rt = rot[(tok0 // 128) % 2]
            kr = ("rot", (tok0 // 128) % 2)
            S.dma("sp", rt[:], rot_d[tok0:tok0 + 128, :, :], writes=[kr])
            Wr, Wrk = WF["r"], WFK["r"]
            Bb = BB["r"]
            qf, kf, qr, kr_ = Wr
            rotary_g(rt, kr, "dve", qf, qr, 0, 1, Wrk[0], Wrk[2], tq[0:2], [("tq", 0), ("tq", 1)])
            rotary_g(rt, kr, "pool", kf, kr_, 2, 3, Wrk[1], Wrk[3], tq[2:4], [("tq", 2), ("tq", 3)])
            S.op("act", lambda e: e.copy(out=Bb["qa"][:], in_=qr[:]), reads=[Wrk[2]], writes=[bk("r", "qa")])
            S.op("dve", lambda e: e.tensor_tensor(out=Bb["qc"][:], in0=qr[:], in1=XI, op=ALU.mult), reads=[Wrk[2], "c32"], writes=[bk("r", "qc")])
            S.op("act", lambda e: e.copy(out=Bb["kb"][:], in_=kr_[:]), reads=[Wrk[3]], writes=[bk("r", "kb")])
            S.op("pool", lambda e: e.tensor_tensor(out=Bb["kd"][:], in0=kr_[:], in1=ZE, op=ALU.mult), reads=[Wrk[3], "c32"], writes=[bk("r", "kd")])

        def hg_b1(par, s, tok0):
            Bb = BB["g"]
            qf, f, key, logf, A, Bm, Cq, Dk = Wg
            hg_gates(f, Wgk[1], key, Wgk[2], logf, Wgk[3])
            S.op("pe", lambda e: e.matmul(P[0][:], lhsT=MA, rhs=logf[:], start=True, stop=True), reads=[Wgk[3], "mats"], writes=[Pk[0]])
            S.op("pe", lambda e: e.matmul(P[1][:], lhsT=MT, rhs=logf[:], start=True, stop=True), reads=[Wgk[3], "mats"], writes=[Pk[1]])
            S.op("pe", lambda e: e.matmul(P[2][:], lhsT=MD, rhs=logf[:], start=True, stop=True), reads=[Wgk[3], "mats"], writes=[Pk[2]])
            for hh in range(4):
                S.op("pe", lambda e, hh=hh: e.matmul(P[3][:, hh:hh + 1], lhsT=logf[:, hh * 128:(hh + 1) * 128], rhs=ones32[:, 0:1], start=True, stop=True),
                     reads=[Wgk[3], "mats"], writes=[Pk[3]])
            S.op("act", lambda e: e.activation(out=A[:], in_=P[0][:], func=AF.Exp), reads=[Pk[0]], writes=[Wgk[4]])
            S.op("act", lambda e: e.activation(out=Bm[:], in_=P[0][:], func=AF.Exp, scale=-1.0), reads=[Pk[0]], writes=[Wgk[5]])
            S.op("act", lambda e: e.activation(out=Cq[:], in_=P[1][:], func=AF.Exp), reads=[Pk[1]], writes=[Wgk[6]])
            S.op("act", lambda e: e.activation(out=Dk[:], in_=P[2][:], func=AF.Exp), reads=[Pk[2]], writes=[Wgk[7]])
            S.op("act", lambda e: e.activation(out=Ecol[:], in_=P[3][:, 0:4], func=AF.Exp), reads=[Pk[3]], writes=["Ecol"])
            S.op("dve", lambda e: e.tensor_tensor(out=Bb["qa"][:], in0=qf[:], in1=A[:], op=ALU.mult), reads=[Wgk[0], Wgk[4]], writes=[bk("g", "qa")])
            S.op("pool", lambda e: e.tensor_tensor(out=Bb["kb"][:], in0=key[:], in1=Bm[:], op=ALU.mult), reads=[Wgk[2], Wgk[5]], writes=[bk("g", "kb")])
            S.op("dve", lambda e: e.tensor_tensor(out=Bb["qc"][:], in0=qf[:], in1=Cq[:], op=ALU.mult), reads=[Wgk[0], Wgk[6]], writes=[bk("g", "qc")])
            S.op("pool", lambda e: e.tensor_tensor(out=Bb["kd"][:], in0=key[:], in1=Dk[:], op=ALU.mult), reads=[Wgk[2], Wgk[7]], writes=[bk("g", "kd")])
            S.op("pe", lambda e: e.matmul(P[0][:], lhsT=MB, rhs=logf[:], start=True, stop=True), reads=[Wgk[3], "mats"], writes=[Pk[0]])
            A2, B2 = f, junk[:, 512:1024]
            S.op("act", lambda e: e.activation(out=A2[:], in_=P[0][:], func=AF.Exp, bias=bq), reads=[Pk[0], "mats", Wgk[2], Wgk[3]], writes=[Wgk[1]])
            S.op("act", lambda e: e.activation(out=B2, in_=P[0][:], func=AF.Exp, scale=-1.0, bias=bkk_), reads=[Pk[0], "mats"], writes=["junk"])
            S.op("dve", lambda e: e.tensor_tensor(out=Bb["qa2"][:], in0=qf[:], in1=A2[:], op=ALU.mult), reads=[Wgk[0], Wgk[1]], writes=[bk("g", "qa2")])
            S.op("pool", lambda e: e.tensor_tensor(out=Bb["kb2"][:], in0=key[:], in1=B2, op=ALU.mult), reads=[Wgk[2], "junk"], writes=[bk("g", "kb2")])

        def ret_b2(par, s, tok0):
            linattn(dmask, lambda hh: g128[hh], "r")
            o4 = X[1]
            ytmp, gate = YT["r"], GATE["r"]
            for hh in range(4):
                S.op("dve", lambda e, hh=hh: e.bn_stats(out=stat[:, hh * 6:(hh + 1) * 6], in_=o4[:, hh * 128:(hh + 1) * 128]), reads=[Xk[1]], writes=["stat"])
            for hh in range(4):
                S.op("dve", lambda e, hh=hh: e.bn_aggr(out=stat[:, 24 + hh * 2:26 + hh * 2], in_=stat[:, hh * 6:(hh + 1) * 6]), reads=["stat"], writes=["stat"])
            var4 = stat[:, 24:32].rearrange("p (h two) -> p h two", two=2)[:, :, 1]
            S.op("act", lambda e: e.activation(out=stat[:, 32:36], in_=var4, func=AF.Ln, bias=EPS), reads=["stat"], writes=["stat"])
            S.op("act", lambda e: e.activation(out=stat[:, 36:40], in_=stat[:, 32:36], func=AF.Exp, scale=-0.5), reads=["stat"], writes=["stat"])
            for hh in range(4):
                S.op("dve", lambda e, hh=hh: e.tensor_scalar(out=ytmp[:, hh * 128:(hh + 1) * 128], in0=o4[:, hh * 128:(hh + 1) * 128],
                                                              scalar1=stat[:, 24 + 2 * hh:25 + 2 * hh], scalar2=stat[:, 36 + hh:37 + hh], op0=ALU.subtract, op1=ALU.mult),
                     reads=[Xk[1], "stat"], writes=[("ytmp", "r")])
            S.op("pool", lambda e: e.tensor_tensor(out=y[:, 0:512], in0=ytmp[:], in1=gate[:], op=ALU.mult), reads=[("ytmp", "r"), ("gate", "r")], writes=[("y", 0)])

        def hg_b2(par, s, tok0):
            linattn(cmask, lambda hh: Ecol[:, hh:hh + 1], "g", second=True)
            o4 = X[1]
            ytmp, gate = YT["g"], GATE["g"]
            S.op("act", lambda e: e.activation(out=junk[:, 0:512], in_=o4[:], func=AF.Square), reads=[Xk[1]], writes=["junk"])
            S.op("dve", lambda e: e.tensor_reduce(out=stat[:, 40:44], in_=junk[:, 0:512].rearrange("p (h d) -> p h d", h=4), axis=AX.X, op=ALU.add), reads=["junk"], writes=["stat2"])
            S.op("act", lambda e: e.activation(out=stat[:, 44:48], in_=stat[:, 40:44], func=AF.Ln, scale=1.0 / 128.0, bias=EPS), reads=["stat2"], writes=["stat2"])
            S.op("act", lambda e: e.activation(out=stat[:, 48:52], in_=stat[:, 44:48], func=AF.Exp, scale=-0.5), reads=["stat2"], writes=["stat2"])
            for hh in range(4):
                S.op("dve", lambda e, hh=hh: e.tensor_scalar(out=ytmp[:, hh * 128:(hh + 1) * 128], in0=o4[:, hh * 128:(hh + 1) * 128],
                                                              scalar1=stat[:, 48 + hh:49 + hh], scalar2=None, op0=ALU.mult),
                     reads=[Xk[1], "stat2"], writes=[("ytmp", "g")])
            S.op("pool", lambda e: e.tensor_tensor(out=y[:, 512:1024], in0=ytmp[:], in1=gate[:], op=ALU.mult), reads=[("ytmp", "g"), ("gate", "g")], writes=[("y", 1)])

        def outproj(tok0, s):
            for c in range(8):
                S.op("pe", lambda e, c=c: e.transpose(out=psT[1][:, c * 128:(c + 1) * 128], in_=y[:, c * 128:(c + 1) * 128], identity=ident[:]),
                     reads=[("y", 0), ("y", 1), "ident"], writes=[("psT", 1)])
            S.op("act", lambda e: e.copy(out=yT[:], in_=psT[1][:].rearrange("p (k t) -> p k t", k=8)), reads=[("psT", 1)], writes=["yT"])
            xr = xres[0]
            kx = ("xres", 0)
            S.dma("sp", xr[:], x_in[tok0:tok0 + 128, :], writes=[kx])
            for n in range(2):
                for c in range(8):
                    S.op("pe", lambda e, c=c, n=n: e.matmul(P[2 + n][:], lhsT=yT[:, c, :], rhs=Wout[:, c, n * 512:(n + 1) * 512], start=(c == 0), stop=(c == 7)),
                         reads=["yT"], writes=[Pk[2 + n]])
                S.op("dve", lambda e, n=n, xr=xr: e.tensor_tensor(out=xr[:, n * 512:(n + 1) * 512], in0=xr[:, n * 512:(n + 1) * 512], in1=P[2 + n][:], op=ALU.add),
                     reads=[Pk[2 + n], kx], writes=[kx])
            S.dma("pool", x_out[tok0:tok0 + 128, :], xr[:], reads=[kx], writes=[("xout", tok0)])

        if prev is not None:
            xp_d, rotp_d, flag_d, Tp = prev
            flg = C.sb(st, [128, 2], F32, "flg")
            S.dma("sp", flg[:], flag_d[:, :], writes=["flg"])
            x_main = x_in
            x_in = xp_d
            A_vec(0)
            A_pe(0)
            for t in range(Tp // TT):
                for s in range(NS):
                    state_only(t % 2, s, t * TT + s * 128, rotp_d)
                    if s == 0 and t + 1 < Tp // TT:
                        A_vec(t + 1)
                if t + 1 < Tp // TT:
                    A_pe(t + 1)
            x_in = x_main
            for m_ in ("r", "g"):
                S.op("dve", lambda e, m_=m_: e.tensor_scalar(out=Sst[m_][:], in0=Sst[m_][:], scalar1=flg[:, 0:1], scalar2=None, op0=ALU.mult), reads=[("S", m_), "flg"], writes=[("S", m_)])
                S.op("act", lambda e, m_=m_: e.copy(out=Sbf[m_][:], in_=Sst[m_][:]), reads=[("S", m_)], writes=[("Sb", m_)])
        A_vec(0)
        A_pe(0)
        for t in range(NT):
            for s in range(NS):
                tok0 = t * TT + s * 128
                par = t % 2
                ret_a(par, s, tok0)
                hg_a(par, s, tok0)
                ret_b1(par, s, tok0)
                hg_b1(par, s, tok0)
                if s == 0 and t + 1 < NT:
                    A_vec(t + 1)
                ret_b2(par, s, tok0)
                hg_b2(par, s, tok0)
                outproj(tok0, s)
            if t + 1 < NT:
                A_pe(t + 1)
        S.barrier()


BRANCHES = ((128, 1), (512, 4), (2048, 16))
NEG = -30000.0


def odd_consts():
    import math

    def bucket(d):
        d = np.maximum(d, 0)
        lr = np.log(np.maximum(d, 1).astype(np.float32) / np.float32(16)) / np.float32(math.log(2048 / 16))
        large = np.minimum(16 + (lr * np.float32(16)).astype(np.int32), 31)
        return np.where(d < 16, d, large)

    OH = np.zeros((32, 1536), np.float32)
    NEGc = np.zeros((8, 1536), np.float32)
    for b, (window, dil) in enumerate(BRANCHES):
        for var in (0, 1):
            for w in range(255):
                u = w + 1
                if var == 0:
                    delta, valid = u - 128, (u - 128) >= 0
                else:
                    delta, valid = u, u <= 128
                col = (b * 2 + var) * 255 + w
                if valid:
                    OH[int(bucket(np.array(delta * dil))), col] = 1.0
                else:
                    NEGc[:, col] = NEG
    J = np.zeros((128, 128), np.float32)
    J[np.arange(128), 127 - np.arange(128)] = 1.0
    return OH, NEGc, J


def odd_phase(C, x_in, x_out, w_in_d, w_out_d, pv, relb_d, OH_d, NEGc_d, J_d, ident_d, scr, T, TT=256, split=None):
    S = C.S
    NS = TT // 128
    NT = T // TT
    SCALE = 128.0 ** -0.5
    with ExitStack() as st:
        ident = C.sb(st, [128, 128], BF16, "ident")
        ones = C.sb(st, [128, 128], BF16, "ones")
        BMh = C.sb(st, [128, 48, 128], BF16, "BMh")
        BMl = C.sb(st, [128, 48, 128], BF16, "BMl")
        pvs = C.sb(st, [128, 8 + 256], F32, "pv")
        gqk = C.sb(st, [128, 128], F32, "gqk")
        stages = [C.sb(st, [128, 1024], F32, "stg") for _ in range(2)]
        Wout = C.sb(st, [128, 8, D], BF16, "Wout")
        xres = [C.sb(st, [128, D], F32, "xres") for _ in range(2)]
        psT = [C.ps(st, [128, D], BF16, "psT") for _ in range(2)]
        P = [C.ps(st, [128, 512], F32, "P") for _ in range(6)]
        Pk = [("P", i) for i in range(6)]

        S.dma("sp", pvs[:], pv[:, :], writes=["gk"])
        S.dma("sp", ident[:], ident_d[:, :], writes=["ident"])
        S.op("pool", lambda e: e.memset(ones[:], 1.0), writes=["ones"])
        S.op("dve", lambda e: e.tensor_tensor(out=gqk[:], in0=pvs[:, 8:136], in1=pvs[:, 136:264], op=ALU.mult), reads=["gk"], writes=["gqk"])

        with ExitStack() as st0:
            relb = C.sb(st0, [32, 8], F32, "relb")
            OH = C.sb(st0, [32, 1536], F32, "OH")
            NEGc = C.sb(st0, [8, 1536], F32, "NEGc")
            Fsb = C.sb(st0, [8, 1536], F32, "Fsb")
            Jm = C.sb(st0, [128, 128], F32, "J")
            Hsb = C.sb(st0, [128, 48, 128], F32, "Hsb")
            BM = C.sb(st0, [128, 48, 128], F32, "BM")
            S.dma("sp", relb[:], relb_d[:, :], writes=["relb"])
            S.dma("sp", OH[:], OH_d[:, :], writes=["OH"])
            S.dma("sp", NEGc[:], NEGc_d[:, :], writes=["NEGc"])
            S.dma("sp", Jm[:], J_d[:, :], writes=["J"])
            for c in range(3):
                S.op("pe", lambda e, c=c: e.matmul(P[c][0:8, :], lhsT=relb[:, :], rhs=OH[:, c * 512:(c + 1) * 512], start=True, stop=True),
                     reads=["relb", "OH"], writes=[Pk[c]])
                S.op("dve", lambda e, c=c: e.tensor_tensor(out=Fsb[:, c * 512:(c + 1) * 512], in0=P[c][0:8, :], in1=NEGc[:, c * 512:(c + 1) * 512], op=ALU.add),
                     reads=[Pk[c], "NEGc"], writes=["Fsb"])
            S.dma("sp", scr["F"][:, :], Fsb[:], reads=["Fsb"], writes=["Fd"])
            Ft = scr["F"].tensor
            for hh in range(8):
                for b in range(3):
                    for var in range(2):
                        idx = (hh * 3 + b) * 2 + var
                        src = bass.AP(Ft, hh * 1536 + (b * 2 + var) * 255, [[1, 128], [1, 128]])
                        S.dma("sp" if idx % 2 else "pool", Hsb[:, idx, :], src, reads=["Fd"], writes=[("Hsb", idx)])
            for g in range(12):
                pb = P[3 + g % 3]
                S.op("pe", lambda e, g=g, pb=pb: e.matmul(pb[:], lhsT=Jm[:], rhs=Hsb[:, 4 * g:4 * g + 4, :].rearrange("p a b -> p (a b)"), start=True, stop=True),
                     reads=[("Hsb", 4 * g + i) for i in range(4)] + ["J"], writes=[Pk[3 + g % 3]])
                S.op("act", lambda e, g=g, pb=pb: e.copy(out=BM[:, 4 * g:4 * g + 4, :].rearrange("p a b -> p (a b)"), in_=pb[:]), reads=[Pk[3 + g % 3]], writes=[("BM", g)])
            for g in range(12):
                sl_ = slice(4 * g, 4 * g + 4)
                S.op("dve", lambda e, sl_=sl_: e.tensor_scalar(out=Hsb[:, sl_, :], in0=BM[:, sl_, :], scalar1=1.0 / SCALE, scalar2=None, op0=ALU.mult),
                     reads=[("BM", g)] + [("Hsb", 4 * g + i) for i in range(4)], writes=[("Hs", g)])
                S.op("act", lambda e, sl_=sl_: e.copy(out=BMh[:, sl_, :], in_=Hsb[:, sl_, :]), reads=[("Hs", g)], writes=[("BMh", g)])
                S.op("dve", lambda e, sl_=sl_: e.tensor_tensor(out=BMl[:, sl_, :], in0=Hsb[:, sl_, :], in1=BMh[:, sl_, :], op=ALU.subtract),
                     reads=[("Hs", g), ("BMh", g)], writes=[("BMl", g)])
            S.barrier()
        BMk = []

        with ExitStack() as st1:
            Win = C.sb(st1, [128, 8, 3072], BF16, "Win")
            bufs = dict(
                xin=[C.sb(st1, [128, D], F32, "xin") for _ in range(2)],
                h=[C.sb(st1, [128, D], BF16, "h") for _ in range(NS)],
                ss=[C.sb(st1, [128, 4], F32, "ss") for _ in range(2)],
                junk=C.sb(st1, [128, D], F32, "junk"),
                hT=[C.sb(st1, [128, 8, TT], BF16, "hT") for _ in range(2)],
                psT=psT,
                ident=ident,
            )
            junk = bufs["junk"]
            junk2 = C.sb(st1, [128, D], F32, "junk2")
            nst = [C.sb(st1, [128, 48], F32, "nst") for _ in range(2)]
            qn = [C.sb(st1, [128, D], BF16, "qn") for _ in range(2)]
            kn = [C.sb(st1, [128, D], BF16, "kn") for _ in range(2)]
            vb = [C.sb(st1, [128, D], BF16, "vb") for _ in range(2)]
            qTt = [C.sb(st1, [128, 8, 128], BF16, "qTt") for _ in range(2)]
            kTt = [C.sb(st1, [128, 8, 128], BF16, "kTt") for _ in range(2)]
            load_weight_bf16(C, w_in_d, Win, stages, 8, 3072, gk=pvs[:, 0:8], colblk=1024, tag="Win")
            Wink = wkeys("Win", 8, 3072, 1024)
            qTd = scr["qT"].rearrange("h d t -> d h t")
            kTd = scr["kT"].rearrange("h d t -> d h t")

            def A_vec(t):
                for s in range(NS):
                    front_vec(C, bufs, x_in, t * TT + s * 128, s, t % 2)

            def A_pe(t):
                for s in range(NS):
                    front_pe(C, bufs, s, t % 2, TT)

            def O1(t, s):
                par = t % 2
                tok0 = t * TT + s * 128
                i2 = (tok0 // 128) % 2
                hT = bufs["hT"][par]
                for g in range(6):
                    for k in range(8):
                        S.op("pe", lambda e, g=g, k=k: e.matmul(P[g][:], lhsT=hT[:, k, s * 128:(s + 1) * 128], rhs=Win[:, k, g * 512:(g + 1) * 512], start=(k == 0), stop=(k == 7)),
                             reads=[("hT", par, s)] + Wink, writes=[Pk[g]])
                ns = nst[i2]
                kns = ("nst", i2)
                for g in range(2):
                    S.op("act", lambda e, g=g: e.activation(out=junk[:, g * 512:(g + 1) * 512], in_=P[g][:], func=AF.Square), reads=[Pk[g]], writes=["junk"])
                S.op("dve", lambda e: e.tensor_reduce(out=ns[:, 0:8], in_=junk[:].rearrange("p (h d) -> p h d", h=8), axis=AX.X, op=ALU.add), reads=["junk"], writes=[kns])
                for g in range(2):
                    S.op("act", lambda e, g=g: e.activation(out=junk2[:, g * 512:(g + 1) * 512], in_=P[2 + g][:], func=AF.Square), reads=[Pk[2 + g]], writes=["junk2"])
                S.op("dve", lambda e: e.tensor_reduce(out=ns[:, 8:16], in_=junk2[:].rearrange("p (h d) -> p h d", h=8), axis=AX.X, op=ALU.add), reads=["junk2"], writes=[kns])
                S.op("act", lambda e: e.activation(out=ns[:, 16:32], in_=ns[:, 0:16], func=AF.Ln, scale=1.0 / 128.0, bias=EPS), reads=[kns], writes=[kns])
                S.op("act", lambda e: e.activation(out=ns[:, 32:48], in_=ns[:, 16:32], func=AF.Exp, scale=-0.5), reads=[kns], writes=[kns])
                q, k_, v = qn[i2], kn[i2], vb[i2]
                for hh in range(8):
                    src = P[hh // 4][:, (hh % 4) * 128:(hh % 4 + 1) * 128]
                    dst = q[:, hh * 128:(hh + 1) * 128]
                    if hh % 2 == 0:
                        S.op("act", lambda e, src=src, dst=dst, hh=hh: e.activation(out=dst, in_=src, func=AF.Copy, scale=ns[:, 32 + hh:33 + hh]), reads=[Pk[hh // 4], kns], writes=[("qn", i2)])
                    else:
                        S.op("dve", lambda e, src=src, dst=dst, hh=hh: e.tensor_scalar(out=dst, in0=src, scalar1=ns[:, 32 + hh:33 + hh], scalar2=None, op0=ALU.mult), reads=[Pk[hh // 4], kns], writes=[("qn", i2)])
                for hh in range(8):
                    src = P[2 + hh // 4][:, (hh % 4) * 128:(hh % 4 + 1) * 128]
                    dst = k_[:, hh * 128:(hh + 1) * 128]
                    S.op("dve", lambda e, src=src, dst=dst, hh=hh: e.scalar_tensor_tensor(out=dst, in0=src, scalar=ns[:, 40 + hh:41 + hh], in1=gqk[:], op0=ALU.mult, op1=ALU.mult),
                         reads=[Pk[2 + hh // 4], kns, "gqk"], writes=[("kn", i2)])
                for g in range(2):
                    S.op("act", lambda e, g=g: e.copy(out=v[:, g * 512:(g + 1) * 512], in_=P[4 + g][:]), reads=[Pk[4 + g]], writes=[("vb", i2)])
                S.dma("pool", scr["v"][tok0:tok0 + 128, :], v[:], reads=[("vb", i2)], writes=[("vd", tok0)])
                for c in range(8):
                    S.op("pe", lambda e, c=c: e.transpose(out=psT[0][:, c * 128:(c + 1) * 128], in_=q[:, c * 128:(c + 1) * 128], identity=ident[:]), reads=[("qn", i2), "ident"], writes=[("psT", 0)])
                S.op("act", lambda e: e.copy(out=qTt[i2][:], in_=psT[0][:].rearrange("p (k t) -> p k t", k=8)), reads=[("psT", 0)], writes=[("qTt", i2)])
                S.dma("sp", qTd[:, :, tok0:tok0 + 128], qTt[i2][:], reads=[("qTt", i2)], writes=[("qTd", tok0)])
                for c in range(8):
                    S.op("pe", lambda e, c=c: e.transpose(out=psT[1][:, c * 128:(c + 1) * 128], in_=k_[:, c * 128:(c + 1) * 128], identity=ident[:]), reads=[("kn", i2), "ident"], writes=[("psT", 1)])
                S.op("dve", lambda e: e.tensor_copy(out=kTt[i2][:], in_=psT[1][:].rearrange("p (k t) -> p k t", k=8)), reads=[("psT", 1)], writes=[("kTt", i2)])
                S.dma("sp", kTd[:, :, tok0:tok0 + 128], kTt[i2][:], reads=[("kTt", i2)], writes=[("kTd", tok0)])

            A_vec(0)
            A_pe(0)
            for t in range(NT):
                for s in range(NS):
                    O1(t, s)
                    if s == 0 and t + 1 < NT:
                        A_vec(t + 1)
                if t + 1 < NT:
                    A_pe(t + 1)
            S.barrier()

        koff = T if split is not None else 0
        Tc = koff + T
        if split is not None:
            flag_d, groups = split
            flg = C.sb(st, [128, 2], F32, "flg")
            S.dma("sp", flg[:], flag_d[:, :], writes=["flg"])
            assert T == 2048
            kT2 = scr["kT"].rearrange("h d t -> (h d) t")
            for p_ in range(2):
                S.cc("AllGather", ALU.bypass, groups, kT2[p_ * 512:(p_ + 1) * 512, :], scr["kTg"][p_], writes=[("kTg", p_)])
                S.cc("AllGather", ALU.bypass, groups, scr["v"][p_ * 1024:(p_ + 1) * 1024, :], scr["vg"][p_], writes=[("vg", p_)])
        with ExitStack() as st2:
            qTh = [C.sb(st2, [128, T], BF16, "qTh") for _ in range(2)]
            kTh = [C.sb(st2, [128, Tc], BF16, "kTh") for _ in range(2)]
            accs = [C.sb(st2, [128, 2, T], F32, "acc") for _ in range(2)]
            oT = C.sb(st2, [128, 8, T], BF16, "oT")
            NV = 8
            SKEW = 3
            Vb = [C.sb(st2, [128, 128], BF16, "Vb") for _ in range(NV)]
            Pb = [C.sb(st2, [128, 256], BF16, "Pb") for _ in range(NV)]
            tmp = [C.sb(st2, [128, 256], F32, "tmp") for _ in range(3)]
            SC = [P[0], P[1], P[2]]
            OB = [P[3], P[4], P[5]]
            load_weight_bf16(C, w_out_d, Wout, stages, 8, D, gk=None, colblk=1024, tag="Wout")
            Woutk = wkeys("Wout", 8, D, 1024)
            its = []
            for hh in range(8):
                for b, (window, dil) in enumerate(BRANCHES):
                    nb = Tc // dil // 128
                    n_own = koff // (dil * 128)
                    for r in range(dil):
                        prev = None
                        for n in range(n_own - (1 if split is not None else 0), nb):
                            rec = dict(hh=hh, b=b, dil=dil, base=r + dil * 128 * n, past=(n < n_own), prev=prev, idx=len(its),
                                       nq=(256 if n + 1 < nb else 128), last_of_head=False)
                            prev = rec["idx"]
                            its.append(rec)
                its[-1]["last_of_head"] = True

            def sl(ap, start, count, step):
                return ap[:, start:start + step * (count - 1) + 1:step] if step > 1 else ap[:, start:start + count]

            def load_head(hh):
                qh, kh = qTh[hh % 2], kTh[hh % 2]
                kq, kk = ("qTh", hh % 2), ("kTh", hh % 2)
                S.dma("sp", qh[:], scr["qT"][hh, :, :], writes=[kq])
                S.dma("sp", kh[:, koff:Tc], scr["kT"][hh, :, :], writes=[kk])
                if split is not None:
                    S.dma("sp", kh[:, 0:koff], scr["kTg"][hh // 4][(hh % 4) * 128:(hh % 4 + 1) * 128, :], reads=[("kTg", hh // 4)], writes=[kk])

            def s0(rec):
                i, hh, dil, base = rec["idx"], rec["hh"], rec["dil"], rec["base"]
                qh, kh = qTh[hh % 2], kTh[hh % 2]
                kq, kk = ("qTh", hh % 2), ("kTh", hh % 2)
                bmh = BMh[:, (hh * 3 + rec["b"]) * 2:(hh * 3 + rec["b"]) * 2 + 2, :].rearrange("p a b -> p (a b)")
                bml = BMl[:, (hh * 3 + rec["b"]) * 2:(hh * 3 + rec["b"]) * 2 + 2, :].rearrange("p a b -> p (a b)")
                vi = i % NV
                Vt, Pt = Vb[vi], Pb[vi]
                kV, kP = ("Vb", vi), ("Pb", vi)
                sc, ksc = SC[i % 3], Pk[i % 3]
                tm, ktm = tmp[i % 3], ("tmp", i % 3)
                q_eng = "pool" if i % 2 else "sp"
                if rec["past"]:
                    r0 = 0
                    while r0 < 128:
                        tr = base + dil * r0
                        pc = tr // 1024
                        cnt = min(128 - r0, (1024 * (pc + 1) - tr + dil - 1) // dil)
                        vsrc = bass.AP(scr["vg"][pc].tensor, (tr - 1024 * pc) * D + hh * 128, [[dil * D, cnt], [1, 128]])
                        S.dma(q_eng, Vt[r0:r0 + cnt, :], vsrc, reads=[("vg", pc)], writes=[kV])
                        r0 += cnt
                    qb = base + dil * 128 - koff
                    lo, hi = 128, 256
                    qsl = sl(qh, qb, 128, dil)
                else:
                    vsrc = bass.AP(scr["v"].tensor, (base - koff) * D + hh * 128, [[dil * D, 128], [1, 128]])
                    S.dma(q_eng, Vt[:], vsrc, writes=[kV])
                    qb = base - koff
                    lo, hi = 0, rec["nq"]
                    qsl = sl(qh, qb, rec["nq"], dil)
                ksl = sl(kh, base, 128, dil)
                S.op("pe", lambda e: e.matmul(sc[:, lo:hi], lhsT=ksl, rhs=qsl, start=True, stop=False), reads=[kq, kk], writes=[ksc])
                S.op("pe", lambda e: e.matmul(sc[:, lo:hi], lhsT=ident[:], rhs=bmh[:, lo:hi], start=False, stop=False), reads=["ident"], writes=[ksc])
                S.op("pe", lambda e: e.matmul(sc[:, lo:hi], lhsT=ident[:], rhs=bml[:, lo:hi], start=False, stop=True), reads=["ident"], writes=[ksc])
                if rec["past"]:
                    S.op("act", lambda e: e.activation(out=Pt[:, lo:hi], in_=sc[:, lo:hi], func=AF.Exp, scale=SCALE, bias=flg[:, 1:2]), reads=[ksc, "flg"], writes=[kP])
                else:
                    S.op("act", lambda e: e.activation(out=Pt[:, lo:hi], in_=sc[:, lo:hi], func=AF.Exp, scale=SCALE), reads=[ksc], writes=[kP])

            def s1(rec):
                if rec["past"]:
                    return
                i, hh, dil, base, b = rec["idx"], rec["hh"], rec["dil"], rec["base"], rec["b"]
                acc = accs[hh % 2]
                kacc = ("acc", hh % 2)
                vi = i % NV
                Vt, Pt = Vb[vi], Pb[vi]
                kV, kP = ("Vb", vi), ("Pb", vi)
                ob, kob = OB[i % 3], Pk[3 + i % 3]
                pr = rec["prev"]
                for j, lhs in enumerate((Vt, ones)):
                    rk_ = [kV] if j == 0 else ["ones"]
                    S.op("pe", lambda e, j=j, lhs=lhs: e.matmul(ob[:, j * 128:(j + 1) * 128], lhsT=lhs[:], rhs=Pt[:, 0:128], start=True, stop=(pr is None)),
                         reads=[kP] + rk_, writes=[kob])
                    if pr is not None:
                        pvi = pr % NV
                        lhs2 = Vb[pvi] if j == 0 else ones
                        rk2 = [("Vb", pvi)] if j == 0 else ["ones"]
                        S.op("pe", lambda e, j=j, lhs2=lhs2, pvi=pvi: e.matmul(ob[:, j * 128:(j + 1) * 128], lhsT=lhs2[:], rhs=Pb[pvi][:, 128:256], start=False, stop=True),
                             reads=[("Pb", pvi)] + rk2, writes=[kob])
                qb = base - koff
                dst = acc[:, :, qb:qb + dil * 127 + 1:dil] if dil > 1 else acc[:, :, qb:qb + 128]
                srcv = ob[:, 0:256].rearrange("p (a b) -> p a b", a=2)
                if b == 0:
                    S.op("act", lambda e: e.copy(out=dst, in_=srcv), reads=[kob], writes=[kacc])
                else:
                    S.op("dve", lambda e: e.tensor_tensor(out=dst, in0=dst, in1=srcv, op=ALU.add), reads=[kob, kacc], writes=[kacc])
                if rec["last_of_head"]:
                    S.op("dve", lambda e: e.reciprocal(out=acc[:, 1, :], in_=acc[:, 1, :]), reads=[kacc], writes=[kacc])
                    S.op("dve", lambda e: e.tensor_tensor(out=oT[:, hh, :], in0=acc[:, 0, :], in1=acc[:, 1, :], op=ALU.mult), reads=[kacc], writes=[("oT", hh)])

            load_head(0)
            for step in range(len(its) + SKEW):
                if step < len(its):
                    rec = its[step]
                    if step + 1 < len(its) and its[step + 1]["hh"] != rec["hh"]:
                        pass
                    if (step == 0 or its[step - 1]["hh"] != rec["hh"]) and rec["hh"] + 1 < 8:
                        load_head(rec["hh"] + 1)
                    s0(rec)
                if step - SKEW >= 0:
                    s1(its[step - SKEW])

            oTk = [("oT", hh) for hh in range(8)]
            for i in range(T // 128):
                tok0 = i * 128
                xr = xres[i % 2]
                kx = ("xres", i % 2)
                S.dma("sp", xr[:], x_in[tok0:tok0 + 128, :], writes=[kx])
                for n in range(2):
                    pb = P[(i % 2) * 2 + n]
                    kpb = Pk[(i % 2) * 2 + n]
                    for hh in range(8):
                        S.op("pe", lambda e, hh=hh, n=n, pb=pb, tok0=tok0: e.matmul(pb[:], lhsT=oT[:, hh, tok0:tok0 + 128], rhs=Wout[:, hh, n * 512:(n + 1) * 512], start=(hh == 0), stop=(hh == 7)),
                             reads=oTk + Woutk, writes=[kpb])
                    S.op("dve", lambda e, n=n, xr=xr, pb=pb: e.tensor_tensor(out=xr[:, n * 512:(n + 1) * 512], in0=xr[:, n * 512:(n + 1) * 512], in1=pb[:], op=ALU.add),
                         reads=[kpb, kx], writes=[kx])
                S.dma("pool", x_out[tok0:tok0 + 128, :], xr[:], reads=[kx], writes=[("xout", tok0)])
            S.barrier()


SEQ = 4096
NMEM = 256
NCORES = 8
TH = SEQ // 2
GROUPS = [[0, 1], [2, 3], [4, 5], [6, 7]]


def _col8(v):
    return np.ascontiguousarray(np.asarray(v, np.float32).reshape(8, 128).T)


def build_program(T=TH):
    nc = bass.Bass("TRN2", target_bir_lowering=False)

    def din(name, shape, dt=F32):
        return nc.dram_tensor(name, list(shape), dt, kind="ExternalInput").ap()

    x = din("x", [T, D])
    xprev = din("xprev", [T, D])
    rotp = din("rotp", [T, 4, 256])
    flag = din("flag", [128, 2])
    mem = din("mem", [NMEM, D])
    ev_w_in = din("ev_w_in", [D, 4096])
    ev_w_out = din("ev_w_out", [D, D])
    od_w_in = din("od_w_in", [D, 3072])
    od_w_out = din("od_w_out", [D, D])
    xa_w_q = [din(f"xa_w_q{l}", [D, D]) for l in range(2)]
    xa_w_kv = [din(f"xa_w_kv{l}", [D, 2 * D]) for l in range(2)]
    xa_w_o = [din(f"xa_w_o{l}", [D, D]) for l in range(2)]
    ffn_w_in = [din(f"ffn_w_in{l}", [D, 2 * DFF]) for l in range(2)]
    ffn_w_out = [din(f"ffn_w_out{l}", [DFF, D]) for l in range(2)]
    pv_even = din("pv_even", [128, 16 + 1536])
    pv_odd = din("pv_odd", [128, 8 + 256])
    pv_xa = [din(f"pv_xa{l}", [128, 16 + 512]) for l in range(2)]
    pv_ffn = [din(f"pv_ffn{l}", [128, 8 + NFF * 4]) for l in range(2)]
    relb = din("rel_bias", [32, 8])
    ident = din("ident", [128, 128], BF16)
    rot = din("rot", [T, 4, 256])
    c32 = din("c32", [128, 2048])
    mats = din("mats", [128, 642])
    OH = din("OH", [32, 1536])
    NEGc = din("NEGc", [8, 1536])
    J = din("J", [128, 128])
    y = nc.dram_tensor("y", [T, D], F32, kind="ExternalOutput").ap()
    xa = nc.dram_tensor("s_xa", [T, D], F32).ap()
    xb = nc.dram_tensor("s_xb", [T, D], F32).ap()
    scr = dict(qT=nc.dram_tensor("s_qT", [8, 128, T], BF16).ap(), kT=nc.dram_tensor("s_kT", [8, 128, T], BF16).ap(),
               v=nc.dram_tensor("s_v", [T, D], BF16).ap(), F=nc.dram_tensor("s_F", [8, 1536], F32).ap(),
               kTg=[nc.dram_tensor(f"s_kTg{p}", [2 * 512, T], BF16).ap() for p in range(2)],
               vg=[nc.dram_tensor(f"s_vg{p}", [2 * 1024, D], BF16).ap() for p in range(2)])
    hsrc = [nc.dram_tensor(f"s_hsrc{l}", [128, 2 * NFF], F32).ap() for l in range(2)]
    hdst = [nc.dram_tensor(f"s_hdst{l}", [256, 2 * NFF], F32).ap() for l in range(2)]
    g128 = even_consts(128)[3]
    with ExitStack() as st:
        S = Sched(nc, st)
        C = Ctx(nc, S)
        even_phase(C, x, xa, ev_w_in, ev_w_out, pv_even, rot, c32, mats, g128, ident, T, prev=(xprev, rotp, flag, T))
        xattn_phase(C, xa, xb, mem, xa_w_q[0], xa_w_kv[0], xa_w_o[0], pv_xa[0], ident, T)
        ffn_phase(C, xb, xa, ffn_w_in[0], ffn_w_out[0], pv_ffn[0], ident, T, halo_x=(flag, hsrc[0], hdst[0], GROUPS))
        odd_phase(C, xa, xb, od_w_in, od_w_out, pv_odd, relb, OH, NEGc, J, ident, scr, T, split=(flag, GROUPS))
        xattn_phase(C, xb, xa, mem, xa_w_q[1], xa_w_kv[1], xa_w_o[1], pv_xa[1], ident, T)
        ffn_phase(C, xa, y, ffn_w_in[1], ffn_w_out[1], pv_ffn[1], ident, T, halo_x=(flag, hsrc[1], hdst[1], GROUPS))
        S.finalize()
    return nc


_CONSTS = None


def _consts():
    global _CONSTS
    if _CONSTS is None:
        import ml_dtypes
        rot, c32, mats, _ = even_consts(SEQ)
        OH, NEGc, J = odd_consts()
        _CONSTS = dict(rot=rot, c32=c32, mats=mats, OH=OH, NEGc=NEGc, J=J,
                       ident=np.eye(128).astype(ml_dtypes.bfloat16))
    return _CONSTS


def kernel(x, mem, mix_norm_g, ev_w_in, ev_ret_norm_g, ev_hg_norm_g, hg_lb_logits, ev_w_out,
           od_w_in, od_q_norm_g, od_k_norm_g, rel_bias, od_w_out,
           xa_norm_g, xa_mem_norm_g, xa_w_q, xa_w_kv, xa_q_norm_g, xa_k_norm_g, xa_w_o,
           ffn_norm_g, ffn_w_in, ffn_conv_w, ffn_conv_b, ffn_w_out):
    f = lambda a: np.ascontiguousarray(np.asarray(a, np.float32))
    x, mem = f(x), f(mem)
    B = x.shape[0]
    shared = dict(_consts())
    shared.update(ev_w_in=f(ev_w_in)[0], ev_w_out=f(ev_w_out)[0], od_w_in=f(od_w_in)[0], od_w_out=f(od_w_out)[0],
                  rel_bias=f(rel_bias))
    pv = np.zeros((128, 16 + 1536), np.float32)
    pv[:, 0:8] = _col8(f(mix_norm_g)[0])
    pv[:, 8:16] = _col8(np.concatenate([f(ev_ret_norm_g)[0].reshape(-1), f(ev_hg_norm_g)[0].reshape(-1)]))
    pv[:, 16:] = f(hg_lb_logits).reshape(1, 1536)
    shared["pv_even"] = pv
    pv = np.zeros((128, 8 + 256), np.float32)
    pv[:, 0:8] = _col8(f(mix_norm_g)[1])
    pv[:, 8:136] = f(od_q_norm_g)[0][None, :]
    pv[:, 136:264] = f(od_k_norm_g)[0][None, :]
    shared["pv_odd"] = pv
    for l in range(2):
        shared[f"xa_w_q{l}"] = f(xa_w_q)[l]
        shared[f"xa_w_kv{l}"] = f(xa_w_kv)[l]
        shared[f"xa_w_o{l}"] = f(xa_w_o)[l]
        shared[f"ffn_w_in{l}"] = f(ffn_w_in)[l]
        shared[f"ffn_w_out{l}"] = f(ffn_w_out)[l]
        pv = np.zeros((128, 16 + 512), np.float32)
        pv[:, 0:8] = _col8(f(xa_norm_g)[l])
        pv[:, 8:16] = _col8(f(xa_mem_norm_g)[l])
        pv[:, 16:272] = f(xa_q_norm_g)[l][None, :]
        pv[:, 272:528] = f(xa_k_norm_g)[l][None, :]
        shared[f"pv_xa{l}"] = pv
        pv = np.zeros((128, 8 + NFF * 4), np.float32)
        pv[:, 0:8] = _col8(f(ffn_norm_g)[l])
        pv[:, 8:8 + NFF * 3] = f(ffn_conv_w)[l].reshape(3, NFF, 128).transpose(2, 1, 0).reshape(128, NFF * 3)
        pv[:, 8 + NFF * 3:] = f(ffn_conv_b)[l].reshape(NFF, 128).T
        shared[f"pv_ffn{l}"] = pv
    nc = build_program(TH)
    rot_full = shared.pop("rot")
    in_maps = []
    for b in range(B):
        for j in range(2):
            m = dict(shared)
            m["x"] = np.ascontiguousarray(x[b, j * TH:(j + 1) * TH])
            m["xprev"] = np.ascontiguousarray(x[b, 0:TH])
            m["mem"] = mem[b]
            m["rot"] = np.ascontiguousarray(rot_full[j * TH:(j + 1) * TH])
            m["rotp"] = np.ascontiguousarray(rot_full[0:TH])
            fl = np.zeros((128, 2), np.float32)
            fl[:, 0] = float(j)
            fl[:, 1] = NEG * (1 - j)
            m["flag"] = fl
            in_maps.append(m)
    res = run_bass_kernel_spmd(nc, in_maps, core_ids=list(range(2 * B)))
    out = np.empty((B, SEQ, D), np.float32)
    for b in range(B):
        for j in range(2):
            out[b, j * TH:(j + 1) * TH] = np.asarray(res.results[2 * b + j]["y"], np.float32)
    return out
```

```python
import numpy as np
from contextlib import ExitStack
import concourse.bass as bass
import concourse.mybir as mybir
from concourse.bass_utils import run_bass_kernel_spmd

EPOCH = 4000
DMA_SLOTS = 32


class Sched:
    ENGS = ("pe", "act", "dve", "pool", "sp")

    def __init__(self, nc, stack: ExitStack):
        self.nc = nc
        self.stack = stack
        self.ops = []
        self.last_w = {}
        self.readers = {}
        self.dma_count = {}
        self.dma_last_slot = {}
        self.serial = False

    def _deps(self, reads, writes):
        deps = set()
        for k in reads:
            w = self.last_w.get(k)
            if w is not None:
                deps.add(w)
        for k in writes:
            w = self.last_w.get(k)
            if w is not None:
                deps.add(w)
            deps.update(self.readers.get(k, ()))
        return deps

    def _commit(self, oid, reads, writes):
        for k in writes:
            self.last_w[k] = oid
            self.readers[k] = []
        for k in reads:
            if k in writes:
                continue
            self.readers.setdefault(k, []).append(oid)

    def op(self, eng, fn, reads=(), writes=()):
        oid = len(self.ops)
        deps = self._deps(reads, writes)
        if self.serial and oid > 0 and self.ops[oid - 1]["kind"] in ("op", "dma", "cc"):
            deps.add(oid - 1)
        self.ops.append(dict(id=oid, eng=eng, fn=fn, deps=deps, kind="op"))
        self._commit(oid, reads, writes)
        return oid

    def dma(self, queue, out, in_, reads=(), writes=(), **kw):
        oid = len(self.ops)
        deps = self._deps(reads, writes)
        if self.serial and oid > 0 and self.ops[oid - 1]["kind"] in ("op", "dma", "cc"):
            deps.add(oid - 1)
        n = self.dma_count.get(queue, 0)
        self.dma_count[queue] = n + 1
        slot = n % DMA_SLOTS
        prev = self.dma_last_slot.get((queue, slot))
        if prev is not None:
            deps.add(prev)
        self.dma_last_slot[(queue, slot)] = oid
        fn = lambda e: e.dma_start(out=out, in_=in_, **kw)
        self.ops.append(dict(id=oid, eng=queue, fn=fn, deps=deps, kind="dma",
                             slot=slot, val=16 * (n // DMA_SLOTS + 1)))
        self._commit(oid, reads, writes)
        return oid

    def cc(self, kind, alu, groups, in_ap, out_ap, reads=(), writes=()):
        oid = len(self.ops)
        deps = self._deps(reads, writes)
        fn = lambda e: e.collective_compute(kind, alu, replica_groups=groups, ins=[in_ap], outs=[out_ap])
        self.ops.append(dict(id=oid, eng="pool", fn=fn, deps=deps, kind="cc"))
        self._commit(oid, reads, writes)
        return oid

    def barrier(self):
        last = {}
        for o in self.ops:
            if o["kind"] == "dma":
                last[("d", o["eng"], o["slot"])] = o["id"]
            elif o["kind"] == "cc":
                last[("c", o["id"])] = o["id"]
            elif o["kind"] == "op":
                last[("e", o["eng"])] = o["id"]
        deps = set(last.values())
        for e in self.ENGS:
            self.ops.append(dict(id=len(self.ops), eng=e, fn=None, deps=set(deps), kind="bar"))
        self.last_w = {}
        self.readers = {}

    def finalize(self, final_wait_ops=()):
        nc = self.nc
        ops = self.ops
        fin = dict(id=len(ops), eng="sp", fn=None, deps=set(final_wait_ops), kind="fin")
        ops.append(fin)
        needed = set()
        for o in ops:
            for d in list(o["deps"]):
                dop = ops[d]
                if dop["kind"] == "op" and dop["eng"] == "pe" and o["eng"] == "pe" and o["kind"] == "op":
                    o["deps"].discard(d)
                    continue
                needed.add(d)
        cnt = {e: 0 for e in self.ENGS}
        for o in ops:
            if o["kind"] == "op" and o["id"] in needed:
                cnt[o["eng"]] += 1
                o["cval"] = cnt[o["eng"]]
        sems = {}
        for e in self.ENGS:
            n = (cnt[e] + EPOCH - 1) // EPOCH
            sems[e] = [self.stack.enter_context(nc.semaphore(f"s_{e}_{i}")) for i in range(max(n, 1))]
        dsems = {}
        for q in self.dma_count:
            dsems[q] = [self.stack.enter_context(nc.semaphore(f"d_{q}_{i}")) for i in range(DMA_SLOTS)]
        streams = {e: [] for e in self.ENGS}
        for o in ops:
            streams[o["eng"]].append(o)
            if o["kind"] == "cc":
                o["sem"] = self.stack.enter_context(nc.semaphore(f"cc_{o['id']}"))
        self.stats = {e: len(streams[e]) for e in self.ENGS}
        self.stats["needed"] = dict(cnt)

        def emit(eng_name, e):
            clock = {}
            for o in streams[eng_name]:
                waits = {}
                for d in o["deps"]:
                    dop = ops[d]
                    if dop["kind"] == "dma":
                        key = ("d", dop["eng"], dop["slot"])
                        val = dop["val"]
                    elif dop["kind"] == "cc":
                        key = ("c", dop["id"])
                        val = 1
                    else:
                        key = ("e", dop["eng"])
                        val = dop["cval"]
                    if clock.get(key, 0) >= val:
                        continue
                    if waits.get(key, 0) < val:
                        waits[key] = val
                for key, val in waits.items():
                    clock[key] = val
                    if key[0] == "d":
                        e.wait_ge(dsems[key[1]][key[2]], val)
                    elif key[0] == "c":
                        e.wait_ge(ops[key[1]]["sem"], 1)
                    else:
                        idx, loc = (val - 1) // EPOCH, (val - 1) % EPOCH + 1
                        e.wait_ge(sems[key[1]][idx], loc)
                if o["kind"] in ("fin", "bar"):
                    continue
                ins = o["fn"](e)
                if o["kind"] == "dma":
                    ins.then_inc(dsems[o["eng"]][o["slot"]], 16)
                elif o["kind"] == "cc":
                    ins.then_inc(o["sem"])
                elif "cval" in o:
                    v = o["cval"]
                    ins.then_inc(sems[eng_name][(v - 1) // EPOCH], 1)

        with nc.Block() as block:
            @block.tensor
            def _(e):
                emit("pe", e)

            @block.scalar
            def _(e):
                emit("act", e)

            @block.vector
            def _(e):
                emit("dve", e)

            @block.gpsimd
            def _(e):
                emit("pool", e)

            @block.sync
            def _(e):
                emit("sp", e)

F32 = mybir.dt.float32
BF16 = mybir.dt.bfloat16
AF = mybir.ActivationFunctionType
ALU = mybir.AluOpType
AX = mybir.AxisListType

D = 1024
DFF = 2816
NFF = DFF // 128
EPS = 1e-6


class Ctx:
    def __init__(self, nc, S):
        self.nc = nc
        self.S = S
        self.uid = 0

    def sb(self, st, shape, dt, name=None):
        self.uid += 1
        return st.enter_context(self.nc.sbuf_tensor(f"{name or 't'}_{self.uid}", list(shape), dt))

    def ps(self, st, shape, dt, name=None):
        self.uid += 1
        return st.enter_context(self.nc.psum_tensor(f"{name or 'p'}_{self.uid}", list(shape), dt))


def load_weight_bf16(C, W_dram, Wsb, stages, nk, ncols, gk=None, colblk=2048, tag="w", dmaq="sp"):
    S = C.S
    for k in range(nk):
        for c0 in range(0, ncols, colblk):
            cw = min(colblk, ncols - c0)
            i = C.stg_i = getattr(C, "stg_i", -1) + 1
            stg = stages[i % len(stages)]
            skey = ("stg", i % len(stages))
            S.dma(dmaq, stg[:, 0:cw], W_dram[k * 128:(k + 1) * 128, c0:c0 + cw], writes=[skey])
            eng = ("act", "dve")[i % 2]
            dst = Wsb[:, k, c0:c0 + cw]
            if gk is None:
                if eng == "act":
                    S.op("act", lambda e, d=dst, s=stg[:, 0:cw]: e.copy(out=d, in_=s), reads=[skey], writes=[(tag, k, c0)])
                else:
                    S.op(eng, lambda e, d=dst, s=stg[:, 0:cw]: e.tensor_copy(out=d, in_=s), reads=[skey], writes=[(tag, k, c0)])
            else:
                g = gk[:, k:k + 1]
                if eng == "act":
                    S.op("act", lambda e, d=dst, s=stg[:, 0:cw], g=g: e.activation(out=d, in_=s, func=AF.Copy, scale=g),
                         reads=[skey, "gk"], writes=[(tag, k, c0)])
                else:
                    S.op(eng, lambda e, d=dst, s=stg[:, 0:cw], g=g: e.tensor_scalar(out=d, in0=s, scalar1=g, scalar2=None, op0=ALU.mult),
                         reads=[skey, "gk"], writes=[(tag, k, c0)])


def wkeys(tag, nk, ncols, colblk=2048):
    return [(tag, k, c0) for k in range(nk) for c0 in range(0, ncols, colblk)]


def front_vec(C, bufs, x_dram, tok0, sub, par):
    S = C.S
    xin = bufs["xin"][sub % 2]
    nh = len(bufs["h"])
    hb = bufs["h"][sub % nh]
    ss = bufs["ss"][sub % 2]
    junk = bufs["junk"]
    kx = ("xin", sub % 2)
    kh = ("h", sub % nh)
    ks = ("ss", sub % 2)
    S.dma("sp", xin[:], x_dram[tok0:tok0 + 128, :], writes=[kx])
    S.op("act", lambda e: e.activation(out=junk[:], in_=xin[:], func=AF.Square),
         reads=[kx], writes=["junk"])
    S.op("dve", lambda e: e.tensor_reduce(out=ss[:, 0:1], in_=junk[:], axis=AX.X, op=ALU.add),
         reads=["junk"], writes=[ks])
    S.op("act", lambda e: e.activation(out=ss[:, 1:2], in_=ss[:, 0:1], func=AF.Ln, scale=1.0 / D, bias=EPS),
         reads=[ks], writes=[ks])
    S.op("act", lambda e: e.activation(out=ss[:, 2:3], in_=ss[:, 1:2], func=AF.Exp, scale=-0.5),
         reads=[ks], writes=[ks])
    S.op("act", lambda e: e.activation(out=hb[:], in_=xin[:], func=AF.Copy, scale=ss[:, 2:3]),
         reads=[kx, ks], writes=[kh])


def front_pe(C, bufs, sub, par, TT):
    S = C.S
    nh = len(bufs["h"])
    hb = bufs["h"][sub % nh]
    kh = ("h", sub % nh)
    pT = bufs["psT"][sub % 2]
    kp = ("psT", sub % 2)
    ident = bufs["ident"]
    hT = bufs["hT"][par]
    for k in range(8):
        S.op("pe", lambda e, k=k: e.transpose(out=pT[:, k * 128:(k + 1) * 128], in_=hb[:, k * 128:(k + 1) * 128], identity=ident[:]),
             reads=[kh, "ident"], writes=[kp])
    eng = "dve" if sub % 2 == 0 else "act"
    src = pT[:].rearrange("p (k t) -> p k t", k=8)
    dst = hT[:, :, sub * 128:(sub + 1) * 128]
    if eng == "act":
        S.op("act", lambda e: e.copy(out=dst, in_=src), reads=[kp], writes=[("hT", par, sub)])
    else:
        S.op("dve", lambda e: e.tensor_copy(out=dst, in_=src), reads=[kp], writes=[("hT", par, sub)])


def ffn_phase(C, x_in, x_out, w_in_d, w_out_d, pv, ident_d, T, TT=256, halo_x=None):
    S = C.S
    nc = C.nc
    NS = TT // 128
    NT = T // TT
    NB = 5
    with ExitStack() as st:
        W1 = C.sb(st, [128, 8, 2 * DFF], BF16, "W1")
        W2 = C.sb(st, [128, NFF, D], BF16, "W2")
        stages = [C.sb(st, [128, 2048], F32, "stg") for _ in range(2)]
        pvs = C.sb(st, [128, 8 + NFF * 4], F32, "pv")
        ident = C.sb(st, [128, 128], BF16, "ident")
        bufs = dict(
            xin=[C.sb(st, [128, D], F32, "xin") for _ in range(2)],
            h=[C.sb(st, [128, D], BF16, "h") for _ in range(2)],
            ss=[C.sb(st, [128, 4], F32, "ss") for _ in range(2)],
            junk=C.sb(st, [128, D], F32, "junk"),
            hT=[C.sb(st, [128, 8, TT], BF16, "hT") for _ in range(2)],
            psT=[C.ps(st, [128, D], BF16, "psT") for _ in range(2)],
            ident=ident,
        )
        actT = C.sb(st, [128, NFF, TT], BF16, "actT")
        G = [C.sb(st, [128, TT + 2], F32, "G") for _ in range(NB)]
        acc = [C.sb(st, [128, TT], F32, "acc") for _ in range(NB)]
        ge = [C.sb(st, [128, TT], F32, "ge") for _ in range(NB)]
        halo = C.sb(st, [128, NFF, 2], F32, "halo")
        xres = [C.sb(st, [128, D], F32, "xres") for _ in range(2)]
        psG = [C.ps(st, [128, 2, TT], F32, "psG") for _ in range(5)]
        psO = [C.ps(st, [128, 512], F32, "psO") for _ in range(1)]

        S.dma("sp", pvs[:], pv[:, :], writes=["gk"])
        S.dma("sp", ident[:], ident_d[:, :], writes=["ident"])
        S.op("pool", lambda e: e.memset(halo[:], 0.0), writes=[("halo", c) for c in range(NFF)])
        gk = pvs[:, 0:8]
        if halo_x is not None:
            front_vec(C, bufs, x_in, T - 128, 0, 1)
            front_pe(C, bufs, 0, 1, TT)
        w1v = w_in_d.rearrange("(k p) c -> p k c", p=128)
        ci = 0
        for half in range(2):
            for j in range(NFF // 2):
                c0 = half * DFF + j * 256
                i = C.stg_i = getattr(C, "stg_i", -1) + 1
                stg = stages[i % 2]
                skey = ("stg", i % 2)
                sv = stg[:].rearrange("p (k c) -> p k c", k=8)
                S.dma("sp", sv, w1v[:, :, c0:c0 + 256], writes=[skey])
                for k in range(8):
                    eng = ("act", "dve")[ci % 2]
                    ci += 1
                    dst = W1[:, k, c0:c0 + 256]
                    src = sv[:, k, :]
                    g = gk[:, k:k + 1]
                    if eng == "act":
                        S.op("act", lambda e, d=dst, s=src, g=g: e.activation(out=d, in_=s, func=AF.Copy, scale=g), reads=[skey, "gk"], writes=[("W1", half, j)])
                    else:
                        S.op(eng, lambda e, d=dst, s=src, g=g: e.tensor_scalar(out=d, in0=s, scalar1=g, scalar2=None, op0=ALU.mult), reads=[skey, "gk"], writes=[("W1", half, j)])
        load_weight_bf16(C, w_out_d, W2, stages, NFF, D, gk=None, tag="W2")
        W1k = [("W1", half, j) for half in range(2) for j in range(NFF // 2)]
        W2k = wkeys("W2", NFF, D)
        cwv = lambda c, j: pvs[:, 8 + c * 3 + j: 8 + c * 3 + j + 1]
        cbv = lambda c: pvs[:, 8 + NFF * 3 + c: 8 + NFF * 3 + c + 1]

        def A_vec(t):
            for s in range(NS):
                front_vec(C, bufs, x_in, t * TT + s * 128, s, t % 2)

        def A_pe(t):
            for s in range(NS):
                front_pe(C, bufs, s, t % 2, TT)

        cnt = [0]
        NB = 5

        def B(t, hook=None):
            par = t % 2
            hT = bufs["hT"][par]
            hk = [("hT", par, s) for s in range(NS)]
            slot = {}

            def st0(c):
                bi = cnt[0] % NB
                cnt[0] += 1
                slot[c] = bi
                pg = psG[bi]
                kg = ("psG", bi)
                for half, col0 in ((0, c * 128), (1, DFF + c * 128)):
                    for k in range(8):
                        S.op("pe", lambda e, k=k, half=half, col0=col0, pg=pg: e.matmul(
                            pg[:, half, :], lhsT=W1[:, k, col0:col0 + 128], rhs=hT[:, k, :], start=(k == 0), stop=(k == 7)),
                            reads=hk + [("W1", half, c // 2)], writes=[kg])
                g = G[bi]
                kG = ("G", bi)
                S.op("act", lambda e, g=g, pg=pg: e.copy(out=g[:, 2:TT + 2], in_=pg[:, 0, :]), reads=[kg], writes=[kG])
                S.op("dve", lambda e, g=g, c=c: e.tensor_copy(out=g[:, 0:2], in_=halo[:, c, :]), reads=[("halo", c)], writes=[kG])
                S.op("dve", lambda e, g=g, c=c: e.tensor_copy(out=halo[:, c, :], in_=g[:, TT:TT + 2]), reads=[kG], writes=[("halo", c)])

            def st1(c):
                bi = slot[c]
                g, a = G[bi], acc[bi]
                kG, ka = ("G", bi), ("acc", bi)
                S.op("act", lambda e, g=g, a=a, c=c: e.activation(out=a[:], in_=g[:, 2:TT + 2], func=AF.Identity, scale=cwv(c, 2), bias=cbv(c)),
                     reads=[kG, "gk"], writes=[ka])
                S.op("dve", lambda e, g=g, a=a, c=c: e.scalar_tensor_tensor(out=a[:], in0=g[:, 1:TT + 1], scalar=cwv(c, 1), in1=a[:], op0=ALU.mult, op1=ALU.add),
                     reads=[kG, "gk", ka], writes=[ka])
                S.op("dve", lambda e, g=g, a=a, c=c: e.scalar_tensor_tensor(out=a[:], in0=g[:, 0:TT], scalar=cwv(c, 0), in1=a[:], op0=ALU.mult, op1=ALU.add),
                     reads=[kG, "gk", ka], writes=[ka])

            def st2(c):
                bi = slot[c]
                a, gg = acc[bi], ge[bi]
                ka, kge = ("acc", bi), ("ge", bi)
                S.op("act", lambda e, gg=gg, a=a: e.activation(out=gg[:], in_=a[:], func=AF.Gelu_apprx_tanh), reads=[ka], writes=[kge])

            def st3(c):
                bi = slot[c]
                gg, pg = ge[bi], psG[bi]
                kge, kg = ("ge", bi), ("psG", bi)
                S.op("dve", lambda e, gg=gg, pg=pg, c=c: e.tensor_tensor(out=actT[:, c, :], in0=gg[:], in1=pg[:, 1, :], op=ALU.mult),
                     reads=[kge, kg], writes=[("actT", c)])

            stages_ = (st0, st1, st2, st3)
            for i in range(NFF + 3):
                for s_, fn in enumerate(stages_):
                    c = i - s_
                    if 0 <= c < NFF:
                        fn(c)
                if hook is not None and i == 8:
                    hook()

        def Cc(t):
            ak = [("actT", c) for c in range(NFF)]
            for s in range(NS):
                tok0 = t * TT + s * 128
                xr = xres[s % 2]
                kx = ("xres", s % 2)
                S.dma("sp", xr[:], x_in[tok0:tok0 + 128, :], writes=[kx])
                for n in range(2):
                    po = psO[0]
                    kp = ("psO", 0)
                    for c in range(NFF):
                        S.op("pe", lambda e, c=c, n=n, s=s, po=po: e.matmul(
                            po[:], lhsT=actT[:, c, s * 128:(s + 1) * 128], rhs=W2[:, c, n * 512:(n + 1) * 512], start=(c == 0), stop=(c == NFF - 1)),
                            reads=ak + W2k, writes=[kp])
                    S.op("dve", lambda e, n=n, xr=xr, po=po: e.tensor_tensor(out=xr[:, n * 512:(n + 1) * 512], in0=xr[:, n * 512:(n + 1) * 512], in1=po[:], op=ALU.add),
                         reads=[kp, kx], writes=[kx])
                S.dma("pool", x_out[tok0:tok0 + 128, :], xr[:], reads=[kx], writes=[("xout", tok0)])

        if halo_x is not None:
            flag_d, hsrc, hdst, groups = halo_x
            flg = C.sb(st, [128, 2], F32, "flg")
            hs = C.sb(st, [128, NFF, 2], F32, "hs")
            S.dma("pool", flg[:], flag_d[:, :], writes=["flg"])
            hT1 = bufs["hT"][1]
            for c in range(NFF):
                pg = psG[c % 2]
                for k in range(8):
                    S.op("pe", lambda e, k=k, c=c, pg=pg: e.matmul(pg[:, 0, 0:128], lhsT=W1[:, k, c * 128:(c + 1) * 128], rhs=hT1[:, k, 0:128], start=(k == 0), stop=(k == 7)),
                         reads=[("hT", 1, 0), ("W1", 0, c // 2)], writes=[("psG", c % 2)])
                S.op("act", lambda e, c=c, pg=pg: e.copy(out=hs[:, c, :], in_=pg[:, 0, 126:128]), reads=[("psG", c % 2)], writes=["hs"])
            S.dma("pool", hsrc[:, :], hs[:].rearrange("p c j -> p (c j)"), reads=["hs"], writes=["hsrc"])
            S.cc("AllGather", ALU.bypass, groups, hsrc, hdst, reads=["hsrc"], writes=["hdst"])
            S.dma("pool", halo[:].rearrange("p c j -> p (c j)"), hdst[0:128, :], reads=["hdst"], writes=[("halo", c) for c in range(NFF)])
            S.op("dve", lambda e: e.tensor_scalar(out=halo[:], in0=halo[:], scalar1=flg[:, 0:1], scalar2=None, op0=ALU.mult),
                 reads=["flg"] + [("halo", c) for c in range(NFF)], writes=[("halo", c) for c in range(NFF)])
        A_vec(0)
        A_pe(0)
        for t in range(NT):
            nxt = (lambda t=t: A_vec(t + 1)) if t + 1 < NT else None
            B(t, hook=nxt)
            if t + 1 < NT:
                A_pe(t + 1)
            Cc(t)
        S.barrier()


def xattn_phase(C, x_in, x_out, mem_d, wq_d, wkv_d, wo_d, pv, ident_d, T, TT=512):
    S = C.S
    NS = TT // 128
    NT = T // TT
    with ExitStack() as st:
        Wq = C.sb(st, [128, 8, D], BF16, "Wq")
        Wo = C.sb(st, [128, 8, D], BF16, "Wo")
        Wkv = C.sb(st, [128, 8, 2 * D], BF16, "Wkv")
        stages = [C.sb(st, [128, 2048], F32, "stg") for _ in range(2)]
        pvs = C.sb(st, [128, 16 + 512], F32, "pv")
        ident = C.sb(st, [128, 128], BF16, "ident")
        ones = C.sb(st, [128, 128], BF16, "ones")
        gqk = C.sb(st, [128, 256], F32, "gqk")
        bufs = dict(
            xin=[C.sb(st, [128, D], F32, "xin") for _ in range(2)],
            h=[C.sb(st, [128, D], BF16, "h") for _ in range(NS)],
            ss=[C.sb(st, [128, 4], F32, "ss") for _ in range(2)],
            junk=C.sb(st, [128, D], F32, "junk"),
            hT=[C.sb(st, [128, 8, TT], BF16, "hT") for _ in range(2)],
            psT=[C.ps(st, [128, D], BF16, "psT") for _ in range(2)],
            ident=ident,
        )
        mkT = C.sb(st, [128, 8, 256], BF16, "mkT")
        mv = C.sb(st, [128, 2, D], BF16, "mv")
        qn = [C.sb(st, [128, D], BF16, "qn") for _ in range(2)]
        qss = [C.sb(st, [128, 12], F32, "qss") for _ in range(2)]
        qT = C.sb(st, [128, 8, TT], BF16, "qT")
        pT = [C.sb(st, [128, 2, TT], BF16, "pT") for _ in range(2)]
        oT = C.sb(st, [128, 8, TT], BF16, "oT")
        rec = [C.sb(st, [128, TT], F32, "rec") for _ in range(2)]
        xres = [C.sb(st, [128, D], F32, "xres") for _ in range(2)]
        psA = [C.ps(st, [128, 512], F32, "psA") for _ in range(2)]
        psL = [C.ps(st, [128, 512], F32, "psL") for _ in range(2)]
        psO = [C.ps(st, [128, 512], F32, "psO") for _ in range(2)]
        psT = bufs["psT"]
        junk = bufs["junk"]

        S.dma("sp", pvs[:], pv[:, :], writes=["gk"])
        S.dma("sp", ident[:], ident_d[:, :], writes=["ident"])
        S.op("pool", lambda e: e.memset(ones[:], 1.0), writes=["ones"])
        S.op("dve", lambda e: e.scalar_tensor_tensor(out=gqk[:], in0=pvs[:, 16:272], scalar=1.0 / 16.0, in1=pvs[:, 272:528], op0=ALU.mult, op1=ALU.mult),
             reads=["gk"], writes=["gqk"])
        for mt in range(2):
            front_vec(C, bufs, mem_d, mt * 128, mt, 0)
            front_pe(C, bufs, mt, 0, TT)
        load_weight_bf16(C, wkv_d, Wkv, stages, 8, 2 * D, gk=pvs[:, 8:16], tag="Wkv")
        load_weight_bf16(C, wq_d, Wq, stages, 8, D, gk=pvs[:, 0:8], tag="Wq")
        load_weight_bf16(C, wo_d, Wo, stages, 8, D, gk=None, tag="Wo")
        Wkvk, Wqk, Wok = wkeys("Wkv", 8, 2 * D), wkeys("Wq", 8, D), wkeys("Wo", 8, D)

        def headnorm_stats(ps_banks, ss, kss, bank_keys):
            for n in range(2):
                S.op("act", lambda e, n=n: e.activation(out=junk[:, 0:512], in_=ps_banks[n][:], func=AF.Square), reads=[bank_keys[n]], writes=["junk"])
                S.op("dve", lambda e, n=n: e.tensor_reduce(out=ss[:, 2 * n:2 * n + 2], in_=junk[:, 0:512].rearrange("p (a b) -> p a b", a=2), axis=AX.X, op=ALU.add),
                     reads=["junk"], writes=[kss])
            S.op("act", lambda e: e.activation(out=ss[:, 4:8], in_=ss[:, 0:4], func=AF.Ln, scale=1.0 / 256.0, bias=EPS), reads=[kss], writes=[kss])
            S.op("act", lambda e: e.activation(out=ss[:, 8:12], in_=ss[:, 4:8], func=AF.Exp, scale=-0.5), reads=[kss], writes=[kss])

        memT = bufs["hT"][0]
        banks = [psA[0], psA[1], psL[0], psL[1]]
        bkeys = [("psA", 0), ("psA", 1), ("psL", 0), ("psL", 1)]
        for mt in range(2):
            for n in range(4):
                for k in range(8):
                    S.op("pe", lambda e, k=k, n=n, mt=mt: e.matmul(banks[n][:], lhsT=memT[:, k, mt * 128:(mt + 1) * 128], rhs=Wkv[:, k, n * 512:(n + 1) * 512], start=(k == 0), stop=(k == 7)),
                         reads=[("hT", 0, mt)] + Wkvk, writes=[bkeys[n]])
            ss = qss[mt]
            kss = ("qss", mt)
            headnorm_stats(banks[0:2], ss, kss, bkeys[0:2])
            mkn = qn[mt]
            for hd in range(4):
                S.op("dve", lambda e, hd=hd, ss=ss, mkn=mkn: e.scalar_tensor_tensor(
                    out=mkn[:, hd * 256:(hd + 1) * 256], in0=banks[hd // 2][:, (hd % 2) * 256:(hd % 2 + 1) * 256],
                    scalar=ss[:, 8 + hd:9 + hd], in1=gqk[:], op0=ALU.mult, op1=ALU.mult),
                    reads=[bkeys[hd // 2], kss, "gqk"], writes=[("qn", mt)])
            for n in (2, 3):
                S.op("act", lambda e, n=n, mt=mt: e.copy(out=mv[:, mt, (n - 2) * 512:(n - 1) * 512], in_=banks[n][:]), reads=[bkeys[n]], writes=[("mv", mt)])
            pt = psT[mt]
            for c in range(8):
                S.op("pe", lambda e, c=c, pt=pt, mkn=mkn: e.transpose(out=pt[:, c * 128:(c + 1) * 128], in_=mkn[:, c * 128:(c + 1) * 128], identity=ident[:]),
                     reads=[("qn", mt), "ident"], writes=[("psT", mt)])
            S.op("dve", lambda e, pt=pt, mt=mt: e.tensor_copy(out=mkT[:, :, mt * 128:(mt + 1) * 128], in_=pt[:].rearrange("p (k t) -> p k t", k=8)),
                 reads=[("psT", mt)], writes=[("mkT", mt)])
        mkTk = [("mkT", 0), ("mkT", 1)]
        mvk = [("mv", 0), ("mv", 1)]

        def A_vec(t):
            for s in range(NS):
                front_vec(C, bufs, x_in, t * TT + s * 128, s, t % 2)

        def A_pe(t):
            for s in range(NS):
                front_pe(C, bufs, s, t % 2, TT)

        def Q(t):
            par = t % 2
            hT = bufs["hT"][par]
            pairs = ((psA, [("psA", 0), ("psA", 1)]), (psL, [("psL", 0), ("psL", 1)]))

            def proj(s):
                bk, bkk = pairs[s % 2]
                for n in range(2):
                    for k in range(8):
                        S.op("pe", lambda e, k=k, n=n, s=s, bk=bk: e.matmul(bk[n][:], lhsT=hT[:, k, s * 128:(s + 1) * 128], rhs=Wq[:, k, n * 512:(n + 1) * 512], start=(k == 0), stop=(k == 7)),
                             reads=[("hT", par, s)] + Wqk, writes=[bkk[n]])

            def norm(s):
                bk, bkk = pairs[s % 2]
                ss = qss[s % 2]
                kss = ("qss", s % 2)
                headnorm_stats(bk, ss, kss, bkk)
                q = qn[s % 2]
                for hd in range(4):
                    src = bk[hd // 2][:, (hd % 2) * 256:(hd % 2 + 1) * 256]
                    dst = q[:, hd * 256:(hd + 1) * 256]
                    if hd % 2 == 0:
                        S.op("act", lambda e, src=src, dst=dst, ss=ss, hd=hd: e.activation(out=dst, in_=src, func=AF.Copy, scale=ss[:, 8 + hd:9 + hd]),
                             reads=[bkk[hd // 2], kss], writes=[("qn", s % 2)])
                    else:
                        S.op("dve", lambda e, src=src, dst=dst, ss=ss, hd=hd: e.tensor_scalar(out=dst, in0=src, scalar1=ss[:, 8 + hd:9 + hd], scalar2=None, op0=ALU.mult),
                             reads=[bkk[hd // 2], kss], writes=[("qn", s % 2)])

            def trans(s):
                q = qn[s % 2]
                pt = psT[s % 2]
                for c in range(8):
                    S.op("pe", lambda e, c=c, pt=pt, q=q: e.transpose(out=pt[:, c * 128:(c + 1) * 128], in_=q[:, c * 128:(c + 1) * 128], identity=ident[:]),
                         reads=[("qn", s % 2), "ident"], writes=[("psT", s % 2)])
                S.op("act" if s % 2 else "dve",
                     (lambda e, pt=pt, s=s: e.copy(out=qT[:, :, s * 128:(s + 1) * 128], in_=pt[:].rearrange("p (k t) -> p k t", k=8))) if s % 2 else
                     (lambda e, pt=pt, s=s: e.tensor_copy(out=qT[:, :, s * 128:(s + 1) * 128], in_=pt[:].rearrange("p (k t) -> p k t", k=8))),
                     reads=[("psT", s % 2)], writes=[("qT", s)])

            proj(0)
            for s in range(NS):
                norm(s)
                if s + 1 < NS:
                    proj(s + 1)
                trans(s)

        def ATT(t):
            qTk = [("qT", s) for s in range(NS)]

            def logits(hd):
                p = pT[hd % 2]
                kp = ("pT", hd % 2)
                for mt in range(2):
                    for dc in range(2):
                        S.op("pe", lambda e, mt=mt, dc=dc, hd=hd: e.matmul(psL[mt][:, 0:TT], lhsT=mkT[:, hd * 2 + dc, mt * 128:(mt + 1) * 128], rhs=qT[:, hd * 2 + dc, :], start=(dc == 0), stop=(dc == 1)),
                             reads=qTk + mkTk, writes=[("psL", mt)])
                    S.op("act", lambda e, mt=mt, p=p: e.activation(out=p[:, mt, :], in_=psL[mt][:, 0:TT], func=AF.Exp), reads=[("psL", mt)], writes=[kp])

            def pv(hd):
                p = pT[hd % 2]
                kp = ("pT", hd % 2)
                pa = psA[hd % 2]
                for mt in range(2):
                    S.op("pe", lambda e, mt=mt, p=p, pa=pa: e.matmul(pa[:, 0:TT], lhsT=ones[:], rhs=p[:, mt, :], start=(mt == 0), stop=(mt == 1)),
                         reads=[kp, "ones"], writes=[("psA", hd % 2)])
                r = rec[hd % 2]
                S.op("act", lambda e, r=r, pa=pa: e.activation(out=r[:], in_=pa[:, 0:TT], func=AF.Ln), reads=[("psA", hd % 2)], writes=[("rec", hd % 2)])
                S.op("act", lambda e, r=r: e.activation(out=r[:], in_=r[:], func=AF.Exp, scale=-1.0), reads=[("rec", hd % 2)], writes=[("rec", hd % 2)])
                for dc in range(2):
                    for mt in range(2):
                        S.op("pe", lambda e, mt=mt, dc=dc, hd=hd, p=p: e.matmul(psO[dc][:, 0:TT], lhsT=mv[:, mt, hd * 256 + dc * 128: hd * 256 + (dc + 1) * 128], rhs=p[:, mt, :], start=(mt == 0), stop=(mt == 1)),
                             reads=[kp] + mvk, writes=[("psO", dc)])
                    S.op("dve", lambda e, dc=dc, hd=hd, r=r: e.tensor_tensor(out=oT[:, hd * 2 + dc, :], in0=psO[dc][:, 0:TT], in1=r[:], op=ALU.mult),
                         reads=[("psO", dc), ("rec", hd % 2)], writes=[("oT", hd * 2 + dc)])

            logits(0)
            for hd in range(4):
                if hd + 1 < 4:
                    logits(hd + 1)
                pv(hd)

        def OUT(t):
            ok = [("oT", c) for c in range(8)]
            pairs = ((psA, [("psA", 0), ("psA", 1)]), (psL, [("psL", 0), ("psL", 1)]))
            for s in range(NS):
                tok0 = t * TT + s * 128
                bk, bkk = pairs[s % 2]
                xr = xres[s % 2]
                kx = ("xres", s % 2)
                S.dma("sp", xr[:], x_in[tok0:tok0 + 128, :], writes=[kx])
                for n in range(2):
                    for c in range(8):
                        S.op("pe", lambda e, c=c, n=n, s=s, bk=bk: e.matmul(bk[n][:], lhsT=oT[:, c, s * 128:(s + 1) * 128], rhs=Wo[:, c, n * 512:(n + 1) * 512], start=(c == 0), stop=(c == 7)),
                             reads=ok + Wok, writes=[bkk[n]])
                    S.op("dve", lambda e, n=n, xr=xr, bk=bk: e.tensor_tensor(out=xr[:, n * 512:(n + 1) * 512], in0=xr[:, n * 512:(n + 1) * 512], in1=bk[n][:], op=ALU.add),
                         reads=[bkk[n], kx], writes=[kx])
                S.dma("pool", x_out[tok0:tok0 + 128, :], xr[:], reads=[kx], writes=[("xout", tok0)])

        C.dbg_src = dict(qT=qT, mkT=mkT, oT=oT, mv=mv, pT0=pT[0], pT1=pT[1], rec0=rec[0], rec1=rec[1])
        A_vec(0)
        A_pe(0)
        for t in range(NT):
            Q(t)
            if t + 1 < NT:
                A_vec(t + 1)
            ATT(t)
            if t + 1 < NT:
                A_pe(t + 1)
            OUT(t)
        S.barrier()
        for name, ap in getattr(C, "dbg", {}).items():
            S.dma("sp", ap, C.dbg_src[name][:], writes=[("dbg", name)])
        S.barrier()


def even_consts(T):
    half = 64
    inv = (1.0 / (10000.0 ** (np.arange(half, dtype=np.float32) / half))).astype(np.float32)
    pos = np.arange(T, dtype=np.float32)
    ang = (pos[:, None] * inv[None, :]).astype(np.float32)
    cos, sin = np.cos(ang.astype(np.float64)), np.sin(ang.astype(np.float64))
    sc = 128.0 ** -0.5
    rot = np.stack([np.tile(cos, (1, 4)), np.tile(sin, (1, 4)), np.tile(cos * sc, (1, 4)), np.tile(sin * sc, (1, 4))], axis=1)
    rot = rot.astype(np.float32)
    lg = np.log(1.0 - np.exp2(-5.0 - np.arange(4, dtype=np.float64)))
    idx = np.arange(128, dtype=np.float64)
    diff = idx[None, :] - idx[:, None]
    dmask = np.where(diff[None] >= 0, np.exp(lg[:, None, None] * np.maximum(diff[None], 0)), 0.0)
    dmask = dmask.transpose(1, 0, 2).reshape(128, 512)
    xi = np.exp(lg[:, None] * (idx + 1.0))
    zeta = np.exp(lg[:, None] * (127.0 - idx))
    XI = np.repeat(xi.T[:, :, None], 128, axis=2).reshape(128, 512)
    ZE = np.repeat(zeta.T[:, :, None], 128, axis=2).reshape(128, 512)
    ch0 = (idx >= 64)
    cm = ((idx[:, None] <= idx[None, :]) & (ch0[:, None] == ch0[None, :])).astype(np.float64)
    cmask = np.tile(cm[:, None, :], (1, 4, 1)).reshape(128, 512)
    c32 = np.concatenate([dmask, XI, ZE, cmask], axis=1).astype(np.float32)
    MT = (idx[:, None] <= idx[None, :]).astype(np.float64)
    ch = (idx >= 64).astype(np.int64)
    same = (ch[:, None] == ch[None, :]).astype(np.float64)
    midt = np.where(ch == 0, 31, 95)
    MA = same * MT - same * (idx[:, None] <= midt[None, :]).astype(np.float64)
    MD = (idx[:, None] > idx[None, :]).astype(np.float64)
    ones = np.ones((128, 128))
    MB = MT - (idx[:, None] <= 63).astype(np.float64)
    bq = np.where(idx < 64, -300.0, 0.0)[:, None]
    bk = np.where(idx < 64, 0.0, -300.0)[:, None]
    mats = np.concatenate([MA, MT, MD, ones, MB, bq, bk], axis=1).astype(np.float32)
    g128 = [float(np.exp(l * 128.0)) for l in lg]
    return rot, c32, mats, g128


def even_phase(C, x_in, x_out, w_in_d, w_out_d, pv, rot_d, c32_d, mats_d, g128, ident_d, T, TT=256, prev=None):
    S = C.S
    NS = TT // 128
    NT = T // TT
    with ExitStack() as st:
        Win = C.sb(st, [128, 8, 4096], BF16, "Win")
        Wout = C.sb(st, [128, 8, D], BF16, "Wout")
        pvs = C.sb(st, [128, 16 + 1536], F32, "pv")
        ident = C.sb(st, [128, 128], BF16, "ident")
        c32 = C.sb(st, [128, 2048], F32, "c32")
        mats = C.sb(st, [128, 642], F32, "mats")
        lbt = C.sb(st, [128, 1024], F32, "lbt")
        bufs = dict(
            xin=[C.sb(st, [128, D], F32, "xin") for _ in range(2)],
            h=[C.sb(st, [128, D], BF16, "h") for _ in range(NS)],
            ss=[C.sb(st, [128, 4], F32, "ss") for _ in range(2)],
            junk=C.sb(st, [128, D], F32, "junk"),
            hT=[C.sb(st, [128, 8, TT], BF16, "hT") for _ in range(2)],
            psT=[C.ps(st, [128, D], BF16, "psT") for _ in range(2)],
            ident=ident,
        )
        psT = bufs["psT"]
        junk = bufs["junk"]
        P = [C.ps(st, [128, 512], F32, "P") for _ in range(4)]
        X = [C.ps(st, [128, 512], F32, "X") for _ in range(2)]
        Pk = [("P", i) for i in range(4)]
        Xk = [("X", i) for i in range(2)]
        rot = [C.sb(st, [128, 4, 256], F32, "rot") for _ in range(2)]
        tq = [C.sb(st, [128, 256], F32, "tq") for _ in range(4)]
        WF = {"r": [C.sb(st, [128, 512], F32, "wr") for _ in range(4)], "g": [C.sb(st, [128, 512], F32, "wg") for _ in range(8)]}
        WFK = {"r": [("wr", i) for i in range(4)], "g": [("wg", i) for i in range(8)]}
        BB = {"r": {n: C.sb(st, [128, 512], BF16, "r" + n) for n in ("qa", "kb", "qc", "kd", "v", "PT", "qcT")},
              "g": {n: C.sb(st, [128, 512], BF16, "g" + n) for n in ("qa", "kb", "qc", "kd", "v", "PT", "qcT", "qa2", "kb2")}}
        QKT = {m: C.sb(st, [128, 1024], BF16, "qkT" + m) for m in ("r", "g")}
        qkT2 = C.sb(st, [128, 1024], BF16, "qkT2")
        GATE = {m: C.sb(st, [128, 512], F32, "gate" + m) for m in ("r", "g")}
        YT = {m: C.sb(st, [128, 512], F32, "ytmp" + m) for m in ("r", "g")}
        y = C.sb(st, [128, D], BF16, "y")
        yT = C.sb(st, [128, 8, 128], BF16, "yT")
        Sst = {m: C.sb(st, [128, 512], F32, "S" + m) for m in ("r", "g")}
        Sbf = {m: C.sb(st, [128, 512], BF16, "Sb" + m) for m in ("r", "g")}
        stat = C.sb(st, [128, 64], F32, "stat")
        Ecol = C.sb(st, [128, 4], F32, "Ecol")
        xres = [C.sb(st, [128, D], F32, "xres") for _ in range(1)]
        Wg = WF["g"]
        Wgk = WFK["g"]

        def bk(m, n):
            return (m, n)

        S.dma("sp", pvs[:], pv[:, :], writes=["gk"])
        S.dma("sp", ident[:], ident_d[:, :], writes=["ident"])
        S.dma("sp", c32[:], c32_d[:, :], writes=["c32"])
        S.dma("sp", mats[:], mats_d[:, :], writes=["mats"])
        for m in ("r", "g"):
            S.op("pool", lambda e, m=m: e.memset(Sst[m][:], 0.0), writes=[("S", m)])
            S.op("pool", lambda e, m=m: e.memset(Sbf[m][:], 0.0), writes=[("Sb", m)])
        lg3 = pvs[:, 16:16 + 1536]
        S.op("act", lambda e: e.activation(out=junk[:, :], in_=lg3[:, 0:1024], func=AF.Exp), reads=["gk"], writes=["junk"])
        S.op("act", lambda e: e.activation(out=Wg[0][:], in_=lg3[:, 1024:1536], func=AF.Exp), reads=["gk"], writes=[Wgk[0]])
        S.op("dve", lambda e: e.tensor_tensor(out=Wg[0][:], in0=Wg[0][:], in1=junk[:, 512:1024], op=ALU.add), reads=["junk", Wgk[0]], writes=[Wgk[0]])
        S.op("dve", lambda e: e.tensor_tensor(out=Wg[0][:], in0=Wg[0][:], in1=junk[:, 0:512], op=ALU.add), reads=["junk", Wgk[0]], writes=[Wgk[0]])
        S.op("dve", lambda e: e.reciprocal(out=Wg[0][:], in_=Wg[0][:]), reads=[Wgk[0]], writes=[Wgk[0]])
        S.op("dve", lambda e: e.tensor_tensor(out=Wg[1][:], in0=Wg[0][:], in1=junk[:, 0:512], op=ALU.mult), reads=["junk", Wgk[0]], writes=[Wgk[1]])
        S.op("dve", lambda e: e.tensor_scalar(out=lbt[:, 512:1024], in0=Wg[1][:], scalar1=-0.5, scalar2=0.5, op0=ALU.mult, op1=ALU.add), reads=[Wgk[1]], writes=["lbt"])
        S.op("dve", lambda e: e.tensor_tensor(out=lbt[:, 0:512], in0=Wg[1][:], in1=lbt[:, 512:1024], op=ALU.add), reads=[Wgk[1], "lbt"], writes=["lbt"])
        S.op("dve", lambda e: e.tensor_scalar(out=pvs[:, 8:16], in0=pvs[:, 8:16], scalar1=0.5, scalar2=None, op0=ALU.mult), reads=["gk"], writes=["gk"])
        stages = [Wg[4], Wg[5], Wg[6], Wg[7]]
        C.stg_i = -1
        load_weight_bf16(C, w_in_d, Win, stages, 8, 4096, gk=pvs[:, 0:8], colblk=512, tag="Win")
        load_weight_bf16(C, w_out_d, Wout, stages, 8, D, gk=pvs[:, 8:16], colblk=512, tag="Wout")
        S.barrier()
        Wink, Woutk = [], []
        dmask, XI, ZE, cmask = c32[:, 0:512], c32[:, 512:1024], c32[:, 1024:1536], c32[:, 1536:2048]
        MA, MT, MD, ones32, MB = mats[:, 0:128], mats[:, 128:256], mats[:, 256:384], mats[:, 384:512], mats[:, 512:640]
        bq, bkk_ = mats[:, 640:641], mats[:, 641:642]
        lbh, omlh = lbt[:, 0:512], lbt[:, 512:1024]

        def A_vec(t):
            for s in range(NS):
                front_vec(C, bufs, x_in, t * TT + s * 128, s, t % 2)

        def A_pe(t):
            for s in range(NS):
                front_pe(C, bufs, s, t % 2, TT)

        def proj(par, s, groups):
            hT = bufs["hT"][par]
            for gi_, g in enumerate(groups):
                for k in range(8):
                    S.op("pe", lambda e, gi_=gi_, g=g, k=k: e.matmul(P[gi_][:], lhsT=hT[:, k, s * 128:(s + 1) * 128], rhs=Win[:, k, g * 512:(g + 1) * 512], start=(k == 0), stop=(k == 7)),
                         reads=[("hT", par, s)], writes=[Pk[gi_]])

        def rotary_g(rt, kr, eng, src, dst, ci, si, kin, kout, tmps, tk):
            x1 = src[:].rearrange("p (h two d) -> p h two d", h=4, two=2)[:, :, 0, :]
            x2 = src[:].rearrange("p (h two d) -> p h two d", h=4, two=2)[:, :, 1, :]
            o1 = dst[:].rearrange("p (h two d) -> p h two d", h=4, two=2)[:, :, 0, :]
            o2 = dst[:].rearrange("p (h two d) -> p h two d", h=4, two=2)[:, :, 1, :]
            cs = rt[:, ci, :].rearrange("p (h d) -> p h d", h=4)
            sn = rt[:, si, :].rearrange("p (h d) -> p h d", h=4)
            t1 = tmps[0][:].rearrange("p (h d) -> p h d", h=4)
            t2 = tmps[1][:].rearrange("p (h d) -> p h d", h=4)
            S.op(eng, lambda e: e.tensor_tensor(out=t1, in0=x1, in1=cs, op=ALU.mult), reads=[kin, kr], writes=[tk[0]])
            S.op(eng, lambda e: e.tensor_tensor(out=t2, in0=x2, in1=sn, op=ALU.mult), reads=[kin, kr], writes=[tk[1]])
            S.op(eng, lambda e: e.tensor_tensor(out=o1, in0=t1, in1=t2, op=ALU.subtract), reads=[tk[0], tk[1]], writes=[kout])
            S.op(eng, lambda e: e.tensor_tensor(out=t1, in0=x1, in1=sn, op=ALU.mult), reads=[kin, kr, kout], writes=[tk[0]])
            S.op(eng, lambda e: e.tensor_tensor(out=t2, in0=x2, in1=cs, op=ALU.mult), reads=[kin, kr, kout], writes=[tk[1]])
            S.op(eng, lambda e: e.tensor_tensor(out=o2, in0=t1, in1=t2, op=ALU.add), reads=[tk[0], tk[1]], writes=[kout])

        def linattn(mask, Eh, m, second=False):
            Bb = BB[m]
            qa, kb, qc, kd, v, PT, qcT = (Bb[n] for n in ("qa", "kb", "qc", "kd", "v", "PT", "qcT"))
            qkT = QKT[m]
            kqkT = ("qkT", m)
            ytmp = YT[m]
            for hh in range(4):
                blk = slice(hh * 128, (hh + 1) * 128)
                S.op("pe", lambda e, blk=blk: e.transpose(out=psT[0][:, blk], in_=qa[:, blk], identity=ident[:]), reads=[bk(m, "qa"), "ident"], writes=[("psT", 0)])
            for hh in range(4):
                blk = slice(hh * 128, (hh + 1) * 128)
                blk2 = slice(512 + hh * 128, 512 + (hh + 1) * 128)
                S.op("pe", lambda e, blk=blk, blk2=blk2: e.transpose(out=psT[0][:, blk2], in_=kb[:, blk], identity=ident[:]), reads=[bk(m, "kb"), "ident"], writes=[("psT", 0)])
            for hh in range(4):
                blk = slice(hh * 128, (hh + 1) * 128)
                S.op("pe", lambda e, blk=blk: e.transpose(out=psT[1][:, blk], in_=qc[:, blk], identity=ident[:]), reads=[bk(m, "qc"), "ident"], writes=[("psT", 1)])
            S.op("act", lambda e: e.copy(out=qkT[:], in_=psT[0][:]), reads=[("psT", 0)], writes=[kqkT])
            S.op("dve", lambda e: e.tensor_copy(out=qcT[:], in_=psT[1][:, 0:512]), reads=[("psT", 1)], writes=[bk(m, "qcT")])
            for hh in range(4):
                blk = slice(hh * 128, (hh + 1) * 128)
                blk2 = slice(512 + hh * 128, 512 + (hh + 1) * 128)
                S.op("pe", lambda e, blk=blk, blk2=blk2: e.matmul(X[0][:, blk], lhsT=qkT[:, blk2], rhs=qkT[:, blk], start=True, stop=True), reads=[kqkT], writes=[Xk[0]])
            if second:
                qa2, kb2 = Bb["qa2"], Bb["kb2"]
                for hh in range(4):
                    blk = slice(hh * 128, (hh + 1) * 128)
                    S.op("pe", lambda e, blk=blk: e.transpose(out=psT[0][:, blk], in_=qa2[:, blk], identity=ident[:]), reads=[bk(m, "qa2"), "ident"], writes=[("psT", 0)])
                for hh in range(4):
                    blk = slice(hh * 128, (hh + 1) * 128)
                    blk2 = slice(512 + hh * 128, 512 + (hh + 1) * 128)
                    S.op("pe", lambda e, blk=blk, blk2=blk2: e.transpose(out=psT[0][:, blk2], in_=kb2[:, blk], identity=ident[:]), reads=[bk(m, "kb2"), "ident"], writes=[("psT", 0)])
                S.op("act", lambda e: e.copy(out=qkT2[:], in_=psT[0][:]), reads=[("psT", 0)], writes=["qkT2"])
                for hh in range(4):
                    blk = slice(hh * 128, (hh + 1) * 128)
                    blk2 = slice(512 + hh * 128, 512 + (hh + 1) * 128)
                    S.op("pe", lambda e, blk=blk, blk2=blk2: e.matmul(P[0][:, blk], lhsT=qkT2[:, blk2], rhs=qkT2[:, blk], start=True, stop=True), reads=["qkT2"], writes=[Pk[0]])
                S.op("dve", lambda e: e.tensor_tensor(out=ytmp[:], in0=X[0][:], in1=mask, op=ALU.mult), reads=[Xk[0], "c32"], writes=[("ytmp", m)])
                S.op("dve", lambda e: e.tensor_tensor(out=PT[:], in0=ytmp[:], in1=P[0][:], op=ALU.add), reads=[("ytmp", m), Pk[0]], writes=[bk(m, "PT")])
            else:
                S.op("dve", lambda e: e.tensor_tensor(out=PT[:], in0=X[0][:], in1=mask, op=ALU.mult), reads=[Xk[0], "c32"], writes=[bk(m, "PT")])
            for hh in range(4):
                blk = slice(hh * 128, (hh + 1) * 128)
                S.op("pe", lambda e, blk=blk: e.matmul(X[1][:, blk], lhsT=PT[:, blk], rhs=v[:, blk], start=True, stop=False), reads=[bk(m, "PT"), bk(m, "v")], writes=[Xk[1]])
                S.op("pe", lambda e, blk=blk: e.matmul(X[1][:, blk], lhsT=qcT[:, blk], rhs=Sbf[m][:, blk], start=False, stop=True), reads=[bk(m, "qcT"), ("Sb", m)], writes=[Xk[1]])
            for hh in range(4):
                blk = slice(hh * 128, (hh + 1) * 128)
                S.op("pe", lambda e, blk=blk: e.matmul(X[0][:, blk], lhsT=kd[:, blk], rhs=v[:, blk], start=True, stop=True), reads=[bk(m, "kd"), bk(m, "v")], writes=[Xk[0]])
            for hh in range(4):
                blk = slice(hh * 128, (hh + 1) * 128)
                S.op("dve", lambda e, blk=blk, hh=hh: e.scalar_tensor_tensor(out=Sst[m][:, blk], in0=Sst[m][:, blk], scalar=Eh(hh), in1=X[0][:, blk], op0=ALU.mult, op1=ALU.add),
                     reads=[Xk[0], ("S", m), "Ecol"], writes=[("S", m)])
            S.op("act", lambda e: e.copy(out=Sbf[m][:], in_=Sst[m][:]), reads=[("S", m)], writes=[("Sb", m)])

        def gate_evac(m, bank, kbank):
            g = GATE[m]
            S.op("act", lambda e: e.activation(out=g[:], in_=bank[:], func=AF.Tanh, scale=0.5), reads=[kbank], writes=[("gate", m)])
            S.op("dve", lambda e: e.scalar_tensor_tensor(out=g[:], in0=g[:], scalar=1.0, in1=bank[:], op0=ALU.add, op1=ALU.mult), reads=[kbank, ("gate", m)], writes=[("gate", m)])

        def hg_gates(f, kf, key, kkey, logf, klogf):
            S.op("dve", lambda e: e.tensor_tensor(out=f[:], in0=f[:], in1=omlh, op=ALU.mult), reads=[kf, "lbt"], writes=[kf])
            S.op("dve", lambda e: e.tensor_tensor(out=f[:], in0=f[:], in1=lbh, op=ALU.add), reads=[kf, "lbt"], writes=[kf])
            S.op("pool", lambda e: e.tensor_scalar(out=key[:], in0=f[:], scalar1=-1.0, scalar2=1.0, op0=ALU.mult, op1=ALU.add), reads=[kf], writes=[kkey])
            S.op("dve", lambda e: e.tensor_scalar_max(out=logf[:], in0=f[:], scalar1=1e-6), reads=[kf], writes=[klogf])
            S.op("act", lambda e: e.activation(out=logf[:], in_=logf[:], func=AF.Ln), reads=[klogf], writes=[klogf])

        def state_only(par, s, tok0, rotp_d):
            proj(par, s, (1, 2, 5, 6))
            rt = rot[(tok0 // 128) % 2]
            kr = ("rot", (tok0 // 128) % 2)
            S.dma("sp", rt[:], rotp_d[tok0:tok0 + 128, :, :], writes=[kr])
            Wr, Wrk = WF["r"], WFK["r"]
            kf, kr_ = Wr[1], Wr[3]
            f, key, logf, Dk = Wg[1], Wg[2], Wg[3], Wg[7]
            v1, v2, kd1, kd2 = BB["r"]["v"], BB["g"]["v"], BB["r"]["kd"], BB["g"]["kd"]
            S.op("act", lambda e: e.copy(out=kf[:], in_=P[0][:]), reads=[Pk[0]], writes=[Wrk[1]])
            S.op("act", lambda e: e.copy(out=v1[:], in_=P[1][:]), reads=[Pk[1]], writes=[bk("r", "v")])
            S.op("act", lambda e: e.activation(out=f[:], in_=P[2][:], func=AF.Tanh, scale=0.5), reads=[Pk[2]], writes=[Wgk[1]])
            S.op("act", lambda e: e.copy(out=v2[:], in_=P[3][:]), reads=[Pk[3]], writes=[bk("g", "v")])
            rotary_g(rt, kr, "dve", kf, kr_, 2, 3, Wrk[1], Wrk[3], tq[2:4], [("tq", 2), ("tq", 3)])
            S.op("pool", lambda e: e.tensor_tensor(out=kd1[:], in0=kr_[:], in1=ZE, op=ALU.mult), reads=[Wrk[3], "c32"], writes=[bk("r", "kd")])
            hg_gates(f, Wgk[1], key, Wgk[2], logf, Wgk[3])
            S.op("pe", lambda e: e.matmul(P[2][:], lhsT=MD, rhs=logf[:], start=True, stop=True), reads=[Wgk[3], "mats"], writes=[Pk[2]])
            for hh in range(4):
                S.op("pe", lambda e, hh=hh: e.matmul(P[3][:, hh:hh + 1], lhsT=logf[:, hh * 128:(hh + 1) * 128], rhs=ones32[:, 0:1], start=True, stop=True),
                     reads=[Wgk[3], "mats"], writes=[Pk[3]])
            S.op("act", lambda e: e.activation(out=Dk[:], in_=P[2][:], func=AF.Exp), reads=[Pk[2]], writes=[Wgk[7]])
            S.op("act", lambda e: e.activation(out=Ecol[:], in_=P[3][:, 0:4], func=AF.Exp), reads=[Pk[3]], writes=["Ecol"])
            S.op("dve", lambda e: e.tensor_tensor(out=kd2[:], in0=key[:], in1=Dk[:], op=ALU.mult), reads=[Wgk[2], Wgk[7]], writes=[bk("g", "kd")])
            for m, kd_, v_, xi in (("r", kd1, v1, 0), ("g", kd2, v2, 1)):
                for hh in range(4):
                    blk = slice(hh * 128, (hh + 1) * 128)
                    S.op("pe", lambda e, blk=blk, kd_=kd_, v_=v_, xi=xi: e.matmul(X[xi][:, blk], lhsT=kd_[:, blk], rhs=v_[:, blk], start=True, stop=True), reads=[bk(m, "kd"), bk(m, "v")], writes=[Xk[xi]])
                for hh in range(4):
                    blk = slice(hh * 128, (hh + 1) * 128)
                    sc = g128[hh] if m == "r" else Ecol[:, hh:hh + 1]
                    S.op("dve", lambda e, blk=blk, sc=sc, m=m, xi=xi: e.scalar_tensor_tensor(out=Sst[m][:, blk], in0=Sst[m][:, blk], scalar=sc, in1=X[xi][:, blk], op0=ALU.mult, op1=ALU.add),
                         reads=[Xk[xi], ("S", m), "Ecol"], writes=[("S", m)])

        def ret_a(par, s, tok0):
            proj(par, s, (0, 1, 2, 3))
            Wr, Wrk = WF["r"], WFK["r"]
            S.op("act", lambda e: e.copy(out=Wr[0][:], in_=P[0][:]), reads=[Pk[0]], writes=[Wrk[0]])
            S.op("act", lambda e: e.copy(out=Wr[1][:], in_=P[1][:]), reads=[Pk[1]], writes=[Wrk[1]])
            S.op("act", lambda e: e.copy(out=BB["r"]["v"][:], in_=P[2][:]), reads=[Pk[2]], writes=[bk("r", "v")])
            gate_evac("r", P[3], Pk[3])

        def hg_a(par, s, tok0):
            proj(par, s, (4, 5, 6, 7))
            S.op("act", lambda e: e.copy(out=Wg[0][:], in_=P[0][:]), reads=[Pk[0]], writes=[Wgk[0]])
            S.op("act", lambda e: e.activation(out=Wg[1][:], in_=P[1][:], func=AF.Tanh, scale=0.5), reads=[Pk[1]], writes=[Wgk[1]])
            S.op("act", lambda e: e.copy(out=BB["g"]["v"][:], in_=P[2][:]), reads=[Pk[2]], writes=[bk("g", "v")])
            gate_evac("g", P[3], Pk[3])

        def ret_b1(par, s, tok0):
            rt = rot[(tok0 // 128) % 2]
            kr = ("rot", (tok0 // 128) % 2)
            S.dma("sp", rt[:], rot_d[tok0:tok0 + 128, :, :], writes=[kr])
            Wr, Wrk = WF["r"], WFK["r"]
            Bb = BB["r"]
            qf, kf, qr, kr_ = Wr
            rotary_g(rt, kr, "dve", qf, qr, 0, 1, Wrk[0], Wrk[2], tq[0:2], [("tq", 0), ("tq", 1)])
            rotary_g(rt, kr, "pool", kf, kr_, 2, 3, Wrk[1], Wrk[3], tq[2:4], [("tq", 2), ("tq", 3)])
            S.op("act", lambda e: e.copy(out=Bb["qa"][:], in_=qr[:]), reads=[Wrk[2]], writes=[bk("r", "qa")])
            S.op("dve", lambda e: e.tensor_tensor(out=Bb["qc"][:], in0=qr[:], in1=XI, op=ALU.mult), reads=[Wrk[2], "c32"], writes=[bk("r", "qc")])
            S.op("act", lambda e: e.copy(out=Bb["kb"][:], in_=kr_[:]), reads=[Wrk[3]], writes=[bk("r", "kb")])
            S.op("pool", lambda e: e.tensor_tensor(out=Bb["kd"][:], in0=kr_[:], in1=ZE, op=ALU.mult), reads=[Wrk[3], "c32"], writes=[bk("r", "kd")])

        def hg_b1(par, s, tok0):
            Bb = BB["g"]
            qf, f, key, logf, A, Bm, Cq, Dk = Wg
            hg_gates(f, Wgk[1], key, Wgk[2], logf, Wgk[3])
            S.op("pe", lambda e: e.matmul(P[0][:], lhsT=MA, rhs=logf[:], start=True, stop=True), reads=[Wgk[3], "mats"], writes=[Pk[0]])
            S.op("pe", lambda e: e.matmul(P[1][:], lhsT=MT, rhs=logf[:], start=True, stop=True), reads=[Wgk[3], "mats"], writes=[Pk[1]])
            S.op("pe", lambda e: e.matmul(P[2][:], lhsT=MD, rhs=logf[:], start=True, stop=True), reads=[Wgk[3], "mats"], writes=[Pk[2]])
            for hh in range(4):
                S.op("pe", lambda e, hh=hh: e.matmul(P[3][:, hh:hh + 1], lhsT=logf[:, hh * 128:(hh + 1) * 128], rhs=ones32[:, 0:1], start=True, stop=True),
                     reads=[Wgk[3], "mats"], writes=[Pk[3]])
            S.op("act", lambda e: e.activation(out=A[:], in_=P[0][:], func=AF.Exp), reads=[Pk[0]], writes=[Wgk[4]])
            S.op("act", lambda e: e.activation(out=Bm[:], in_=P[0][:], func=AF.Exp, scale=-1.0), reads=[Pk[0]], writes=[Wgk[5]])
            S.op("act", lambda e: e.activation(out=Cq[:], in_=P[1][:], func=AF.Exp), reads=[Pk[1]], writes=[Wgk[6]])
            S.op("act", lambda e: e.activation(out=Dk[:], in_=P[2][:], func=AF.Exp), reads=[Pk[2]], writes=[Wgk[7]])
            S.op("act", lambda e: e.activation(out=Ecol[:], in_=P[3][:, 0:4], func=AF.Exp), reads=[Pk[3]], writes=["Ecol"])
            S.op("dve", lambda e: e.tensor_tensor(out=Bb["qa"][:], in0=qf[:], in1=A[:], op=ALU.mult), reads=[Wgk[0], Wgk[4]], writes=[bk("g", "qa")])
            S.op("pool", lambda e: e.tensor_tensor(out=Bb["kb"][:], in0=key[:], in1=Bm[:], op=ALU.mult), reads=[Wgk[2], Wgk[5]], writes=[bk("g", "kb")])
            S.op("dve", lambda e: e.tensor_tensor(out=Bb["qc"][:], in0=qf[:], in1=Cq[:], op=ALU.mult), reads=[Wgk[0], Wgk[6]], writes=[bk("g", "qc")])
            S.op("pool", lambda e: e.tensor_tensor(out=Bb["kd"][:], in0=key[:], in1=Dk[:], op=ALU.mult), reads=[Wgk[2], Wgk[7]], writes=[bk("g", "kd")])
            S.op("pe", lambda e: e.matmul(P[0][:], lhsT=MB, rhs=logf[:], start=True, stop=True), reads=[Wgk[3], "mats"], writes=[Pk[0]])
            A2, B2 = f, junk[:, 512:1024]
            S.op("act", lambda e: e.activation(out=A2[:], in_=P[0][:], func=AF.Exp, bias=bq), reads=[Pk[0], "mats", Wgk[2], Wgk[3]], writes=[Wgk[1]])
            S.op("act", lambda e: e.activation(out=B2, in_=P[0][:], func=AF.Exp, scale=-1.0, bias=bkk_), reads=[Pk[0], "mats"], writes=["junk"])
            S.op("dve", lambda e: e.tensor_tensor(out=Bb["qa2"][:], in0=qf[:], in1=A2[:], op=ALU.mult), reads=[Wgk[0], Wgk[1]], writes=[bk("g", "qa2")])
            S.op("pool", lambda e: e.tensor_tensor(out=Bb["kb2"][:], in0=key[:], in1=B2, op=ALU.mult), reads=[Wgk[2], "junk"], writes=[bk("g", "kb2")])

        def ret_b2(par, s, tok0):
            linattn(dmask, lambda hh: g128[hh], "r")
            o4 = X[1]
            ytmp, gate = YT["r"], GATE["r"]
            for hh in range(4):
                S.op("dve", lambda e, hh=hh: e.bn_stats(out=stat[:, hh * 6:(hh + 1) * 6], in_=o4[:, hh * 128:(hh + 1) * 128]), reads=[Xk[1]], writes=["stat"])
            for hh in range(4):
                S.op("dve", lambda e, hh=hh: e.bn_aggr(out=stat[:, 24 + hh * 2:26 + hh * 2], in_=stat[:, hh * 6:(hh + 1) * 6]), reads=["stat"], writes=["stat"])
            var4 = stat[:, 24:32].rearrange("p (h two) -> p h two", two=2)[:, :, 1]
            S.op("act", lambda e: e.activation(out=stat[:, 32:36], in_=var4, func=AF.Ln, bias=EPS), reads=["stat"], writes=["stat"])
            S.op("act", lambda e: e.activation(out=stat[:, 36:40], in_=stat[:, 32:36], func=AF.Exp, scale=-0.5), reads=["stat"], writes=["stat"])
            for hh in range(4):
                S.op("dve", lambda e, hh=hh: e.tensor_scalar(out=ytmp[:, hh * 128:(hh + 1) * 128], in0=o4[:, hh * 128:(hh + 1) * 128],
                                                              scalar1=stat[:, 24 + 2 * hh:25 + 2 * hh], scalar2=stat[:, 36 + hh:37 + hh], op0=ALU.subtract, op1=ALU.mult),
                     reads=[Xk[1], "stat"], writes=[("ytmp", "r")])
            S.op("pool", lambda e: e.tensor_tensor(out=y[:, 0:512], in0=ytmp[:], in1=gate[:], op=ALU.mult), reads=[("ytmp", "r"), ("gate", "r")], writes=[("y", 0)])

        def hg_b2(par, s, tok0):
            linattn(cmask, lambda hh: Ecol[:, hh:hh + 1], "g", second=True)
            o4 = X[1]
            ytmp, gate = YT["g"], GATE["g"]
            S.op("act", lambda e: e.activation(out=junk[:, 0:512], in_=o4[:], func=AF.Square), reads=[Xk[1]], writes=["junk"])
            S.op("dve", lambda e: e.tensor_reduce(out=stat[:, 40:44], in_=junk[:, 0:512].rearrange("p (h d) -> p h d", h=4), axis=AX.X, op=ALU.add), reads=["junk"], writes=["stat2"])
            S.op("act", lambda e: e.activation(out=stat[:, 44:48], in_=stat[:, 40:44], func=AF.Ln, scale=1.0 / 128.0, bias=EPS), reads=["stat2"], writes=["stat2"])
            S.op("act", lambda e: e.activation(out=stat[:, 48:52], in_=stat[:, 44:48], func=AF.Exp, scale=-0.5), reads=["stat2"], writes=["stat2"])
            for hh in range(4):
                S.op("dve", lambda e, hh=hh: e.tensor_scalar(out=ytmp[:, hh * 128:(hh + 1) * 128], in0=o4[:, hh * 128:(hh + 1) * 128],
                                                              scalar1=stat[:, 48 + hh:49 + hh], scalar2=None, op0=ALU.mult),
                     reads=[Xk[1], "stat2"], writes=[("ytmp", "g")])
            S.op("pool", lambda e: e.tensor_tensor(out=y[:, 512:1024], in0=ytmp[:], in1=gate[:], op=ALU.mult), reads=[("ytmp", "g"), ("gate", "g")], writes=[("y", 1)])

        def outproj(tok0, s):
            for c in range(8):
                S.op("pe", lambda e, c=c: e.transpose(out=psT[1][:, c * 128:(c + 1) * 128], in_=y[:, c * 128:(c + 1) * 128], identity=ident[:]),
                     reads=[("y", 0), ("y", 1), "ident"], writes=[("psT", 1)])
            S.op("act", lambda e: e.copy(out=yT[:], in_=psT[1][:].rearrange("p (k t) -> p k t", k=8)), reads=[("psT", 1)], writes=["yT"])
            xr = xres[0]
            kx = ("xres", 0)
            S.dma("sp", xr[:], x_in[tok0:tok0 + 128, :], writes=[kx])
            for n in range(2):
                for c in range(8):
                    S.op("pe", lambda e, c=c, n=n: e.matmul(P[2 + n][:], lhsT=yT[:, c, :], rhs=Wout[:, c, n * 512:(n + 1) * 512], start=(c == 0), stop=(c == 7)),
                         reads=["yT"], writes=[Pk[2 + n]])
                S.op("dve", lambda e, n=n, xr=xr: e.tensor_tensor(out=xr[:, n * 512:(n + 1) * 512], in0=xr[:, n * 512:(n + 1) * 512], in1=P[2 + n][:], op=ALU.add),
                     reads=[Pk[2 + n], kx], writes=[kx])
            S.dma("pool", x_out[tok0:tok0 + 128, :], xr[:], reads=[kx], writes=[("xout", tok0)])

        if prev is not None:
            xp_d, rotp_d, flag_d, Tp = prev
            flg = C.sb(st, [128, 2], F32, "flg")
            S.dma("sp", flg[:], flag_d[:, :], writes=["flg"])
            x_main = x_in
            x_in = xp_d
            A_vec(0)
            A_pe(0)
            for t in range(Tp // TT):
                for s in range(NS):
                    state_only(t % 2, s, t * TT + s * 128, rotp_d)
                    if s == 0 and t + 1 < Tp // TT:
                        A_vec(t + 1)
                if t + 1 < Tp // TT:
                    A_pe(t + 1)
            x_in = x_main
            for m_ in ("r", "g"):
                S.op("dve", lambda e, m_=m_: e.tensor_scalar(out=Sst[m_][:], in0=Sst[m_][:], scalar1=flg[:, 0:1], scalar2=None, op0=ALU.mult), reads=[("S", m_), "flg"], writes=[("S", m_)])
                S.op("act", lambda e, m_=m_: e.copy(out=Sbf[m_][:], in_=Sst[m_][:]), reads=[("S", m_)], writes=[("Sb", m_)])
        A_vec(0)
        A_pe(0)
        for t in range(NT):
            for s in range(NS):
                tok0 = t * TT + s * 128
                par = t % 2
                ret_a(par, s, tok0)
                hg_a(par, s, tok0)
                ret_b1(par, s, tok0)
                hg_b1(par, s, tok0)
                if s == 0 and t + 1 < NT:
                    A_vec(t + 1)
                ret_b2(par, s, tok0)
                hg_b2(par, s, tok0)
                outproj(tok0, s)
            if t + 1 < NT:
                A_pe(t + 1)
        S.barrier()


BRANCHES = ((128, 1), (512, 4), (2048, 16))
NEG = -30000.0


def odd_consts():
    import math

    def bucket(d):
        d = np.maximum(d, 0)
        lr = np.log(np.maximum(d, 1).astype(np.float32) / np.float32(16)) / np.float32(math.log(2048 / 16))
        large = np.minimum(16 + (lr * np.float32(16)).astype(np.int32), 31)
        return np.where(d < 16, d, large)

    OH = np.zeros((32, 1536), np.float32)
    NEGc = np.zeros((8, 1536), np.float32)
    for b, (window, dil) in enumerate(BRANCHES):
        for var in (0, 1):
            for w in range(255):
                u = w + 1
                if var == 0:
                    delta, valid = u - 128, (u - 128) >= 0
                else:
                    delta, valid = u, u <= 128
                col = (b * 2 + var) * 255 + w
                if valid:
                    OH[int(bucket(np.array(delta * dil))), col] = 1.0
                else:
                    NEGc[:, col] = NEG
    J = np.zeros((128, 128), np.float32)
    J[np.arange(128), 127 - np.arange(128)] = 1.0
    return OH, NEGc, J


def odd_phase(C, x_in, x_out, w_in_d, w_out_d, pv, relb_d, OH_d, NEGc_d, J_d, ident_d, scr, T, TT=256, split=None):
    S = C.S
    NS = TT // 128
    NT = T // TT
    SCALE = 128.0 ** -0.5
    with ExitStack() as st:
        ident = C.sb(st, [128, 128], BF16, "ident")
        ones = C.sb(st, [128, 128], BF16, "ones")
        BMh = C.sb(st, [128, 48, 128], BF16, "BMh")
        BMl = C.sb(st, [128, 48, 128], BF16, "BMl")
        pvs = C.sb(st, [128, 8 + 256], F32, "pv")
        gqk = C.sb(st, [128, 128], F32, "gqk")
        stages = [C.sb(st, [128, 1024], F32, "stg") for _ in range(2)]
        Wout = C.sb(st, [128, 8, D], BF16, "Wout")
        xres = [C.sb(st, [128, D], F32, "xres") for _ in range(2)]
        psT = [C.ps(st, [128, D], BF16, "psT") for _ in range(2)]
        P = [C.ps(st, [128, 512], F32, "P") for _ in range(6)]
        Pk = [("P", i) for i in range(6)]

        S.dma("sp", pvs[:], pv[:, :], writes=["gk"])
        S.dma("sp", ident[:], ident_d[:, :], writes=["ident"])
        S.op("pool", lambda e: e.memset(ones[:], 1.0), writes=["ones"])
        S.op("dve", lambda e: e.tensor_tensor(out=gqk[:], in0=pvs[:, 8:136], in1=pvs[:, 136:264], op=ALU.mult), reads=["gk"], writes=["gqk"])

        with ExitStack() as st0:
            relb = C.sb(st0, [32, 8], F32, "relb")
            OH = C.sb(st0, [32, 1536], F32, "OH")
            NEGc = C.sb(st0, [8, 1536], F32, "NEGc")
            Fsb = C.sb(st0, [8, 1536], F32, "Fsb")
            Jm = C.sb(st0, [128, 128], F32, "J")
            Hsb = C.sb(st0, [128, 48, 128], F32, "Hsb")
            BM = C.sb(st0, [128, 48, 128], F32, "BM")
            S.dma("sp", relb[:], relb_d[:, :], writes=["relb"])
            S.dma("sp", OH[:], OH_d[:, :], writes=["OH"])
            S.dma("sp", NEGc[:], NEGc_d[:, :], writes=["NEGc"])
            S.dma("sp", Jm[:], J_d[:, :], writes=["J"])
            for c in range(3):
                S.op("pe", lambda e, c=c: e.matmul(P[c][0:8, :], lhsT=relb[:, :], rhs=OH[:, c * 512:(c + 1) * 512], start=True, stop=True),
                     reads=["relb", "OH"], writes=[Pk[c]])
                S.op("dve", lambda e, c=c: e.tensor_tensor(out=Fsb[:, c * 512:(c + 1) * 512], in0=P[c][0:8, :], in1=NEGc[:, c * 512:(c + 1) * 512], op=ALU.add),
                     reads=[Pk[c], "NEGc"], writes=["Fsb"])
            S.dma("sp", scr["F"][:, :], Fsb[:], reads=["Fsb"], writes=["Fd"])
            Ft = scr["F"].tensor
            for hh in range(8):
                for b in range(3):
                    for var in range(2):
                        idx = (hh * 3 + b) * 2 + var
                        src = bass.AP(Ft, hh * 1536 + (b * 2 + var) * 255, [[1, 128], [1, 128]])
                        S.dma("sp" if idx % 2 else "pool", Hsb[:, idx, :], src, reads=["Fd"], writes=[("Hsb", idx)])
            for g in range(12):
                pb = P[3 + g % 3]
                S.op("pe", lambda e, g=g, pb=pb: e.matmul(pb[:], lhsT=Jm[:], rhs=Hsb[:, 4 * g:4 * g + 4, :].rearrange("p a b -> p (a b)"), start=True, stop=True),
                     reads=[("Hsb", 4 * g + i) for i in range(4)] + ["J"], writes=[Pk[3 + g % 3]])
                S.op("act", lambda e, g=g, pb=pb: e.copy(out=BM[:, 4 * g:4 * g + 4, :].rearrange("p a b -> p (a b)"), in_=pb[:]), reads=[Pk[3 + g % 3]], writes=[("BM", g)])
            for g in range(12):
                sl_ = slice(4 * g, 4 * g + 4)
                S.op("dve", lambda e, sl_=sl_: e.tensor_scalar(out=Hsb[:, sl_, :], in0=BM[:, sl_, :], scalar1=1.0 / SCALE, scalar2=None, op0=ALU.mult),
                     reads=[("BM", g)] + [("Hsb", 4 * g + i) for i in range(4)], writes=[("Hs", g)])
                S.op("act", lambda e, sl_=sl_: e.copy(out=BMh[:, sl_, :], in_=Hsb[:, sl_, :]), reads=[("Hs", g)], writes=[("BMh", g)])
                S.op("dve", lambda e, sl_=sl_: e.tensor_tensor(out=BMl[:, sl_, :], in0=Hsb[:, sl_, :], in1=BMh[:, sl_, :], op=ALU.subtract),
                     reads=[("Hs", g), ("BMh", g)], writes=[("BMl", g)])
            S.barrier()
        BMk = []

        with ExitStack() as st1:
            Win = C.sb(st1, [128, 8, 3072], BF16, "Win")
            bufs = dict(
                xin=[C.sb(st1, [128, D], F32, "xin") for _ in range(2)],
                h=[C.sb(st1, [128, D], BF16, "h") for _ in range(NS)],
                ss=[C.sb(st1, [128, 4], F32, "ss") for _ in range(2)],
                junk=C.sb(st1, [128, D], F32, "junk"),
                hT=[C.sb(st1, [128, 8, TT], BF16, "hT") for _ in range(2)],
                psT=psT,
                ident=ident,
            )
            junk = bufs["junk"]
            junk2 = C.sb(st1, [128, D], F32, "junk2")
            nst = [C.sb(st1, [128, 48], F32, "nst") for _ in range(2)]
            qn = [C.sb(st1, [128, D], BF16, "qn") for _ in range(2)]
            kn = [C.sb(st1, [128, D], BF16, "kn") for _ in range(2)]
            vb = [C.sb(st1, [128, D], BF16, "vb") for _ in range(2)]
            qTt = [C.sb(st1, [128, 8, 128], BF16, "qTt") for _ in range(2)]
            kTt = [C.sb(st1, [128, 8, 128], BF16, "kTt") for _ in range(2)]
            load_weight_bf16(C, w_in_d, Win, stages, 8, 3072, gk=pvs[:, 0:8], colblk=1024, tag="Win")
            Wink = wkeys("Win", 8, 3072, 1024)
            qTd = scr["qT"].rearrange("h d t -> d h t")
            kTd = scr["kT"].rearrange("h d t -> d h t")

            def A_vec(t):
                for s in range(NS):
                    front_vec(C, bufs, x_in, t * TT + s * 128, s, t % 2)

            def A_pe(t):
                for s in range(NS):
                    front_pe(C, bufs, s, t % 2, TT)

            def O1(t, s):
                par = t % 2
                tok0 = t * TT + s * 128
                i2 = (tok0 // 128) % 2
                hT = bufs["hT"][par]
                for g in range(6):
                    for k in range(8):
                        S.op("pe", lambda e, g=g, k=k: e.matmul(P[g][:], lhsT=hT[:, k, s * 128:(s + 1) * 128], rhs=Win[:, k, g * 512:(g + 1) * 512], start=(k == 0), stop=(k == 7)),
                             reads=[("hT", par, s)] + Wink, writes=[Pk[g]])
                ns = nst[i2]
                kns = ("nst", i2)
                for g in range(2):
                    S.op("act", lambda e, g=g: e.activation(out=junk[:, g * 512:(g + 1) * 512], in_=P[g][:], func=AF.Square), reads=[Pk[g]], writes=["junk"])
                S.op("dve", lambda e: e.tensor_reduce(out=ns[:, 0:8], in_=junk[:].rearrange("p (h d) -> p h d", h=8), axis=AX.X, op=ALU.add), reads=["junk"], writes=[kns])
                for g in range(2):
                    S.op("act", lambda e, g=g: e.activation(out=junk2[:, g * 512:(g + 1) * 512], in_=P[2 + g][:], func=AF.Square), reads=[Pk[2 + g]], writes=["junk2"])
                S.op("dve", lambda e: e.tensor_reduce(out=ns[:, 8:16], in_=junk2[:].rearrange("p (h d) -> p h d", h=8), axis=AX.X, op=ALU.add), reads=["junk2"], writes=[kns])
                S.op("act", lambda e: e.activation(out=ns[:, 16:32], in_=ns[:, 0:16], func=AF.Ln, scale=1.0 / 128.0, bias=EPS), reads=[kns], writes=[kns])
                S.op("act", lambda e: e.activation(out=ns[:, 32:48], in_=ns[:, 16:32], func=AF.Exp, scale=-0.5), reads=[kns], writes=[kns])
                q, k_, v = qn[i2], kn[i2], vb[i2]
                for hh in range(8):
                    src = P[hh // 4][:, (hh % 4) * 128:(hh % 4 + 1) * 128]
                    dst = q[:, hh * 128:(hh + 1) * 128]
                    if hh % 2 == 0:
                        S.op("act", lambda e, src=src, dst=dst, hh=hh: e.activation(out=dst, in_=src, func=AF.Copy, scale=ns[:, 32 + hh:33 + hh]), reads=[Pk[hh // 4], kns], writes=[("qn", i2)])
                    else:
                        S.op("dve", lambda e, src=src, dst=dst, hh=hh: e.tensor_scalar(out=dst, in0=src, scalar1=ns[:, 32 + hh:33 + hh], scalar2=None, op0=ALU.mult), reads=[Pk[hh // 4], kns], writes=[("qn", i2)])
                for hh in range(8):
                    src = P[2 + hh // 4][:, (hh % 4) * 128:(hh % 4 + 1) * 128]
                    dst = k_[:, hh * 128:(hh + 1) * 128]
                    S.op("dve", lambda e, src=src, dst=dst, hh=hh: e.scalar_tensor_tensor(out=dst, in0=src, scalar=ns[:, 40 + hh:41 + hh], in1=gqk[:], op0=ALU.mult, op1=ALU.mult),
                         reads=[Pk[2 + hh // 4], kns, "gqk"], writes=[("kn", i2)])
                for g in range(2):
                    S.op("act", lambda e, g=g: e.copy(out=v[:, g * 512:(g + 1) * 512], in_=P[4 + g][:]), reads=[Pk[4 + g]], writes=[("vb", i2)])
                S.dma("pool", scr["v"][tok0:tok0 + 128, :], v[:], reads=[("vb", i2)], writes=[("vd", tok0)])
                for c in range(8):
                    S.op("pe", lambda e, c=c: e.transpose(out=psT[0][:, c * 128:(c + 1) * 128], in_=q[:, c * 128:(c + 1) * 128], identity=ident[:]), reads=[("qn", i2), "ident"], writes=[("psT", 0)])
                S.op("act", lambda e: e.copy(out=qTt[i2][:], in_=psT[0][:].rearrange("p (k t) -> p k t", k=8)), reads=[("psT", 0)], writes=[("qTt", i2)])
                S.dma("sp", qTd[:, :, tok0:tok0 + 128], qTt[i2][:], reads=[("qTt", i2)], writes=[("qTd", tok0)])
                for c in range(8):
                    S.op("pe", lambda e, c=c: e.transpose(out=psT[1][:, c * 128:(c + 1) * 128], in_=k_[:, c * 128:(c + 1) * 128], identity=ident[:]), reads=[("kn", i2), "ident"], writes=[("psT", 1)])
                S.op("dve", lambda e: e.tensor_copy(out=kTt[i2][:], in_=psT[1][:].rearrange("p (k t) -> p k t", k=8)), reads=[("psT", 1)], writes=[("kTt", i2)])
                S.dma("sp", kTd[:, :, tok0:tok0 + 128], kTt[i2][:], reads=[("kTt", i2)], writes=[("kTd", tok0)])

            A_vec(0)
            A_pe(0)
            for t in range(NT):
                for s in range(NS):
                    O1(t, s)
                    if s == 0 and t + 1 < NT:
                        A_vec(t + 1)
                if t + 1 < NT:
                    A_pe(t + 1)
            S.barrier()

        koff = T if split is not None else 0
        Tc = koff + T
        if split is not None:
            flag_d, groups = split
            flg = C.sb(st, [128, 2], F32, "flg")
            S.dma("sp", flg[:], flag_d[:, :], writes=["flg"])
            assert T == 2048
            kT2 = scr["kT"].rearrange("h d t -> (h d) t")
            for p_ in range(2):
                S.cc("AllGather", ALU.bypass, groups, kT2[p_ * 512:(p_ + 1) * 512, :], scr["kTg"][p_], writes=[("kTg", p_)])
                S.cc("AllGather", ALU.bypass, groups, scr["v"][p_ * 1024:(p_ + 1) * 1024, :], scr["vg"][p_], writes=[("vg", p_)])
        with ExitStack() as st2:
            qTh = [C.sb(st2, [128, T], BF16, "qTh") for _ in range(2)]
            kTh = [C.sb(st2, [128, Tc], BF16, "kTh") for _ in range(2)]
            accs = [C.sb(st2, [128, 2, T], F32, "acc") for _ in range(2)]
            oT = C.sb(st2, [128, 8, T], BF16, "oT")
            NV = 8
            SKEW = 3
            Vb = [C.sb(st2, [128, 128], BF16, "Vb") for _ in range(NV)]
            Pb = [C.sb(st2, [128, 256], BF16, "Pb") for _ in range(NV)]
            tmp = [C.sb(st2, [128, 256], F32, "tmp") for _ in range(3)]
            SC = [P[0], P[1], P[2]]
            OB = [P[3], P[4], P[5]]
            load_weight_bf16(C, w_out_d, Wout, stages, 8, D, gk=None, colblk=1024, tag="Wout")
            Woutk = wkeys("Wout", 8, D, 1024)
            its = []
            for hh in range(8):
                for b, (window, dil) in enumerate(BRANCHES):
                    nb = Tc // dil // 128
                    n_own = koff // (dil * 128)
                    for r in range(dil):
                        prev = None
                        for n in range(n_own - (1 if split is not None else 0), nb):
                            rec = dict(hh=hh, b=b, dil=dil, base=r + dil * 128 * n, past=(n < n_own), prev=prev, idx=len(its),
                                       nq=(256 if n + 1 < nb else 128), last_of_head=False)
                            prev = rec["idx"]
                            its.append(rec)
                its[-1]["last_of_head"] = True

            def sl(ap, start, count, step):
                return ap[:, start:start + step * (count - 1) + 1:step] if step > 1 else ap[:, start:start + count]

            def load_head(hh):
                qh, kh = qTh[hh % 2], kTh[hh % 2]
                kq, kk = ("qTh", hh % 2), ("kTh", hh % 2)
                S.dma("sp", qh[:], scr["qT"][hh, :, :], writes=[kq])
                S.dma("sp", kh[:, koff:Tc], scr["kT"][hh, :, :], writes=[kk])
                if split is not None:
                    S.dma("sp", kh[:, 0:koff], scr["kTg"][hh // 4][(hh % 4) * 128:(hh % 4 + 1) * 128, :], reads=[("kTg", hh // 4)], writes=[kk])

            def s0(rec):
                i, hh, dil, base = rec["idx"], rec["hh"], rec["dil"], rec["base"]
                qh, kh = qTh[hh % 2], kTh[hh % 2]
                kq, kk = ("qTh", hh % 2), ("kTh", hh % 2)
                bmh = BMh[:, (hh * 3 + rec["b"]) * 2:(hh * 3 + rec["b"]) * 2 + 2, :].rearrange("p a b -> p (a b)")
                bml = BMl[:, (hh * 3 + rec["b"]) * 2:(hh * 3 + rec["b"]) * 2 + 2, :].rearrange("p a b -> p (a b)")
                vi = i % NV
                Vt, Pt = Vb[vi], Pb[vi]
                kV, kP = ("Vb", vi), ("Pb", vi)
                sc, ksc = SC[i % 3], Pk[i % 3]
                tm, ktm = tmp[i % 3], ("tmp", i % 3)
                q_eng = "pool" if i % 2 else "sp"
                if rec["past"]:
                    r0 = 0
                    while r0 < 128:
                        tr = base + dil * r0
                        pc = tr // 1024
                        cnt = min(128 - r0, (1024 * (pc + 1) - tr + dil - 1) // dil)
                        vsrc = bass.AP(scr["vg"][pc].tensor, (tr - 1024 * pc) * D + hh * 128, [[dil * D, cnt], [1, 128]])
                        S.dma(q_eng, Vt[r0:r0 + cnt, :], vsrc, reads=[("vg", pc)], writes=[kV])
                        r0 += cnt
                    qb = base + dil * 128 - koff
                    lo, hi = 128, 256
                    qsl = sl(qh, qb, 128, dil)
                else:
                    vsrc = bass.AP(scr["v"].tensor, (base - koff) * D + hh * 128, [[dil * D, 128], [1, 128]])
                    S.dma(q_eng, Vt[:], vsrc, writes=[kV])
                    qb = base - koff
                    lo, hi = 0, rec["nq"]
                    qsl = sl(qh, qb, rec["nq"], dil)
                ksl = sl(kh, base, 128, dil)
                S.op("pe", lambda e: e.matmul(sc[:, lo:hi], lhsT=ksl, rhs=qsl, start=True, stop=False), reads=[kq, kk], writes=[ksc])
                S.op("pe", lambda e: e.matmul(sc[:, lo:hi], lhsT=ident[:], rhs=bmh[:, lo:hi], start=False, stop=False), reads=["ident"], writes=[ksc])
                S.op("pe", lambda e: e.matmul(sc[:, lo:hi], lhsT=ident[:], rhs=bml[:, lo:hi], start=False, stop=True), reads=["ident"], writes=[ksc])
                if rec["past"]:
                    S.op("act", lambda e: e.activation(out=Pt[:, lo:hi], in_=sc[:, lo:hi], func=AF.Exp, scale=SCALE, bias=flg[:, 1:2]), reads=[ksc, "flg"], writes=[kP])
                else:
                    S.op("act", lambda e: e.activation(out=Pt[:, lo:hi], in_=sc[:, lo:hi], func=AF.Exp, scale=SCALE), reads=[ksc], writes=[kP])

            def s1(rec):
                if rec["past"]:
                    return
                i, hh, dil, base, b = rec["idx"], rec["hh"], rec["dil"], rec["base"], rec["b"]
                acc = accs[hh % 2]
                kacc = ("acc", hh % 2)
                vi = i % NV
                Vt, Pt = Vb[vi], Pb[vi]
                kV, kP = ("Vb", vi), ("Pb", vi)
                ob, kob = OB[i % 3], Pk[3 + i % 3]
                pr = rec["prev"]
                for j, lhs in enumerate((Vt, ones)):
                    rk_ = [kV] if j == 0 else ["ones"]
                    S.op("pe", lambda e, j=j, lhs=lhs: e.matmul(ob[:, j * 128:(j + 1) * 128], lhsT=lhs[:], rhs=Pt[:, 0:128], start=True, stop=(pr is None)),
                         reads=[kP] + rk_, writes=[kob])
                    if pr is not None:
                        pvi = pr % NV
                        lhs2 = Vb[pvi] if j == 0 else ones
                        rk2 = [("Vb", pvi)] if j == 0 else ["ones"]
                        S.op("pe", lambda e, j=j, lhs2=lhs2, pvi=pvi: e.matmul(ob[:, j * 128:(j + 1) * 128], lhsT=lhs2[:], rhs=Pb[pvi][:, 128:256], start=False, stop=True),
                             reads=[("Pb", pvi)] + rk2, writes=[kob])
                qb = base - koff
                dst = acc[:, :, qb:qb + dil * 127 + 1:dil] if dil > 1 else acc[:, :, qb:qb + 128]
                srcv = ob[:, 0:256].rearrange("p (a b) -> p a b", a=2)
                if b == 0:
                    S.op("act", lambda e: e.copy(out=dst, in_=srcv), reads=[kob], writes=[kacc])
                else:
                    S.op("dve", lambda e: e.tensor_tensor(out=dst, in0=dst, in1=srcv, op=ALU.add), reads=[kob, kacc], writes=[kacc])
                if rec["last_of_head"]:
                    S.op("dve", lambda e: e.reciprocal(out=acc[:, 1, :], in_=acc[:, 1, :]), reads=[kacc], writes=[kacc])
                    S.op("dve", lambda e: e.tensor_tensor(out=oT[:, hh, :], in0=acc[:, 0, :], in1=acc[:, 1, :], op=ALU.mult), reads=[kacc], writes=[("oT", hh)])

            load_head(0)
            for step in range(len(its) + SKEW):
                if step < len(its):
                    rec = its[step]
                    if step + 1 < len(its) and its[step + 1]["hh"] != rec["hh"]:
                        pass
                    if (step == 0 or its[step - 1]["hh"] != rec["hh"]) and rec["hh"] + 1 < 8:
                        load_head(rec["hh"] + 1)
                    s0(rec)
                if step - SKEW >= 0:
                    s1(its[step - SKEW])

            oTk = [("oT", hh) for hh in range(8)]
            for i in range(T // 128):
                tok0 = i * 128
                xr = xres[i % 2]
                kx = ("xres", i % 2)
                S.dma("sp", xr[:], x_in[tok0:tok0 + 128, :], writes=[kx])
                for n in range(2):
                    pb = P[(i % 2) * 2 + n]
                    kpb = Pk[(i % 2) * 2 + n]
                    for hh in range(8):
                        S.op("pe", lambda e, hh=hh, n=n, pb=pb, tok0=tok0: e.matmul(pb[:], lhsT=oT[:, hh, tok0:tok0 + 128], rhs=Wout[:, hh, n * 512:(n + 1) * 512], start=(hh == 0), stop=(hh == 7)),
                             reads=oTk + Woutk, writes=[kpb])
                    S.op("dve", lambda e, n=n, xr=xr, pb=pb: e.tensor_tensor(out=xr[:, n * 512:(n + 1) * 512], in0=xr[:, n * 512:(n + 1) * 512], in1=pb[:], op=ALU.add),
                         reads=[kpb, kx], writes=[kx])
                S.dma("pool", x_out[tok0:tok0 + 128, :], xr[:], reads=[kx], writes=[("xout", tok0)])
            S.barrier()


SEQ = 4096
NMEM = 256
NCORES = 8
TH = SEQ // 2
GROUPS = [[0, 1], [2, 3], [4, 5], [6, 7]]


def _col8(v):
    return np.ascontiguousarray(np.asarray(v, np.float32).reshape(8, 128).T)


def build_program(T=TH):
    nc = bass.Bass("TRN2", target_bir_lowering=False)

    def din(name, shape, dt=F32):
        return nc.dram_tensor(name, list(shape), dt, kind="ExternalInput").ap()

    x = din("x", [T, D])
    xprev = din("xprev", [T, D])
    rotp = din("rotp", [T, 4, 256])
    flag = din("flag", [128, 2])
    mem = din("mem", [NMEM, D])
    ev_w_in = din("ev_w_in", [D, 4096])
    ev_w_out = din("ev_w_out", [D, D])
    od_w_in = din("od_w_in", [D, 3072])
    od_w_out = din("od_w_out", [D, D])
    xa_w_q = [din(f"xa_w_q{l}", [D, D]) for l in range(2)]
    xa_w_kv = [din(f"xa_w_kv{l}", [D, 2 * D]) for l in range(2)]
    xa_w_o = [din(f"xa_w_o{l}", [D, D]) for l in range(2)]
    ffn_w_in = [din(f"ffn_w_in{l}", [D, 2 * DFF]) for l in range(2)]
    ffn_w_out = [din(f"ffn_w_out{l}", [DFF, D]) for l in range(2)]
    pv_even = din("pv_even", [128, 16 + 1536])
    pv_odd = din("pv_odd", [128, 8 + 256])
    pv_xa = [din(f"pv_xa{l}", [128, 16 + 512]) for l in range(2)]
    pv_ffn = [din(f"pv_ffn{l}", [128, 8 + NFF * 4]) for l in range(2)]
    relb = din("rel_bias", [32, 8])
    ident = din("ident", [128, 128], BF16)
    rot = din("rot", [T, 4, 256])
    c32 = din("c32", [128, 2048])
    mats = din("mats", [128, 642])
    OH = din("OH", [32, 1536])
    NEGc = din("NEGc", [8, 1536])
    J = din("J", [128, 128])
    y = nc.dram_tensor("y", [T, D], F32, kind="ExternalOutput").ap()
    xa = nc.dram_tensor("s_xa", [T, D], F32).ap()
    xb = nc.dram_tensor("s_xb", [T, D], F32).ap()
    scr = dict(qT=nc.dram_tensor("s_qT", [8, 128, T], BF16).ap(), kT=nc.dram_tensor("s_kT", [8, 128, T], BF16).ap(),
               v=nc.dram_tensor("s_v", [T, D], BF16).ap(), F=nc.dram_tensor("s_F", [8, 1536], F32).ap(),
               kTg=[nc.dram_tensor(f"s_kTg{p}", [2 * 512, T], BF16).ap() for p in range(2)],
               vg=[nc.dram_tensor(f"s_vg{p}", [2 * 1024, D], BF16).ap() for p in range(2)])
    hsrc = [nc.dram_tensor(f"s_hsrc{l}", [128, 2 * NFF], F32).ap() for l in range(2)]
    hdst = [nc.dram_tensor(f"s_hdst{l}", [256, 2 * NFF], F32).ap() for l in range(2)]
    g128 = even_consts(128)[3]
    with ExitStack() as st:
        S = Sched(nc, st)
        C = Ctx(nc, S)
        even_phase(C, x, xa, ev_w_in, ev_w_out, pv_even, rot, c32, mats, g128, ident, T, prev=(xprev, rotp, flag, T))
        xattn_phase(C, xa, xb, mem, xa_w_q[0], xa_w_kv[0], xa_w_o[0], pv_xa[0], ident, T)
        ffn_phase(C, xb, xa, ffn_w_in[0], ffn_w_out[0], pv_ffn[0], ident, T, halo_x=(flag, hsrc[0], hdst[0], GROUPS))
        odd_phase(C, xa, xb, od_w_in, od_w_out, pv_odd, relb, OH, NEGc, J, ident, scr, T, split=(flag, GROUPS))
        xattn_phase(C, xb, xa, mem, xa_w_q[1], xa_w_kv[1], xa_w_o[1], pv_xa[1], ident, T)
        ffn_phase(C, xa, y, ffn_w_in[1], ffn_w_out[1], pv_ffn[1], ident, T, halo_x=(flag, hsrc[1], hdst[1], GROUPS))
        S.finalize()
    return nc


_CONSTS = None


def _consts():
    global _CONSTS
    if _CONSTS is None:
        import ml_dtypes
        rot, c32, mats, _ = even_consts(SEQ)
        OH, NEGc, J = odd_consts()
        _CONSTS = dict(rot=rot, c32=c32, mats=mats, OH=OH, NEGc=NEGc, J=J,
                       ident=np.eye(128).astype(ml_dtypes.bfloat16))
    return _CONSTS


def kernel(x, mem, mix_norm_g, ev_w_in, ev_ret_norm_g, ev_hg_norm_g, hg_lb_logits, ev_w_out,
           od_w_in, od_q_norm_g, od_k_norm_g, rel_bias, od_w_out,
           xa_norm_g, xa_mem_norm_g, xa_w_q, xa_w_kv, xa_q_norm_g, xa_k_norm_g, xa_w_o,
           ffn_norm_g, ffn_w_in, ffn_conv_w, ffn_conv_b, ffn_w_out):
    f = lambda a: np.ascontiguousarray(np.asarray(a, np.float32))
    x, mem = f(x), f(mem)
    B = x.shape[0]
    shared = dict(_consts())
    shared.update(ev_w_in=f(ev_w_in)[0], ev_w_out=f(ev_w_out)[0], od_w_in=f(od_w_in)[0], od_w_out=f(od_w_out)[0],
                  rel_bias=f(rel_bias))
    pv = np.zeros((128, 16 + 1536), np.float32)
    pv[:, 0:8] = _col8(f(mix_norm_g)[0])
    pv[:, 8:16] = _col8(np.concatenate([f(ev_ret_norm_g)[0].reshape(-1), f(ev_hg_norm_g)[0].reshape(-1)]))
    pv[:, 16:] = f(hg_lb_logits).reshape(1, 1536)
    shared["pv_even"] = pv
    pv = np.zeros((128, 8 + 256), np.float32)
    pv[:, 0:8] = _col8(f(mix_norm_g)[1])
    pv[:, 8:136] = f(od_q_norm_g)[0][None, :]
    pv[:, 136:264] = f(od_k_norm_g)[0][None, :]
    shared["pv_odd"] = pv
    for l in range(2):
        shared[f"xa_w_q{l}"] = f(xa_w_q)[l]
        shared[f"xa_w_kv{l}"] = f(xa_w_kv)[l]
        shared[f"xa_w_o{l}"] = f(xa_w_o)[l]
        shared[f"ffn_w_in{l}"] = f(ffn_w_in)[l]
        shared[f"ffn_w_out{l}"] = f(ffn_w_out)[l]
        pv = np.zeros((128, 16 + 512), np.float32)
        pv[:, 0:8] = _col8(f(xa_norm_g)[l])
        pv[:, 8:16] = _col8(f(xa_mem_norm_g)[l])
        pv[:, 16:272] = f(xa_q_norm_g)[l][None, :]
        pv[:, 272:528] = f(xa_k_norm_g)[l][None, :]
        shared[f"pv_xa{l}"] = pv
        pv = np.zeros((128, 8 + NFF * 4), np.float32)
        pv[:, 0:8] = _col8(f(ffn_norm_g)[l])
        pv[:, 8:8 + NFF * 3] = f(ffn_conv_w)[l].reshape(3, NFF, 128).transpose(2, 1, 0).reshape(128, NFF * 3)
        pv[:, 8 + NFF * 3:] = f(ffn_conv_b)[l].reshape(NFF, 128).T
        shared[f"pv_ffn{l}"] = pv
    nc = build_program(TH)
    rot_full = shared.pop("rot")
    in_maps = []
    for b in range(B):
        for j in range(2):
            m = dict(shared)
            m["x"] = np.ascontiguousarray(x[b, j * TH:(j + 1) * TH])
            m["xprev"] = np.ascontiguousarray(x[b, 0:TH])
            m["mem"] = mem[b]
            m["rot"] = np.ascontiguousarray(rot_full[j * TH:(j + 1) * TH])
            m["rotp"] = np.ascontiguousarray(rot_full[0:TH])
            fl = np.zeros((128, 2), np.float32)
            fl[:, 0] = float(j)
            fl[:, 1] = NEG * (1 - j)
            m["flag"] = fl
            in_maps.append(m)
    res = run_bass_kernel_spmd(nc, in_maps, core_ids=list(range(2 * B)))
    out = np.empty((B, SEQ, D), np.float32)
    for b in range(B):
        for j in range(2):
            out[b, j * TH:(j + 1) * TH] = np.asarray(res.results[2 * b + j]["y"], np.float32)
    return out
```
